# Optimizing a Trainium2 kernel written in Bass

```python
import jax
import jax.numpy as jnp
from jax import lax
import numpy as np


D_MODEL = 2048
BATCH = 2
SEQ = 4096
DEPTH = 1

CTX_LEN = 256
GRID_W = 64
D_MIX = D_MODEL
CONV_DIM = D_MIX // 2
N_HEADS = 4
V_HEAD = (D_MIX - CONV_DIM) // N_HEADS
QK_HEAD = V_HEAD // 2
CHUNK = 128
N_GATES = 4 * N_HEADS
PROJ_SIZES = [CONV_DIM, CONV_DIM, CONV_DIM, N_HEADS * QK_HEAD, N_HEADS * QK_HEAD,
              N_HEADS * V_HEAD, N_HEADS * V_HEAD, N_GATES]
D_PROJ = int(sum(PROJ_SIZES))
PROJ_SPLITS = [int(s) for s in np.cumsum(PROJ_SIZES)[:-1]]
N_EXPERTS = 64
TOP_K = 6
D_EXPERT = 1408
D_SHARED = 1408
ROUTED_SCALE = 2.446
MOE_BLOCK = 256
EPS = 1e-6

kernel_name = 'hybrid_conv_mlstm_moe_dit_block'


def rms_norm(x, g):
    x32 = x.astype(jnp.float32)
    y = x32 * lax.rsqrt(jnp.mean(x32 * x32, axis=-1, keepdims=True) + EPS) * g.astype(jnp.float32)
    return y.astype(x.dtype)


def norm_mod(x, g, shift, scale):
    x32 = x.astype(jnp.float32)
    y = x32 * lax.rsqrt(jnp.mean(x32 * x32, axis=-1, keepdims=True) + EPS) * g.astype(jnp.float32)
    y = y * (1.0 + scale.astype(jnp.float32)) + shift.astype(jnp.float32)
    return y.astype(x.dtype)


def conv3(u, w):
    up = jnp.pad(u, ((0, 0), (1, 1), (0, 0)))
    return up[:, :-2] * w[0] + up[:, 1:-1] * w[1] + up[:, 2:] * w[2]


def short_conv_mixer(bg, cg, hv, w, rows):
    u = cg * hv
    bsz, length, cd = u.shape
    if rows > 0:
        y = conv3(u.reshape(bsz * rows, GRID_W, cd), w).reshape(bsz, length, cd)
    else:
        y = conv3(u, w)
    return bg * y


def to_heads(a, dh):
    bsz, length, _ = a.shape
    return a.reshape(bsz, length, N_HEADS, dh).transpose(0, 2, 1, 3).astype(jnp.float32)


def gate_preacts(g, gate_b):
    bsz, length, _ = g.shape
    g = g.astype(jnp.float32).reshape(bsz, length, 2, 2, N_HEADS) + gate_b.astype(jnp.float32)
    g = jnp.transpose(g, (2, 3, 0, 4, 1))
    return g[:, 0], jax.nn.log_sigmoid(g[:, 1])


def flip_t(a):
    return jnp.flip(a, axis=2)


def empty_state(bsz):
    return (jnp.zeros((bsz, N_HEADS, V_HEAD, QK_HEAD), jnp.float32),
            jnp.zeros((bsz, N_HEADS, QK_HEAD), jnp.float32),
            jnp.full((bsz, N_HEADS), -jnp.inf, jnp.float32))


def mlstm_final_state(k, v, ig, lf):
    b = jnp.cumsum(lf, axis=-1)
    g = b[..., -1:] - b + ig
    m = jnp.max(g, axis=-1)
    w = jnp.exp(g - m[..., None])
    C = jnp.einsum('bhs,bhsv,bhsd->bhvd', w, v, k)
    n = jnp.einsum('bhs,bhsd->bhd', w, k)
    return (C, n, m)


def mlstm_chunkwise(q, k, v, ig, lf, state):
    bsz, nh, length, _ = q.shape
    n_chunks = length // CHUNK

    def to_chunks(a):
        return jnp.moveaxis(a.reshape(bsz, nh, n_chunks, CHUNK, *a.shape[3:]), 2, 0)

    lower = jnp.tril(jnp.ones((CHUNK, CHUNK), bool))

    def step(carry, inp):
        C, n, m = carry
        qc, kc, vc, igc, lfc = inp
        b = jnp.cumsum(lfc, axis=-1)
        inter = b + m[..., None]
        dmat = jnp.where(lower, b[..., :, None] - b[..., None, :] + igc[..., None, :], -jnp.inf)
        m_t = jnp.maximum(inter, jnp.max(dmat, axis=-1))
        w_inter = jnp.exp(inter - m_t)
        sqk = jnp.einsum('bhtd,bhsd->bhts', qc, kc) * jnp.exp(dmat - m_t[..., None])
        num = jnp.einsum('bhts,bhsv->bhtv', sqk, vc) + w_inter[..., None] * jnp.einsum('bhvd,bhtd->bhtv', C, qc)
        den = jnp.sum(sqk, axis=-1) + w_inter * jnp.einsum('bhd,bhtd->bht', n, qc)
        h = num / jnp.maximum(jnp.abs(den), jnp.exp(-m_t))[..., None]
        b_last = b[..., -1]
        g = b_last[..., None] - b + igc
        m_new = jnp.maximum(b_last + m, jnp.max(g, axis=-1))
        a = jnp.exp(b_last + m - m_new)
        ws = jnp.exp(g - m_new[..., None])
        C_new = a[..., None, None] * C + jnp.einsum('bhs,bhsv,bhsd->bhvd', ws, vc, kc)
        n_new = a[..., None] * n + jnp.einsum('bhs,bhsd->bhd', ws, kc)
        return (C_new, n_new, m_new), h

    state, hs = lax.scan(step, state, (to_chunks(q), to_chunks(k), to_chunks(v), to_chunks(ig), to_chunks(lf)))
    h = jnp.moveaxis(hs, 0, 2).reshape(bsz, nh, length, V_HEAD)
    return h, state


def mlstm_output(h, o, head_g):
    h = h * lax.rsqrt(jnp.mean(h * h, axis=-1, keepdims=True) + EPS)
    bsz, nh, length, dv = h.shape
    h = h.transpose(0, 2, 1, 3).reshape(bsz, length, nh * dv) * head_g.astype(jnp.float32)
    return (h * jax.nn.sigmoid(o.astype(jnp.float32))).astype(o.dtype)


def token_mixer(xn, cn, w_in, conv_w, gate_b, head_g, w_out, rows, last):
    px = xn @ w_in
    pc = cn @ w_in
    bx, cx, hx, qx, kx, vx, ox, gx = jnp.split(px, PROJ_SPLITS, axis=-1)
    bc, cc, hc, qc, kc, vc, oc, gc = jnp.split(pc, PROJ_SPLITS, axis=-1)
    qscale = QK_HEAD ** -0.5
    conv_x = short_conv_mixer(bx, cx, hx, conv_w, rows)
    qx_h, kx_h, vx_h = to_heads(qx, QK_HEAD) * qscale, to_heads(kx, QK_HEAD), to_heads(vx, V_HEAD)
    kc_h, vc_h = to_heads(kc, QK_HEAD), to_heads(vc, V_HEAD)
    igx, lfx = gate_preacts(gx, gate_b)
    igc, lfc = gate_preacts(gc, gate_b)
    bsz = xn.shape[0]
    if last:
        st_f = mlstm_final_state(kc_h, vc_h, igc[0], lfc[0])
        st_b = mlstm_final_state(flip_t(kc_h), flip_t(vc_h), flip_t(igc[1]), flip_t(lfc[1]))
    else:
        qc_h = to_heads(qc, QK_HEAD) * qscale
        hcf, st_f = mlstm_chunkwise(qc_h, kc_h, vc_h, igc[0], lfc[0], empty_state(bsz))
        hcb, st_b = mlstm_chunkwise(flip_t(qc_h), flip_t(kc_h), flip_t(vc_h), flip_t(igc[1]), flip_t(lfc[1]), empty_state(bsz))
    hxf, _ = mlstm_chunkwise(qx_h, kx_h, vx_h, igx[0], lfx[0], st_f)
    hxb, _ = mlstm_chunkwise(flip_t(qx_h), flip_t(kx_h), flip_t(vx_h), flip_t(igx[1]), flip_t(lfx[1]), st_b)
    mlstm_x = mlstm_output(hxf + flip_t(hxb), ox, head_g)
    y_lat = jnp.concatenate([conv_x, mlstm_x], axis=-1) @ w_out
    if last:
        return y_lat, None
    conv_c = short_conv_mixer(bc, cc, hc, conv_w, 0)
    mlstm_c = mlstm_output(hcf + flip_t(hcb), oc, head_g)
    y_ctx = jnp.concatenate([conv_c, mlstm_c], axis=-1) @ w_out
    return y_lat, y_ctx


def moe(h, w_router, b_router, we_gate, we_up, we_down, ws_gate, ws_up, ws_down):
    bsz, length, d = h.shape
    t = h.reshape(-1, d)
    n_tok = t.shape[0]
    scores = jax.nn.sigmoid(jnp.dot(t.astype(jnp.float32), w_router.astype(jnp.float32)))
    _, idx = lax.top_k(scores + b_router.astype(jnp.float32), TOP_K)
    sel = jnp.take_along_axis(scores, idx, axis=-1)
    gates = sel / jnp.sum(sel, axis=-1, keepdims=True) * ROUTED_SCALE
    n_assign = n_tok * TOP_K
    flat_e = idx.reshape(n_assign).astype(jnp.int32)
    order = jnp.argsort(flat_e)
    e_sorted = flat_e[order]
    tok_sorted = (order // TOP_K).astype(jnp.int32)
    w_sorted = gates.reshape(n_assign)[order]
    counts = jnp.bincount(flat_e, length=N_EXPERTS).astype(jnp.int32)
    padded = (counts + MOE_BLOCK - 1) // MOE_BLOCK * MOE_BLOCK
    pad_end = jnp.cumsum(padded)
    pad_start = pad_end - padded
    grp_start = jnp.cumsum(counts) - counts
    dest = pad_start[e_sorted] + jnp.arange(n_assign, dtype=jnp.int32) - grp_start[e_sorted]
    n_blocks = -(-n_assign // MOE_BLOCK) + N_EXPERTS
    n_slots = n_blocks * MOE_BLOCK
    slot_tok = jnp.full((n_slots,), n_tok, jnp.int32).at[dest].set(tok_sorted)
    slot_w = jnp.zeros((n_slots,), jnp.float32).at[dest].set(w_sorted)
    block_e = jnp.minimum(jnp.searchsorted(pad_end, jnp.arange(n_blocks, dtype=jnp.int32) * MOE_BLOCK, side='right'), N_EXPERTS - 1)
    t_pad = jnp.concatenate([t, jnp.zeros((1, d), t.dtype)], axis=0)

    def expert_block(args):
        tok, wt, e = args
        xb = t_pad[tok]
        hb = jax.nn.silu(xb @ we_gate[e]) * (xb @ we_up[e])
        return (hb @ we_down[e]) * wt[:, None].astype(xb.dtype)

    yb = lax.map(expert_block, (slot_tok.reshape(n_blocks, MOE_BLOCK), slot_w.reshape(n_blocks, MOE_BLOCK), block_e))
    routed = jax.ops.segment_sum(yb.reshape(n_slots, d), slot_tok, num_segments=n_tok + 1)[:n_tok]
    shared = (jax.nn.silu(t @ ws_gate) * (t @ ws_up)) @ ws_down
    return (routed + shared).reshape(bsz, length, d)


def setup_inputs(seed: int = 0) -> dict:
    key = jax.random.key(seed)
    ks = jax.random.split(key, 24)

    def nrm(k, shape, s):
        return jax.random.normal(k, shape, jnp.float32) * s

    i_bias = nrm(ks[9], (DEPTH, 2, 1, N_HEADS), 0.1)
    f_bias = 3.0 + 3.0 * jax.random.uniform(ks[10], (DEPTH, 2, 1, N_HEADS), jnp.float32)
    return {
        'x': nrm(ks[0], (BATCH, SEQ, D_MODEL), 1.0),
        'c': nrm(ks[1], (BATCH, D_MODEL), 1.0),
        'ctx': nrm(ks[2], (BATCH, CTX_LEN, D_MODEL), 1.0),
        'c_ctx': nrm(ks[3], (D_MODEL,), 1.0),
        'norm1_g': 1.0 + nrm(ks[4], (DEPTH, D_MODEL), 0.02),
        'norm2_g': 1.0 + nrm(ks[5], (DEPTH, D_MODEL), 0.02),
        'w_ada': nrm(ks[6], (DEPTH, D_MODEL, 6 * D_MODEL), 0.5 * D_MODEL ** -0.5),
        'b_ada': nrm(ks[7], (DEPTH, 6 * D_MODEL), 0.02),
        'w_in': nrm(ks[8], (DEPTH, D_MODEL, D_PROJ), D_MODEL ** -0.5),
        'conv_w': nrm(ks[11], (DEPTH, 3, CONV_DIM), 3.0 ** -0.5),
        'gate_b': jnp.concatenate([i_bias, f_bias], axis=2),
        'head_g': 1.0 + nrm(ks[12], (DEPTH, N_HEADS * V_HEAD), 0.02),
        'w_out': nrm(ks[13], (DEPTH, D_MIX, D_MODEL), D_MIX ** -0.5),
        'w_router': nrm(ks[14], (DEPTH, D_MODEL, N_EXPERTS), D_MODEL ** -0.5),
        'b_router': nrm(ks[15], (DEPTH, N_EXPERTS), 0.01),
        'we_gate': nrm(ks[16], (DEPTH, N_EXPERTS, D_MODEL, D_EXPERT), D_MODEL ** -0.5),
        'we_up': nrm(ks[17], (DEPTH, N_EXPERTS, D_MODEL, D_EXPERT), D_MODEL ** -0.5),
        'we_down': nrm(ks[18], (DEPTH, N_EXPERTS, D_EXPERT, D_MODEL), D_EXPERT ** -0.5),
        'ws_gate': nrm(ks[19], (DEPTH, D_MODEL, D_SHARED), D_MODEL ** -0.5),
        'ws_up': nrm(ks[20], (DEPTH, D_MODEL, D_SHARED), D_MODEL ** -0.5),
        'ws_down': nrm(ks[21], (DEPTH, D_SHARED, D_MODEL), D_SHARED ** -0.5),
        'final_g': 1.0 + nrm(ks[22], (D_MODEL,), 0.02),
    }


def reference(x, c, ctx, c_ctx, norm1_g, norm2_g, w_ada, b_ada, w_in, conv_w, gate_b, head_g, w_out,
              w_router, b_router, we_gate, we_up, we_down, ws_gate, ws_up, ws_down, final_g):
    rows = x.shape[1] // GRID_W
    for l in range(DEPTH):
        last = l == DEPTH - 1
        sh1, sc1, gt1, sh2, sc2, gt2 = jnp.split(jax.nn.silu(c) @ w_ada[l] + b_ada[l], 6, axis=-1)
        csh1, csc1, cgt1, csh2, csc2, cgt2 = jnp.split(jax.nn.silu(c_ctx) @ w_ada[l] + b_ada[l], 6, axis=-1)
        xn = norm_mod(x, norm1_g[l], sh1[:, None], sc1[:, None])
        cn = norm_mod(ctx, norm1_g[l], csh1, csc1)
        y_lat, y_ctx = token_mixer(xn, cn, w_in[l], conv_w[l], gate_b[l], head_g[l], w_out[l], rows, last)
        x = x + gt1[:, None] * y_lat
        x = x + gt2[:, None] * moe(norm_mod(x, norm2_g[l], sh2[:, None], sc2[:, None]), w_router[l], b_router[l],
                                   we_gate[l], we_up[l], we_down[l], ws_gate[l], ws_up[l], ws_down[l])
        if not last:
            ctx = ctx + cgt1 * y_ctx
            ctx = ctx + cgt2 * moe(norm_mod(ctx, norm2_g[l], csh2, csc2), w_router[l], b_router[l],
                                   we_gate[l], we_up[l], we_down[l], ws_gate[l], ws_up[l], ws_down[l])
    return rms_norm(x, final_g)
```

```python
import numpy as np
from contextlib import ExitStack
import concourse.bass as bass
import concourse.mybir as mybir
from concourse.bass_utils import run_bass_kernel_spmd

F32 = mybir.dt.float32
BF16 = mybir.dt.bfloat16
I32 = mybir.dt.int32
AF = mybir.ActivationFunctionType
ALU = mybir.AluOpType
AX = mybir.AxisListType

D = 2048
NT_OWN = 8
NT_B = 32
NE = 64
CAP = 512
NST = CAP // 128
EPS = 1e-6
QSCALE = 128 ** -0.5
RSCALE = 2.446
DH = 1408
NHT = 11
ARENA_BYTES = 210432


class Sched:
    def __init__(self, nc, stack):
        self.nc = nc
        self.stack = stack
        self.e = {'pe': nc.tensor, 'act': nc.scalar, 'dve': nc.vector, 'pool': nc.gpsimd, 'sp': nc.sync}
        self.sem = {k: stack.enter_context(nc.semaphore(f"s_{k}")) for k in self.e}
        self.cnt = {k: 0 for k in self.e}
        self.waited = {}
        self.st = {}
        self.dsems = {}
        self.dsem_by_name = {}
        self.epoch = 1

    def _state(self, k):
        s = self.st.get(k)
        if s is None:
            s = self.st[k] = {'w': None, 'r': []}
        return s

    def _deps(self, reads, writes):
        deps = []
        for k in reads:
            s = self._state(k)
            if s['w'] is not None:
                deps.append(s['w'])
        for k in writes:
            s = self._state(k)
            if s['w'] is not None:
                deps.append(s['w'])
            deps.extend(s['r'])
        return deps

    def _emit_waits(self, eng, deps, is_dma):
        need = {}
        for t in deps:
            kind, who, val = t
            if kind == 'e' and who == eng and not is_dma and eng == 'pe':
                continue
            key = (kind, who if kind == 'e' else id(who))
            if key not in need or need[key][1] < val:
                need[key] = (who, val, kind)
        for key, (who, val, kind) in need.items():
            wk = (eng, key)
            if self.waited.get(wk, 0) >= val:
                continue
            self.waited[wk] = val
            sem = self.sem[who] if kind == 'e' else who
            self.e[eng].wait_ge(sem, val)

    def _update(self, tok, reads, writes):
        for k in reads:
            s = self._state(k)
            r = s['r']
            r[:] = [x for x in r if not (x[0] == tok[0] and (x[1] is tok[1] or x[1] == tok[1]))]
            r.append(tok)
        for k in writes:
            s = self._state(k)
            s['w'] = tok
            s['r'] = []

    def op(self, eng, fn, reads=(), writes=()):
        deps = self._deps(reads, writes)
        self._emit_waits(eng, deps, False)
        inst = fn(self.e[eng])
        inst.then_inc(self.sem[eng], 1)
        self.cnt[eng] += 1
        self._update(('e', eng, self.cnt[eng]), reads, writes)

    def dsem(self, name):
        s = self.dsem_by_name.get(name)
        if s is None:
            s = self.stack.enter_context(self.nc.semaphore(name))
            self.dsem_by_name[name] = s
            self.dsems[id(s)] = [s, 0]
        return s

    def dma(self, semname, out, in_, reads=(), writes=(), eng='sp'):
        dsem = self.dsem(semname)
        deps = self._deps(reads, writes)
        self._emit_waits(eng, deps, True)
        self.e[eng].dma_start(out=out, in_=in_).then_inc(dsem, 16)
        rec = self.dsems[id(dsem)]
        rec[1] += 16
        self._update(('d', dsem, rec[1]), reads, writes)

    def barrier(self, new_epoch=False):
        for eng in self.e:
            for other in self.e:
                if (other != eng or eng in ('act', 'dve', 'pool')) and self.cnt[other] > 0:
                    wk = (eng, ('e', other))
                    if self.waited.get(wk, 0) < self.cnt[other]:
                        self.waited[wk] = self.cnt[other]
                        self.e[eng].wait_ge(self.sem[other], self.cnt[other])
            for sid, (s, v) in self.dsems.items():
                if v > 0:
                    wk = (eng, ('d', sid))
                    if self.waited.get(wk, 0) < v:
                        self.waited[wk] = v
                        self.e[eng].wait_ge(s, v)
        self.st = {}
        if new_epoch:
            for k in self.e:
                self.sem[k] = self.stack.enter_context(self.nc.semaphore(f"s_{k}_{self.epoch}"))
                self.cnt[k] = 0
            self.epoch += 1
            self.waited = {wk: v for wk, v in self.waited.items() if wk[1][0] != 'e'}

    def finish(self, eng='sp'):
        deps = []
        for k, s in self.st.items():
            if s['w'] is not None:
                deps.append(s['w'])
            deps.extend(s['r'])
        self._emit_waits(eng, deps, True)


class Arena:
    def __init__(self, ap):
        self.ap = ap
        self.top = 0

    def alloc(self, nbytes):
        off = (self.top + 31) // 32 * 32
        self.top = off + nbytes
        assert self.top <= ARENA_BYTES, f"arena overflow {self.top}"
        return off

    def bf(self, off, n, parts=128):
        return self.ap[0:parts, off // 2: off // 2 + n]

    def f32(self, off, n, parts=128):
        return self.ap[0:parts, off // 2: off // 2 + 2 * n].bitcast(F32)

    def i32(self, off, n, parts=128):
        return self.ap[0:parts, off // 2: off // 2 + 2 * n].bitcast(I32)


def build_nc(stage='full'):
    nc = bass.Bass("TRN2", target_bir_lowering=False)

    def din(name, shape):
        return nc.dram_tensor(name, list(shape), F32, kind="ExternalInput").ap()

    xb = din("xb", [4096, D]); xo = din("xo", [1024, D]); cx = din("cx", [256, D])
    cT = din("cT", [128, 16, 2])
    wada = din("wada", [6, 16, 128, D]); bada2 = din("bada2", [2, 6 * D])
    g1_2 = din("g1_2", [2, D]); g2_2 = din("g2_2", [2, D])
    wkvg = din("wkvg", [16, 128, 1552])
    wtok = din("wtok", [6, 4, 128, 4, 512]); wg16 = din("wg16", [128, 16, 16])
    wconv = din("wconv", [8, 3, 128, 16, 128]); cw = din("cw", [128, 8, 3])
    gb = din("gb", [128, 16]); hg = din("hg", [128, 1024])
    wout = din("wout", [4, 4, 128, 4, 512])
    wr = din("wr", [128, 16, 64]); br = din("br", [128, 64])
    NEd = NE if stage == 'full' else 1
    STAGE = stage
    weg = din("weg", [NEd, NHT, 128, 16, 128]); weu = din("weu", [NEd, NHT, 128, 16, 128])
    wed = din("wed", [NEd, 8, 128, NHT, 256])
    wsg = din("wsg", [NHT, 128, 16, 128]); wsu = din("wsu", [NHT, 128, 16, 128]); wsd = din("wsd", [8, 128, NHT, 256])
    fg = din("fg", [128, D]); flags = din("flags", [128, 68]); shpos = din("shpos", [128, 8, 2])
    out = nc.dram_tensor("out", [1024, D], F32, kind="ExternalOutput").ap()

    with ExitStack() as st:
        S = Sched(nc, st)
        arena_t = st.enter_context(nc.sbuf_tensor("arena", [128, ARENA_BYTES // 2], BF16))
        A = Arena(arena_t)
        banks = [st.enter_context(nc.psum_tensor(f"bank{i}", [128, 512], F32)) for i in range(8)]

        def bk(i):
            return banks[i]

        def bkbf(i):
            return banks[i][:, :].bitcast(BF16)

        o_identb = A.alloc(256); identb = A.bf(o_identb, 128)
        o_iota = A.alloc(2048); iota = A.f32(o_iota, 512)
        o_stage = A.alloc(16384)
        PERSIST = A.top
        o_identf = A.alloc(512); identf = A.f32(o_identf, 128)
        o_Ub = A.alloc(256); Ub = A.bf(o_Ub, 128)
        o_Lb = A.alloc(256); Lb = A.bf(o_Lb, 128)
        o_Uf = A.alloc(512); Uf = A.f32(o_Uf, 128)
        o_Lf = A.alloc(512); Lf = A.f32(o_Lf, 128)
        o_onesf = A.alloc(512); onesf = A.f32(o_onesf, 128)
        o_onesb = A.alloc(256); onesb = A.bf(o_onesb, 128)
        o_sel0 = A.alloc(512); sel0 = A.f32(o_sel0, 128)
        o_sel1 = A.alloc(512); sel1 = A.f32(o_sel1, 128)
        o_flags = A.alloc(68 * 4); flg = A.f32(o_flags, 68)
        o_gb = A.alloc(64); gbt = A.f32(o_gb, 16)
        o_tmpi = A.alloc(2048); tmpi = A.i32(o_tmpi, 512)
        o_gs = A.alloc(16 * 4 * 8); gsm = A.f32(o_gs, 128)
        MIXBASE = A.top
        M = MIXBASE

        def tri(ap_, pattern, chm, cmp, key):
            S.op('pool', lambda e: e.memset(ap_, 1.0), writes=[key])
            S.op('pool', lambda e: e.affine_select(out=ap_, in_=ap_, pattern=pattern, compare_op=cmp, fill=0.0,
                                                   base=0, channel_multiplier=chm), reads=[key], writes=[key])

        tri(identb, [[-1, 128]], 1, ALU.is_equal, 'identb')
        tri(identf, [[-1, 128]], 1, ALU.is_equal, 'identf')
        tri(Ub, [[1, 128]], -1, ALU.is_ge, 'Ub')
        tri(Uf, [[1, 128]], -1, ALU.is_ge, 'Uf')
        tri(Lb, [[-1, 128]], 1, ALU.is_ge, 'Lb')
        tri(Lf, [[-1, 128]], 1, ALU.is_ge, 'Lf')
        S.op('pool', lambda e: e.memset(onesf, 1.0), writes=['onesf'])
        S.op('pool', lambda e: e.memset(onesb, 1.0), writes=['onesb'])
        S.op('pool', lambda e: e.memset(sel0, 1.0), writes=['sel0'])
        S.op('pool', lambda e: e.affine_select(out=sel0, in_=sel0, pattern=[[0, 128]], compare_op=ALU.is_equal, fill=0.0,
                                               base=0, channel_multiplier=1), reads=['sel0'], writes=['sel0'])
        S.op('pool', lambda e: e.memset(sel1, 1.0), writes=['sel1'])
        S.op('pool', lambda e: e.affine_select(out=sel1, in_=sel1, pattern=[[0, 128]], compare_op=ALU.is_equal, fill=0.0,
                                               base=-1, channel_multiplier=1), reads=['sel1'], writes=['sel1'])
        S.op('pool', lambda e: e.iota(tmpi, pattern=[[1, 512]], base=0, channel_multiplier=0), writes=['tmpi'])
        S.op('pool', lambda e: e.tensor_copy(out=iota, in_=tmpi), reads=['tmpi'], writes=['iota'])
        S.dma('d_flags', flg, flags[:, :], writes=['flags'])
        S.dma('d_gb', gbt, gb[:, :], writes=['gb'])

        stage_n = [0]

        def stage_piece(src_ap, nelem, nslots=2, slot_elems=2048):
            i = stage_n[0] % nslots
            stage_n[0] += 1
            key = f"stg{nslots}_{i}"
            ap_ = A.f32(o_stage + i * slot_elems * 4, nelem)
            S.dma(f"d_{key}", ap_, src_ap, writes=[key])
            return ap_, key

        o_row = None

        def mod_vec(j, s2, rowbuf, key):
            for kt in range(16):
                pc, pk = stage_piece(wada[j, kt], 2048)
                for c4 in range(4):
                    S.op('pe', lambda e: e.matmul(bk(c4)[0:2, :], lhsT=s2[:, kt, :], rhs=pc[:, c4 * 512:(c4 + 1) * 512],
                                                  start=(kt == 0), stop=(kt == 15)),
                         reads=[pk, 's2'], writes=[f'B{c4}'])
            for c4 in range(4):
                S.op('dve', lambda e: e.tensor_copy(out=rowbuf[0:2, c4 * 512:(c4 + 1) * 512], in_=bk(c4)[0:2, :]),
                     reads=[f'B{c4}'], writes=[key])

        def bcast(dst, dkey, row, rkey, sel, skey):
            for c4 in range(4):
                S.op('pe', lambda e: e.matmul(bk(4 + c4)[:, :], lhsT=sel[0:2, :], rhs=row[0:2, c4 * 512:(c4 + 1) * 512],
                                              start=True, stop=True), reads=[rkey, skey], writes=[f'B{4 + c4}'])
                S.op('act', lambda e: e.copy(out=dst[:, c4 * 512:(c4 + 1) * 512], in_=bk(4 + c4)[:, :]),
                     reads=[f'B{4 + c4}'], writes=[dkey])

        A.top = M
        o_bc = [A.alloc(8192) for _ in range(4)]
        bc = [A.f32(o, 2048) for o in o_bc]
        tmpl = {}

        def ada_temps(base, gsrc):
            A.top = base
            tmpl['rows'] = [A.f32(A.alloc(8192), 2048) for _ in range(2)]
            tmpl['b2'] = A.f32(A.alloc(8192), 2048)
            tmpl['g12'] = A.f32(A.alloc(8192), 2048)
            tmpl['s2'] = A.f32(A.alloc(128), 32).rearrange("p (k r) -> p k r", r=2)
            S.dma('d_s2', tmpl['s2'], cT[:, :, :], writes=['s2'])
            S.dma('d_g12', tmpl['g12'][0:2, :], gsrc[:, :], writes=['g12'])
            S.op('act', lambda e: e.activation(out=tmpl['s2'], in_=tmpl['s2'], func=AF.Silu), reads=['s2'], writes=['s2'])

        ada_temps(M + 32768, g1_2)
        rows = tmpl['rows']; g12 = tmpl['g12']

        def mod_row(j, rowbuf, key):
            b2 = tmpl['b2']
            S.dma('d_b2', b2[0:2, :], bada2[:, j * D:(j + 1) * D], reads=[], writes=['b2'])
            mod_vec(j, tmpl['s2'], rowbuf, key)
            S.op('dve', lambda e: e.tensor_tensor(out=rowbuf[0:2, :], in0=rowbuf[0:2, :], in1=b2[0:2, :], op=ALU.add),
                 reads=[key, 'b2'], writes=[key])

        mod_row(0, rows[0], 'row0')
        mod_row(1, rows[1], 'row1')
        S.op('dve', lambda e: e.scalar_tensor_tensor(out=rows[1][0:2, :], in0=rows[1][0:2, :], scalar=1.0, in1=g12[0:2, :],
                                                     op0=ALU.add, op1=ALU.mult), reads=['row1', 'g12'], writes=['row1'])
        bcast(bc[0], 'bc0', rows[1], 'row1', sel0, 'sel0')
        bcast(bc[1], 'bc1', rows[0], 'row0', sel0, 'sel0')
        bcast(bc[2], 'bc2', rows[1], 'row1', sel1, 'sel1')
        bcast(bc[3], 'bc3', rows[0], 'row0', sel1, 'sel1')

        if stage == 'ada':
            out_t = out.rearrange("(n p) f -> n p f", p=128)
            for t in range(4):
                S.dma(f'd_out{t}', out_t[t], bc[t], reads=[f'bc{t}'], writes=[f'out{t}'])
            S.finish('sp')
            return nc
        S.barrier()
        A.top = M + 32768
        SCANBASE = A.top
        o_t1 = A.alloc(8192); t1 = A.f32(o_t1, 2048)
        o_ss = A.alloc(64); ssb = A.f32(o_ss, 16)
        o_xn = [A.alloc(4096) for _ in range(2)]
        xnb = [A.bf(o, 2048) for o in o_xn]
        nm_n = [0]

        def norm_mod(xp, xkey, gm, gmkey, sh, shkey, dst, dkey):
            i = nm_n[0] % 8
            nm_n[0] += 1
            ss = ssb[:, i:i + 1]
            sk = f'ss{i}'
            S.op('act', lambda e: e.activation(out=t1, in_=xp, func=AF.Square, accum_out=ss), reads=[xkey], writes=['t1', sk])
            S.op('dve', lambda e: e.tensor_scalar(out=ss, in0=ss, scalar1=1.0 / D, scalar2=EPS, op0=ALU.mult, op1=ALU.add),
                 reads=[sk], writes=[sk])
            S.op('act', lambda e: e.activation(out=ss, in_=ss, func=AF.Ln), reads=[sk], writes=[sk])
            S.op('act', lambda e: e.activation(out=ss, in_=ss, func=AF.Exp, scale=-0.5), reads=[sk], writes=[sk])
            S.op('dve', lambda e: e.scalar_tensor_tensor(out=t1, in0=xp, scalar=ss, in1=gm, op0=ALU.mult, op1=ALU.mult),
                 reads=[xkey, sk, gmkey], writes=['t1'])
            S.op('pool', lambda e: e.tensor_tensor(out=dst, in0=t1, in1=sh, op=ALU.add), reads=['t1', shkey], writes=[dkey])

        def transpose16(src, skey, dst3, dkeys, b0, b1, evac='act'):
            for half, b in ((0, b0), (1, b1)):
                pb = bkbf(b)
                for k8 in range(8):
                    kt = half * 8 + k8
                    S.op('pe', lambda e: e.transpose(out=pb[:, k8 * 128:(k8 + 1) * 128], in_=src[:, kt * 128:(kt + 1) * 128],
                                                     identity=identb), reads=[skey, 'identb'], writes=[f'B{b}'])
                dk = dkeys if isinstance(dkeys, list) else [dkeys]
                wk = dk[half * 8:(half + 1) * 8] if len(dk) == 16 else dk
                eng = evac if half == 0 else ('dve' if evac == 'act' else 'act')
                if eng == 'act':
                    S.op('act', lambda e: e.copy(out=dst3[:, half * 8:(half + 1) * 8, :],
                                                 in_=pb.rearrange("p (k t) -> p k t", t=128)), reads=[f'B{b}'], writes=wk)
                else:
                    S.op('dve', lambda e: e.tensor_copy(out=dst3[:, half * 8:(half + 1) * 8, :],
                                                        in_=pb.rearrange("p (k t) -> p k t", t=128)), reads=[f'B{b}'], writes=wk)

        o_wk = A.alloc(16 * 512 * 2); Wk = A.bf(o_wk, 16 * 512).rearrange("p (k n) -> p k n", n=512)
        o_wv = A.alloc(16 * 1024 * 2); Wv = A.bf(o_wv, 16 * 1024).rearrange("p (k n) -> p k n", n=1024)
        o_wg = A.alloc(16 * 16 * 2); Wg16 = A.bf(o_wg, 256).rearrange("p (k n) -> p k n", n=16)
        for kt in range(16):
            pc, pk = stage_piece(wkvg[kt], 1552)
            S.op('act', lambda e: e.copy(out=Wk[:, kt, :], in_=pc[:, 0:512]), reads=[pk], writes=['Wk'])
            S.op('dve', lambda e: e.tensor_copy(out=Wv[:, kt, :], in_=pc[:, 512:1536]), reads=[pk], writes=['Wv'])
            S.op('pool', lambda e: e.tensor_copy(out=Wg16[:, kt, :], in_=pc[:, 1536:1552]), reads=[pk], writes=['Wg16'])
        o_xnT = [A.alloc(4096) for _ in range(2)]
        xnT = [A.bf(o, 2048).rearrange("p (k t) -> p k t", t=128) for o in o_xnT]
        o_CT = A.alloc(2 * 4 * 256 * 4); CT = A.f32(o_CT, 2048).rearrange("p (d h v) -> p d h v", d=2, h=4)
        o_nst = A.alloc(32); nst = A.f32(o_nst, 8).rearrange("p (d h) -> p d h", d=2)
        o_CS = A.alloc(2 * 4 * 256 * 4); CSv = A.f32(o_CS, 2048).rearrange("p (d h v) -> p d h v", d=2, h=4)
        o_ns = A.alloc(32); nsv = A.f32(o_ns, 8).rearrange("p (d h) -> p d h", d=2)
        o_ks = [A.alloc(1024) for _ in range(2)]
        ksb = [A.bf(o, 512).rearrange("p (h d) -> p h d", h=4) for o in o_ks]
        o_v1 = [A.alloc(2048) for _ in range(2)]
        v1b = [A.bf(o, 1024).rearrange("p (h v) -> p h v", h=4) for o in o_v1]
        for z, zk in ((CT, 'CT'), (CSv, 'CS')):
            S.op('pool', lambda e: e.memset(z.rearrange("p d h v -> p (d h v)"), 0.0), writes=[zk + '0', zk + '1'])
        S.op('pool', lambda e: e.memset(nst.rearrange("p d h -> p (d h)"), 0.0), writes=['n0', 'n1'])
        S.op('pool', lambda e: e.memset(nsv.rearrange("p d h -> p (d h)"), 0.0), writes=['ns0', 'ns1'])

        gs_n = [0]

        def gates_dir(gpre, gkey, d):
            i = gs_n[0] % 2
            gs_n[0] += 1
            base = i * 64
            sp = gsm[:, base + 0:base + 4]; wq = gsm[:, base + 4:base + 8]; wk_ = gsm[:, base + 8:base + 12]
            aa = gsm[:, base + 12:base + 16]; tmp = gsm[:, base + 16:base + 20]
            k_ = f'gs{i}'
            ipre = gpre[:, d * 8 + 0:d * 8 + 4]
            fpre = gpre[:, d * 8 + 4:d * 8 + 8]
            S.op('act', lambda e: e.activation(out=sp, in_=fpre, func=AF.Exp, scale=-1.0), reads=[gkey], writes=[k_])
            S.op('dve', lambda e: e.tensor_scalar(out=sp, in0=sp, scalar1=1.0, scalar2=None, op0=ALU.add), reads=[k_], writes=[k_])
            S.op('act', lambda e: e.activation(out=sp, in_=sp, func=AF.Ln), reads=[k_], writes=[k_])
            tri_ = Uf if d == 0 else Lf
            S.op('pe', lambda e: e.matmul(bk(7)[:, 0:4], lhsT=tri_, rhs=sp, start=True, stop=True), reads=[k_, 'Uf', 'Lf'], writes=['B7'])
            S.op('pe', lambda e: e.matmul(bk(7)[:, 4:8], lhsT=onesf, rhs=sp, start=True, stop=True), reads=[k_, 'onesf'], writes=['B7'])
            S.op('act', lambda e: e.activation(out=wq, in_=bk(7)[:, 0:4], func=AF.Exp, scale=-1.0), reads=['B7'], writes=[k_])
            S.op('dve', lambda e: e.tensor_tensor(out=tmp, in0=bk(7)[:, 0:4], in1=ipre, op=ALU.add), reads=['B7', gkey], writes=[k_])
            S.op('act', lambda e: e.activation(out=aa, in_=bk(7)[:, 4:8], func=AF.Exp, scale=-1.0), reads=['B7'], writes=[k_])
            S.op('act', lambda e: e.activation(out=wk_, in_=tmp, func=AF.Exp), reads=[k_], writes=[k_])
            return wq, wk_, aa, k_

        def state_update(d, ks, kskey, v1, v1key, aa, akey, flag_idx):
            for h in range(4):
                b = 3 + h // 2
                S.op('pe', lambda e: e.matmul(bk(b)[:, (h % 2) * 256:(h % 2) * 256 + 256], lhsT=ks[:, h, :], rhs=v1[:, h, :],
                                              start=True, stop=True), reads=[kskey, v1key], writes=[f'B{b}'])
            for h in range(4):
                S.op('pe', lambda e: e.matmul(bk(7)[:, 8 + h:9 + h], lhsT=ks[:, h, :], rhs=onesb[:, 0:1], start=True, stop=True),
                     reads=[kskey, 'onesb'], writes=['B7'])
            ck = f'CT{d}'
            for hp in range(2):
                S.op('dve', lambda e: e.tensor_tensor(out=CT[:, d, 2 * hp:2 * hp + 2, :],
                                                      in0=CT[:, d, 2 * hp:2 * hp + 2, :],
                                                      in1=bk(3 + hp)[:, :].rearrange("p (h v) -> p h v", h=2), op=ALU.add),
                     reads=[f'B{3 + hp}', ck], writes=[ck])
            for h in range(4):
                S.op('pool', lambda e: e.tensor_scalar(out=CT[:, d, h, :], in0=CT[:, d, h, :], scalar1=aa[:, h:h + 1], scalar2=None,
                                                       op0=ALU.mult), reads=[ck, akey], writes=[ck])
            nk = f'n{d}'
            S.op('dve', lambda e: e.tensor_tensor(out=nst[:, d, :], in0=nst[:, d, :], in1=bk(7)[:, 8:12], op=ALU.add),
                 reads=['B7', nk], writes=[nk])
            S.op('dve', lambda e: e.tensor_tensor(out=nst[:, d, :], in0=nst[:, d, :], in1=aa, op=ALU.mult), reads=[nk, akey], writes=[nk])
            if flag_idx is not None:
                fl = flg[:, flag_idx:flag_idx + 1]
                S.op('dve', lambda e: e.scalar_tensor_tensor(out=CSv[:, d].rearrange("p h v -> p (h v)"),
                                                              in0=CT[:, d].rearrange("p h v -> p (h v)"), scalar=fl,
                                                              in1=CSv[:, d].rearrange("p h v -> p (h v)"), op0=ALU.mult, op1=ALU.add),
                     reads=[ck, 'flags', f'CS{d}'], writes=[f'CS{d}'])
                S.op('dve', lambda e: e.scalar_tensor_tensor(out=nsv[:, d, :], in0=nst[:, d, :], scalar=fl, in1=nsv[:, d, :],
                                                              op0=ALU.mult, op1=ALU.add), reads=[nk, 'flags', f'ns{d}'], writes=[f'ns{d}'])

        sc_n = [0]

        def scan_tile(src_rows, gm, gmk, sh, shk, d, flag_idx):
            i = sc_n[0] % 2
            sc_n[0] += 1
            xp, xk = stage_piece(src_rows, 2048)
            norm_mod(xp, xk, gm, gmk, sh, shk, xnb[i], f'xnb{i}')
            transpose16(xnb[i], f'xnb{i}', xnT[i], f'xnT{i}', 5, 6)
            xt = xnT[i]; xtk = f'xnT{i}'
            for kt in range(16):
                S.op('pe', lambda e: e.matmul(bk(0)[:, :], lhsT=xt[:, kt, :], rhs=Wk[:, kt, :], start=(kt == 0), stop=(kt == 15)),
                     reads=[xtk, 'Wk'], writes=['B0'])
            for vh in range(2):
                for kt in range(16):
                    S.op('pe', lambda e: e.matmul(bk(1 + vh)[:, :], lhsT=xt[:, kt, :], rhs=Wv[:, kt, vh * 512:(vh + 1) * 512],
                                                  start=(kt == 0), stop=(kt == 15)), reads=[xtk, 'Wv'], writes=[f'B{1 + vh}'])
            for kt in range(16):
                S.op('pe', lambda e: e.matmul(bk(7)[:, 16:32], lhsT=xt[:, kt, :], rhs=Wg16[:, kt, :], start=(kt == 0), stop=(kt == 15)),
                     reads=[xtk, 'Wg16'], writes=['B7'])
            gp = gsm[:, 40 + i * 16 - 40 * 0: 40 + i * 16 + 16] if False else gsm[:, 96 + i * 16:96 + i * 16 + 16]
            gk = f'gpre{i}'
            S.op('dve', lambda e: e.tensor_tensor(out=gp, in0=bk(7)[:, 16:32], in1=gbt, op=ALU.add), reads=['B7', 'gb'], writes=[gk])
            wq, wk_, aa, gsk = gates_dir(gp, gk, d)
            ks = ksb[i]; v1 = v1b[i]
            for h in range(4):
                S.op('dve', lambda e: e.tensor_scalar(out=ks[:, h, :], in0=bk(0)[:, h * 128:(h + 1) * 128], scalar1=wk_[:, h:h + 1],
                                                      scalar2=None, op0=ALU.mult), reads=['B0', gsk], writes=[f'ks{i}'])
            for vh in range(2):
                S.op('act', lambda e: e.copy(out=v1[:, 2 * vh:2 * vh + 2, :], in_=bk(1 + vh)[:, :].rearrange("p (h v) -> p h v", h=2)),
                     reads=[f'B{1 + vh}'], writes=[f'v1{i}'])
            state_update(d, ks, f'ks{i}', v1, f'v1{i}', aa, gsk, flag_idx)

        ctx_t = cx.rearrange("(n p) f -> n p f", p=128)
        xb_t = xb.rearrange("(n p) f -> n p f", p=128)
        fwd = [(ctx_t[0], 2, 3, None), (ctx_t[1], 2, 3, 0)] + [(xb_t[j], 0, 1, j + 1) for j in range(NT_B)]
        bwd = [(ctx_t[1], 2, 3, None), (ctx_t[0], 2, 3, 33)] + [(xb_t[NT_B - 1 - j], 0, 1, 34 + j) for j in range(NT_B)]
        if stage.startswith('scan'):
            nst_ = int(stage[4:])
            out_t = out.rearrange("(n p) f -> n p f", p=128)
            if nst_ == 0:
                xp, xk = stage_piece(ctx_t[0], 2048)
                norm_mod(xp, xk, bc[2], 'bc2', bc[3], 'bc3', xnb[0], 'xnb0')
                transpose16(xnb[0], 'xnb0', xnT[0], 'xnT0', 5, 6)
                S.op('dve', lambda e: e.tensor_copy(out=t1, in_=xnT[0].rearrange("p k t -> p (k t)")), reads=['xnT0'], writes=['t1'])
                S.dma('d_out0', out_t[0], t1, reads=['t1'], writes=['out0'])
            else:
                for stp in range(nst_):
                    for d, lst in ((0, fwd), (1, bwd)):
                        src, gi, si, fi = lst[stp]
                        scan_tile(src, bc[gi], f'bc{gi}', bc[si], f'bc{si}', d, 0)
                S.dma('d_out0', out_t[0], CT.rearrange("p d h v -> p (d h v)"), reads=['CT0', 'CT1'], writes=['out0'])
            S.finish('sp')
            return nc
        for stp in range(len(fwd)):
            for d, lst in ((0, fwd), (1, bwd)):
                src, gi, si, fi = lst[stp]
                scan_tile(src, bc[gi], f'bc{gi}', bc[si], f'bc{si}', d, fi)

        S.barrier()
        A.top = M + 16384
        o_wg2 = A.alloc(512); Wg16b = A.bf(o_wg2, 256).rearrange("p (k n) -> p k n", n=16)
        o_CTs = A.alloc(8192); CT2 = A.f32(o_CTs, 2048).rearrange("p (d h v) -> p d h v", d=2, h=4)
        o_n2 = A.alloc(32); n2 = A.f32(o_n2, 8).rearrange("p (d h) -> p d h", d=2)
        o_cw = A.alloc(96); cwt = A.f32(o_cw, 24).rearrange("p (c j) -> p c j", j=3)
        o_hg = A.alloc(4096); hgt = A.f32(o_hg, 1024)
        assert A.top <= M + 32768
        S.op('dve', lambda e: e.tensor_copy(out=CT2.rearrange("p d h v -> p (d h v)"), in_=CSv.rearrange("p d h v -> p (d h v)")),
             writes=['CT0', 'CT1'])
        S.op('dve', lambda e: e.tensor_copy(out=n2.rearrange("p d h -> p (d h)"), in_=nsv.rearrange("p d h -> p (d h)")), writes=['n0', 'n1'])
        S.op('dve', lambda e: e.tensor_copy(out=Wg16b.rearrange("p k n -> p (k n)"), in_=Wg16.rearrange("p k n -> p (k n)")), writes=['Wg16'])
        S.barrier()
        CTo, no_ = CT2, n2
        A.top = o_xn[1] + 4096
        OWN = A.top
        o_q = A.alloc(8 * 512 * 2); qst = A.bf(o_q, 4096).rearrange("p (t n) -> p t n", t=8)
        o_k = A.alloc(8 * 512 * 2); kst = A.bf(o_k, 4096).rearrange("p (t n) -> p t n", t=8)
        o_v = A.alloc(8 * 1024 * 2); vst = A.bf(o_v, 8192).rearrange("p (t n) -> p t n", t=8)
        o_so = A.alloc(8 * 1024 * 2); sgo = A.bf(o_so, 8192).rearrange("p (t n) -> p t n", t=8)
        o_gp = A.alloc(8 * 16 * 4); gpst = A.f32(o_gp, 128).rearrange("p (t n) -> p t n", t=8)
        o_cv = A.alloc(8 * 1024 * 2); convT = A.bf(o_cv, 8192).rearrange("p (c t) -> p c t", c=8)
        S.dma('d_cw', cwt, cw[:, :, :], writes=['cw'])
        S.dma('d_hg', hgt, hg[:, :], writes=['hg'])
        O1 = A.top
        o_xT = A.alloc(16 * 1024 * 2); xTo = A.bf(o_xT, 16384).rearrange("p (k t) -> p k t", k=16)
        o_wc = [A.alloc(16 * 512 * 2) for _ in range(2)]
        Wc = [A.bf(o, 8192).rearrange("p (k n) -> p k n", k=16) for o in o_wc]

        xo_t = xo.rearrange("(n p) f -> n p f", p=128)
        for t in range(NT_OWN):
            xp, xk = stage_piece(xo_t[t], 2048)
            i = t % 2
            norm_mod(xp, xk, bc[0], 'bc0', bc[1], 'bc1', xnb[i], f'xnb{i}')
            transpose16(xnb[i], f'xnb{i}', xTo[:, :, t * 128:(t + 1) * 128], f'xTo{t}', 5, 6)

        for t in range(NT_OWN):
            for kt in range(16):
                S.op('pe', lambda e: e.matmul(bk(7)[:, 16:32], lhsT=xTo[:, kt, t * 128:(t + 1) * 128], rhs=Wg16b[:, kt, :],
                                              start=(kt == 0), stop=(kt == 15)), reads=[f'xTo{t}', 'Wg16'], writes=['B7'])
            S.op('dve', lambda e: e.tensor_tensor(out=gpst[:, t, :], in0=bk(7)[:, 16:32], in1=gbt, op=ALU.add),
                 reads=['B7', 'gb'], writes=[f'gpst{t}'])

        wc_n = [0]

        def load_wchunk(src4):
            i = wc_n[0] % 2
            wc_n[0] += 1
            W = Wc[i]
            for pi in range(4):
                pc, pk = stage_piece(src4[pi].rearrange("p k n -> p (k n)"), 2048)
                eng = ('act', 'dve', 'pool', 'dve')[pi]
                if eng == 'act':
                    S.op('act', lambda e: e.copy(out=W[:, pi * 4:(pi + 1) * 4, :].rearrange("p k n -> p (k n)"), in_=pc), reads=[pk], writes=[f'Wc{i}'])
                else:
                    S.op(eng, lambda e: e.tensor_copy(out=W[:, pi * 4:(pi + 1) * 4, :].rearrange("p k n -> p (k n)"), in_=pc), reads=[pk], writes=[f'Wc{i}'])
            return W, f'Wc{i}'

        for ch in range(6):
            W, wkk = load_wchunk(wtok[ch])
            for t in range(NT_OWN):
                b = t % 2
                for kt in range(16):
                    S.op('pe', lambda e: e.matmul(bk(b)[:, :], lhsT=xTo[:, kt, t * 128:(t + 1) * 128], rhs=W[:, kt, :],
                                                  start=(kt == 0), stop=(kt == 15)), reads=[f'xTo{t}', wkk], writes=[f'B{b}'])
                if ch == 0:
                    S.op('act', lambda e: e.copy(out=qst[:, t, :], in_=bk(b)[:, :]), reads=[f'B{b}'], writes=[f'q{t}'])
                elif ch == 1:
                    S.op('dve', lambda e: e.tensor_copy(out=kst[:, t, :], in_=bk(b)[:, :]), reads=[f'B{b}'], writes=[f'k{t}'])
                elif ch in (2, 3):
                    S.op('act', lambda e: e.copy(out=vst[:, t, (ch - 2) * 512:(ch - 1) * 512], in_=bk(b)[:, :]), reads=[f'B{b}'], writes=[f'v{t}'])
                else:
                    S.op('act', lambda e: e.activation(out=sgo[:, t, (ch - 4) * 512:(ch - 3) * 512], in_=bk(b)[:, :], func=AF.Sigmoid),
                         reads=[f'B{b}'], writes=[f'so{t}'])

        if stage == 'own1a':
            S.barrier()
            out_t = out.rearrange("(n p) f -> n p f", p=128)
            S.dma('d_out0', out_t[0][:, 0:128], gpst.rearrange("p t n -> p (t n)"), writes=['out0'])
            S.finish('sp')
            return nc
        S.barrier()
        A.top = o_wc[0]
        o_cvt = A.alloc(3 * 2048); cvt = [A.f32(o_cvt + i * 2048, 512) for i in range(3)]
        o_wcv = A.alloc(3 * 4096); Wcv = [A.bf(o_wcv + i * 4096, 2048).rearrange("p (k n) -> p k n", k=16) for i in range(3)]
        for ct in range(8):
            for part in range(3):
                pc, pk = stage_piece(wconv[ct, part].rearrange("p k n -> p (k n)"), 2048)
                if part == 0:
                    S.op('act', lambda e: e.copy(out=Wcv[part].rearrange("p k n -> p (k n)"), in_=pc), reads=[pk], writes=[f'Wcv{part}'])
                else:
                    S.op('dve' if part == 1 else 'pool', lambda e: e.tensor_copy(out=Wcv[part].rearrange("p k n -> p (k n)"), in_=pc),
                         reads=[pk], writes=[f'Wcv{part}'])
            for th in range(2):
                for part in range(3):
                    b = 2 + part
                    for kt in range(16):
                        S.op('pe', lambda e: e.matmul(bk(b)[:, :], lhsT=Wcv[part][:, kt, :], rhs=xTo[:, kt, th * 512:(th + 1) * 512],
                                                      start=(kt == 0), stop=(kt == 15)),
                             reads=[f'Wcv{part}'] + [f'xTo{th * 4 + q}' for q in range(4)], writes=[f'B{b}'])
                cs, u, y = cvt
                S.op('act', lambda e: e.copy(out=cs, in_=bk(3)[:, :]), reads=['B3'], writes=['cv_c'])
                S.op('dve', lambda e: e.tensor_tensor(out=u, in0=cs, in1=bk(4)[:, :], op=ALU.mult), reads=['cv_c', 'B4'], writes=['cv_u'])
                u3 = u.rearrange("p (r w) -> p r w", w=64); y3 = y.rearrange("p (r w) -> p r w", w=64)
                S.op('act', lambda e: e.activation(out=y, in_=u, func=AF.Copy, scale=cwt[:, ct, 1:2]), reads=['cv_u', 'cw'], writes=['cv_y'])
                S.op('dve', lambda e: e.scalar_tensor_tensor(out=y3[:, :, 1:64], in0=u3[:, :, 0:63], scalar=cwt[:, ct, 0:1], in1=y3[:, :, 1:64],
                                                             op0=ALU.mult, op1=ALU.add), reads=['cv_u', 'cv_y', 'cw'], writes=['cv_y'])
                S.op('dve', lambda e: e.scalar_tensor_tensor(out=y3[:, :, 0:63], in0=u3[:, :, 1:64], scalar=cwt[:, ct, 2:3], in1=y3[:, :, 0:63],
                                                             op0=ALU.mult, op1=ALU.add), reads=['cv_u', 'cv_y', 'cw'], writes=['cv_y'])
                S.op('dve', lambda e: e.tensor_tensor(out=convT[:, ct, th * 512:(th + 1) * 512], in0=y, in1=bk(2)[:, :], op=ALU.mult),
                     reads=['cv_y', 'B2'], writes=[f'convT{th}'])

        if stage == 'own1b':
            S.barrier()
            out_t = out.rearrange("(n p) f -> n p f", p=128)
            S.dma('d_out0', out_t[0][:, 0:128], gpst.rearrange("p t n -> p (t n)"), writes=['out0'])
            S.finish('sp')
            return nc
        S.barrier()
        A.top = O1
        o_hf = A.alloc(8 * 1024 * 4); hf = A.f32(o_hf, 8192).rearrange("p (t n) -> p t n", t=8)
        o_mx = A.alloc(8 * 1024 * 2); mxT = A.bf(o_mx, 8192).rearrange("p (c t) -> p c t", c=8)
        o_qs = A.alloc(1024); qsb = A.bf(o_qs, 512).rearrange("p (h d) -> p h d", h=4)
        o_ks2 = A.alloc(1024); ks2 = A.bf(o_ks2, 512).rearrange("p (h d) -> p h d", h=4)
        o_qT = A.alloc(1024); qsT = A.bf(o_qT, 512).rearrange("p (h d) -> p h d", h=4)
        o_kT = A.alloc(1024); ksT = A.bf(o_kT, 512).rearrange("p (h d) -> p h d", h=4)
        o_sq = A.alloc(1024); sqk = A.bf(o_sq, 512).rearrange("p (h d) -> p h d", h=4)
        o_CTb = A.alloc(2048); CTb = A.bf(o_CTb, 1024).rearrange("p (h v) -> p h v", h=4)
        o_nb = A.alloc(32); nbb = A.bf(o_nb, 16)
        o_dd = A.alloc(64); ddt = A.f32(o_dd, 16)
        A.top = M + 32768
        o_hs = A.alloc(4096); hs = A.f32(o_hs, 1024)
        o_hq = A.alloc(4096); hq = A.f32(o_hq, 1024)
        o_mb = A.alloc(2048); mxb = A.bf(o_mb, 1024)
        CT = CTo; nst = no_

        class _Stop(Exception):
            pass

        def ckpt(i):
            if stage == f'o2_{i}':
                raise _Stop()

        def own_tile(t, d):
            gk = f'gpst{t}'
            wq, wk_, aa, gsk = gates_dir(gpst[:, t, :], gk, d)
            for h in range(4):
                S.op('dve', lambda e: e.tensor_scalar(out=qsb[:, h, :], in0=qst[:, t, h * 128:(h + 1) * 128], scalar1=wq[:, h:h + 1],
                                                      scalar2=QSCALE, op0=ALU.mult, op1=ALU.mult), reads=[f'q{t}', gsk], writes=['qsb'])
                S.op('pool', lambda e: e.tensor_scalar(out=ks2[:, h, :], in0=kst[:, t, h * 128:(h + 1) * 128], scalar1=wk_[:, h:h + 1],
                                                       scalar2=None, op0=ALU.mult), reads=[f'k{t}', gsk], writes=['ks2'])
            ckpt(1)
            pb = bkbf(5)
            pb6 = bkbf(6)
            for h in range(4):
                S.op('pe', lambda e: e.transpose(out=pb[:, h * 128:(h + 1) * 128], in_=qsb[:, h, :], identity=identb),
                     reads=['qsb', 'identb'], writes=['B5'])
                S.op('pe', lambda e: e.transpose(out=pb6[:, h * 128:(h + 1) * 128], in_=ks2[:, h, :], identity=identb),
                     reads=['ks2', 'identb'], writes=['B6'])
            S.op('act', lambda e: e.copy(out=qsT.rearrange("p h d -> p (h d)"), in_=pb[:, 0:512]), reads=['B5'], writes=['qsT'])
            S.op('dve', lambda e: e.tensor_copy(out=ksT.rearrange("p h d -> p (h d)"), in_=pb6[:, 0:512]), reads=['B6'], writes=['ksT'])
            ckpt(2)
            ck = f'CT{d}'; nk = f'n{d}'
            S.op('act', lambda e: e.copy(out=CTb, in_=CT[:, d]), reads=[ck], writes=['CTb'])
            S.op('dve', lambda e: e.tensor_copy(out=nbb[:, 0:4], in_=nst[:, d, :]), reads=[nk], writes=['nbb'])
            for h in range(4):
                S.op('pe', lambda e: e.matmul(bk(6)[:, h * 128:(h + 1) * 128], lhsT=ksT[:, h, :], rhs=qsT[:, h, :], start=True, stop=True),
                     reads=['ksT', 'qsT'], writes=['B6'])
            ckpt(3)
            msk = Ub if d == 0 else Lb
            for h in range(4):
                S.op('dve', lambda e: e.tensor_tensor(out=sqk[:, h, :], in0=bk(6)[:, h * 128:(h + 1) * 128], in1=msk, op=ALU.mult),
                     reads=['B6', 'Ub', 'Lb'], writes=['sqk'])
            ckpt(4)
            vv = vst[:, t, :].rearrange("p (h v) -> p h v", h=4)
            for h in range(4):
                b = h // 2
                osl = bk(b)[:, (h % 2) * 256:(h % 2) * 256 + 256]
                S.op('pe', lambda e: e.matmul(osl, lhsT=sqk[:, h, :], rhs=vv[:, h, :], start=True, stop=False),
                     reads=['sqk', f'v{t}'], writes=[f'B{b}'])
                S.op('pe', lambda e: e.matmul(osl, lhsT=qsT[:, h, :], rhs=CTb[:, h, :], start=False, stop=True),
                     reads=['qsT', 'CTb'], writes=[f'B{b}'])
                S.op('pe', lambda e: e.matmul(bk(7)[:, 32 + h:33 + h], lhsT=sqk[:, h, :], rhs=onesb[:, 0:1], start=True, stop=False),
                     reads=['sqk', 'onesb'], writes=['B7'])
                S.op('pe', lambda e: e.matmul(bk(7)[:, 32 + h:33 + h], lhsT=qsT[:, h, :], rhs=nbb[:, h:h + 1], start=False, stop=True),
                     reads=['qsT', 'nbb'], writes=['B7'])
            ckpt(5)
            dd = ddt[:, 0:4]
            S.op('act', lambda e: e.activation(out=dd, in_=bk(7)[:, 32:36], func=AF.Abs), reads=['B7'], writes=['dd'])
            S.op('dve', lambda e: e.tensor_scalar(out=dd, in0=dd, scalar1=1.0, scalar2=None, op0=ALU.max), reads=['dd'], writes=['dd'])
            S.op('dve', lambda e: e.reciprocal(out=dd, in_=dd), reads=['dd'], writes=['dd'])
            for h in range(4):
                b = h // 2
                osl = bk(b)[:, (h % 2) * 256:(h % 2) * 256 + 256]
                if d == 0:
                    S.op('act', lambda e: e.activation(out=hf[:, t, h * 256:(h + 1) * 256], in_=osl, func=AF.Copy, scale=dd[:, h:h + 1]),
                         reads=[f'B{b}', 'dd'], writes=[f'hf{t}'])
                else:
                    S.op('dve', lambda e: e.scalar_tensor_tensor(out=hs[:, h * 256:(h + 1) * 256], in0=osl, scalar=dd[:, h:h + 1],
                                                                 in1=hf[:, t, h * 256:(h + 1) * 256], op0=ALU.mult, op1=ALU.add),
                         reads=[f'B{b}', 'dd', f'hf{t}'], writes=['hs'])
            ckpt(6)
            state_update(d, ks2, 'ks2', vv, f'v{t}', aa, gsk, None)
            ckpt(7)
            if d == 1:
                ssq = ddt[:, 4:8]
                S.op('pool', lambda e: e.tensor_tensor(out=hq, in0=hs, in1=hs, op=ALU.mult), reads=['hs'], writes=['hq'])
                S.op('dve', lambda e: e.tensor_reduce(out=ssq, in_=hq.rearrange("p (h v) -> p h v", h=4), axis=AX.X, op=ALU.add),
                     reads=['hq'], writes=['ssq'])
                S.op('dve', lambda e: e.tensor_scalar(out=ssq, in0=ssq, scalar1=1.0 / 256, scalar2=EPS, op0=ALU.mult, op1=ALU.add),
                     reads=['ssq'], writes=['ssq'])
                S.op('act', lambda e: e.activation(out=ssq, in_=ssq, func=AF.Ln), reads=['ssq'], writes=['ssq'])
                S.op('act', lambda e: e.activation(out=ssq, in_=ssq, func=AF.Exp, scale=-0.5), reads=['ssq'], writes=['ssq'])
                for h in range(4):
                    S.op('dve', lambda e: e.scalar_tensor_tensor(out=hq[:, h * 256:(h + 1) * 256], in0=hs[:, h * 256:(h + 1) * 256],
                                                                  scalar=ssq[:, h:h + 1], in1=hgt[:, h * 256:(h + 1) * 256],
                                                                  op0=ALU.mult, op1=ALU.mult), reads=['hs', 'ssq', 'hg'], writes=['hq'])
                S.op('dve', lambda e: e.tensor_tensor(out=mxb, in0=hq, in1=sgo[:, t, :], op=ALU.mult), reads=['hq', f'so{t}'], writes=['mxb'])
                pb2 = bkbf(5)
                for c8 in range(8):
                    S.op('pe', lambda e: e.transpose(out=pb2[:, c8 * 128:(c8 + 1) * 128], in_=mxb[:, c8 * 128:(c8 + 1) * 128], identity=identb),
                         reads=['mxb', 'identb'], writes=['B5'])
                S.op('act', lambda e: e.copy(out=mxT[:, :, t * 128:(t + 1) * 128], in_=pb2.rearrange("p (c t) -> p c t", t=128)),
                     reads=['B5'], writes=[f'mxT{t}'])

        try:
            for t in range(NT_OWN):
                own_tile(t, 0)
            ckpt(8)
            for t in reversed(range(NT_OWN)):
                own_tile(t, 1)
                ckpt(9)
        except _Stop:
            S.barrier()
            out_t = out.rearrange("(n p) f -> n p f", p=128)
            S.dma('d_out0', out_t[0][:, 0:1024], hf[:, 0, :], writes=['out0'])
            S.finish('sp')
            return nc

        if stage == 'own2':
            S.barrier()
            out_t = out.rearrange("(n p) f -> n p f", p=128)
            S.dma('d_out0', out_t[0][:, 0:1024], hf[:, 0, :], writes=['out0'])
            S.finish('sp')
            return nc
        S.barrier()
        A.top = O1
        o_bcx = [A.alloc(8192) for _ in range(3)]
        bcx = [A.f32(o, 2048) for o in o_bcx]
        o_gt2 = A.alloc(8192); gt2bc = A.f32(o_gt2, 2048)
        assert A.top <= o_mx
        A.top = o_mx + 16384
        o_tmp = A.alloc(2048); tmpc = A.f32(o_tmp, 512)
        ada_temps(OWN, g2_2)
        rows = tmpl['rows']; g12 = tmpl['g12']
        assert A.top <= o_cv
        mod_row(2, rows[0], 'row0')
        bcast(bcx[0], 'bcx0', rows[0], 'row0', sel0, 'sel0')
        mod_row(3, rows[0], 'row0')
        mod_row(4, rows[1], 'row1')
        S.op('dve', lambda e: e.scalar_tensor_tensor(out=rows[1][0:2, :], in0=rows[1][0:2, :], scalar=1.0, in1=g12[0:2, :],
                                                     op0=ALU.add, op1=ALU.mult), reads=['row1', 'g12'], writes=['row1'])
        bcast(bcx[1], 'bcx1', rows[1], 'row1', sel0, 'sel0')
        bcast(bcx[2], 'bcx2', rows[0], 'row0', sel0, 'sel0')
        mod_row(5, rows[0], 'row0')
        bcast(gt2bc, 'gt2bc', rows[0], 'row0', sel0, 'sel0')

        if stage == 'ada2':
            S.barrier()
            out_t = out.rearrange("(n p) f -> n p f", p=128)
            S.dma('d_out0', out_t[0], gt2bc, writes=['out0'])
            S.finish('sp')
            return nc
        S.barrier()
        o_x1 = M
        x1 = A.f32(o_x1, 8 * 2048).rearrange("p (t n) -> p t n", t=8)
        A.top = M + 65536
        o_wc2 = [A.alloc(16 * 512 * 2) for _ in range(2)]
        assert A.top <= o_cv, (A.top, o_cv)
        Wc[0] = A.bf(o_wc2[0], 8192).rearrange("p (k n) -> p k n", k=16)
        Wc[1] = A.bf(o_wc2[1], 8192).rearrange("p (k n) -> p k n", k=16)
        for t in range(NT_OWN):
            S.dma(f'd_x1_{t}', x1[:, t, :], xo_t[t], writes=[f'x1_{t}'])
        for ch in range(4):
            W, wkk = load_wchunk(wout[ch])
            for t in range(NT_OWN):
                b = t % 2
                for ft in range(16):
                    lh = convT[:, ft, t * 128:(t + 1) * 128] if ft < 8 else mxT[:, ft - 8, t * 128:(t + 1) * 128]
                    S.op('pe', lambda e: e.matmul(bk(b)[:, :], lhsT=lh, rhs=W[:, ft, :], start=(ft == 0), stop=(ft == 15)),
                         reads=[wkk, f'mxT{t}', 'convT0', 'convT1'], writes=[f'B{b}'])
                S.op('dve', lambda e: e.tensor_tensor(out=tmpc, in0=bk(b)[:, :], in1=bcx[0][:, ch * 512:(ch + 1) * 512], op=ALU.mult),
                     reads=[f'B{b}', 'bcx0'], writes=['tmpc'])
                S.op('pool', lambda e: e.tensor_tensor(out=x1[:, t, ch * 512:(ch + 1) * 512], in0=x1[:, t, ch * 512:(ch + 1) * 512], in1=tmpc,
                                                       op=ALU.add), reads=['tmpc', f'x1_{t}'], writes=[f'x1_{t}'])

        if stage == 'mixer':
            out_t = out.rearrange("(n p) f -> n p f", p=128)
            for t in range(NT_OWN):
                S.dma(f'd_out{t}', out_t[t], x1[:, t, :], reads=[f'x1_{t}'], writes=[f'out{t}'])
            S.finish('sp')
            return nc

        S.barrier()
        o_h2 = M + 65536
        h2tok = A.bf(o_h2, 8 * 2048).rearrange("p (t n) -> p t n", t=8)
        A.top = o_h2 + 32768
        o_t1 = A.alloc(8192); t1 = A.f32(o_t1, 2048)
        o_h2f = A.alloc(8192); h2f = A.f32(o_h2f, 2048)
        assert A.top <= O1 + 8192, (A.top, O1)
        A.top = o_mx
        o_ss = A.alloc(64); ssb = A.f32(o_ss, 16)
        o_h2T = A.alloc(8192); h2T = A.f32(o_h2T, 2048).rearrange("p (k t) -> p k t", t=128)
        o_wr = A.alloc(4096); wrt = A.f32(o_wr, 1024).rearrange("p (k n) -> p k n", n=64)
        o_br = A.alloc(256); brt = A.f32(o_br, 64)
        NV = NE + 2
        o_mask = A.alloc(8 * NV * 4); maskt = A.f32(o_mask, 8 * NV).rearrange("p (t n) -> p t n", t=8)
        o_G = A.alloc(8 * NV * 4); Gt = A.f32(o_G, 8 * NV).rearrange("p (t n) -> p t n", t=8)
        o_pos = A.alloc(8 * NV * 4); posm = A.f32(o_pos, 8 * NV).rearrange("p (t n) -> p t n", t=8)
        o_sc = A.alloc(1024); sct = A.f32(o_sc, 256)
        S.dma('d_wr', wrt, wr[:, :, :], writes=['wr'])
        S.dma('d_br', brt, br[:, :], writes=['br'])
        for t in range(NT_OWN):
            S.dma(f'd_shp{t}', posm[:, t, NE:NE + 2], shpos[:, t, :], writes=[f'posm{t}'])
        for t in range(NT_OWN):
            norm_mod(x1[:, t, :], f'x1_{t}', bcx[1], 'bcx1', bcx[2], 'bcx2', h2f, 'h2f')
            S.op('act', lambda e: e.copy(out=h2tok[:, t, :], in_=h2f), reads=['h2f'], writes=[f'h2tok{t}'])
            for g4 in range(4):
                for k4 in range(4):
                    kt = g4 * 4 + k4
                    S.op('pe', lambda e: e.transpose(out=bk(g4)[:, k4 * 128:(k4 + 1) * 128], in_=h2f[:, kt * 128:(kt + 1) * 128], identity=identf),
                         reads=['h2f', 'identf'], writes=[f'B{g4}'])
                S.op('act' if g4 % 2 == 0 else 'dve',
                     (lambda e: e.copy(out=h2T[:, g4 * 4:(g4 + 1) * 4, :], in_=bk(g4)[:, :].rearrange("p (k t) -> p k t", t=128))) if g4 % 2 == 0 else
                     (lambda e: e.tensor_copy(out=h2T[:, g4 * 4:(g4 + 1) * 4, :], in_=bk(g4)[:, :].rearrange("p (k t) -> p k t", t=128))),
                     reads=[f'B{g4}'], writes=['h2T'])
            for kt in range(16):
                S.op('pe', lambda e: e.matmul(bk(7)[:, 64:128], lhsT=h2T[:, kt, :], rhs=wrt[:, kt, :], start=(kt == 0), stop=(kt == 15)),
                     reads=['h2T', 'wr'], writes=['B7'])
            scr = sct[:, 0:64]; bia = sct[:, 64:128]; top8 = sct[:, 128:136]; den = sct[:, 136:137]
            S.op('act', lambda e: e.activation(out=scr, in_=bk(7)[:, 64:128], func=AF.Sigmoid), reads=['B7'], writes=['scr'])
            S.op('dve', lambda e: e.tensor_tensor(out=bia, in0=scr, in1=brt, op=ALU.add), reads=['scr', 'br'], writes=['bia'])
            S.op('dve', lambda e: e.max(out=top8, in_=bia), reads=['bia'], writes=['top8'])
            S.op('dve', lambda e: e.tensor_scalar(out=maskt[:, t, 0:NE], in0=bia, scalar1=top8[:, 5:6], scalar2=None, op0=ALU.is_ge),
                 reads=['bia', 'top8'], writes=[f'mask{t}'])
            S.op('dve', lambda e: e.tensor_tensor(out=scr, in0=scr, in1=maskt[:, t, 0:NE], op=ALU.mult), reads=['scr', f'mask{t}'], writes=['scr'])
            S.op('dve', lambda e: e.tensor_reduce(out=den, in_=scr, axis=AX.X, op=ALU.add), reads=['scr'], writes=['den'])
            S.op('dve', lambda e: e.reciprocal(out=den, in_=den), reads=['den'], writes=['den'])
            S.op('dve', lambda e: e.tensor_scalar(out=Gt[:, t, 0:NE], in0=scr, scalar1=den, scalar2=RSCALE, op0=ALU.mult, op1=ALU.mult),
                 reads=['scr', 'den'], writes=[f'G{t}'])
            S.op('pool', lambda e: e.memset(Gt[:, t, NE:NE + 2], 1.0), writes=[f'G{t}'])
        for t in range(NT_OWN):
            S.op('pe', lambda e: e.matmul(bk(6)[:, 0:NE], lhsT=Uf, rhs=maskt[:, t, 0:NE], start=True, stop=(t == 0)),
                 reads=['Uf', f'mask{t}'], writes=['B6'])
            for t2 in range(t):
                S.op('pe', lambda e: e.matmul(bk(6)[:, 0:NE], lhsT=onesf, rhs=maskt[:, t2, 0:NE], start=False, stop=(t2 == t - 1)),
                     reads=['onesf', f'mask{t2}'], writes=['B6'])
            S.op('dve', lambda e: e.tensor_tensor(out=posm[:, t, 0:NE], in0=bk(6)[:, 0:NE], in1=maskt[:, t, 0:NE], op=ALU.mult),
                 reads=['B6', f'mask{t}'], writes=[f'posm{t}'])
            S.op('dve', lambda e: e.tensor_scalar(out=posm[:, t, 0:NE], in0=posm[:, t, 0:NE], scalar1=-1.0, scalar2=None, op0=ALU.add),
                 reads=[f'posm{t}'], writes=[f'posm{t}'])

        S.barrier()
        MO = o_h2 + 32768
        A.top = MO
        o_gt2n = A.alloc(8192)
        o_Gn = A.alloc(8 * NV * 4); o_posn = A.alloc(8 * NV * 4)
        stg_f = A.f32(o_stage, 4096)
        S.op('dve', lambda e: e.tensor_copy(out=stg_f[:, 0:2048], in_=gt2bc), writes=['mv'])
        S.op('dve', lambda e: e.tensor_copy(out=stg_f[:, 2048:2048 + 8 * NV], in_=Gt.rearrange("p t n -> p (t n)")), writes=['mv'])
        S.op('dve', lambda e: e.tensor_copy(out=stg_f[:, 2048 + 8 * NV:2048 + 16 * NV], in_=posm.rearrange("p t n -> p (t n)")), writes=['mv'])
        S.barrier()
        gt2bc = A.f32(o_gt2n, 2048)
        Gt = A.f32(o_Gn, 8 * NV).rearrange("p (t n) -> p t n", t=8)
        posm = A.f32(o_posn, 8 * NV).rearrange("p (t n) -> p t n", t=8)
        S.op('dve', lambda e: e.tensor_copy(out=gt2bc, in_=stg_f[:, 0:2048]), writes=['gt2bc'])
        S.op('dve', lambda e: e.tensor_copy(out=Gt.rearrange("p t n -> p (t n)"), in_=stg_f[:, 2048:2048 + 8 * NV]), writes=['G'])
        S.op('dve', lambda e: e.tensor_copy(out=posm.rearrange("p t n -> p (t n)"), in_=stg_f[:, 2048 + 8 * NV:2048 + 16 * NV]), writes=['posm'])
        S.barrier()
        o_P = A.alloc(8 * CAP * 2); Pm = A.bf(o_P, 8 * CAP).rearrange("p (t c) -> p t c", t=8)
        o_R = A.alloc(16 * CAP * 2); xgT = A.bf(o_R, 16 * CAP).rearrange("p (k c) -> p k c", k=16)
        PT = A.bf(o_R, NST * 1024).rearrange("p (s t) -> p s t", s=NST)
        o_hb = A.alloc(NHT * CAP * 2); hbT = A.bf(o_hb, NHT * CAP).rearrange("p (h c) -> p h c", h=NHT)
        o_sg = A.alloc(CAP * 2); sgt = A.bf(o_sg, CAP)
        o_yb = [A.alloc(NST * 256 * 2) for _ in range(2)]
        ybb = [A.bf(o, NST * 256).rearrange("p (s n) -> p s n", s=NST) for o in o_yb]
        o_wgu = [A.alloc(4096) for _ in range(4)]
        Wgu = [A.bf(o, 2048).rearrange("p (k n) -> p k n", k=16) for o in o_wgu]
        o_wd = [A.alloc(NHT * 256 * 2) for _ in range(2)]
        Wdb = [A.bf(o, NHT * 256).rearrange("p (h n) -> p h n", h=NHT) for o in o_wd]
        stage_n[0] = 0

        def piece4(src, nelem):
            return stage_piece(src, nelem, nslots=4, slot_elems=1024)

        wgu_n = [0]; wd_n = [0]; yb_n = [0]
        cast_rr = [0]

        def cast(out_ap, in_ap, rk, wk):
            i = cast_rr[0] % 3
            cast_rr[0] += 1
            if i == 0:
                S.op('act', lambda e: e.copy(out=out_ap, in_=in_ap), reads=[rk], writes=[wk])
            elif i == 1:
                S.op('pool', lambda e: e.tensor_copy(out=out_ap, in_=in_ap), reads=[rk], writes=[wk])
            else:
                S.op('dve', lambda e: e.tensor_copy(out=out_ap, in_=in_ap), reads=[rk], writes=[wk])

        order = [NE, NE + 1] + list(range(NE))
        for e_ in order:
            if e_ < NE:
                sg_, su_, sd_ = weg[e_], weu[e_], wed[e_]
            else:
                sg_, su_, sd_ = wsg, wsu, wsd
            for tt in range(8):
                S.op('dve' if tt % 2 == 0 else 'pool',
                     lambda e: e.tensor_scalar(out=Pm[:, tt, :], in0=iota[:, 0:CAP], scalar1=posm[:, tt, e_:e_ + 1], scalar2=None, op0=ALU.is_equal),
                     reads=['iota', 'posm'], writes=[f'P{tt}'])
            for kt in range(16):
                b = kt % 2
                for tt in range(8):
                    S.op('pe', lambda e: e.matmul(bk(b)[:, 0:CAP], lhsT=h2tok[:, tt, kt * 128:(kt + 1) * 128], rhs=Pm[:, tt, :],
                                                  start=(tt == 0), stop=(tt == 7)), reads=[f'h2tok{tt}', f'P{tt}'], writes=[f'B{b}'])
                if kt % 2 == 0:
                    S.op('act', lambda e: e.copy(out=xgT[:, kt, :], in_=bk(b)[:, 0:CAP]), reads=[f'B{b}'], writes=[f'R{kt}'])
                else:
                    S.op('dve', lambda e: e.tensor_copy(out=xgT[:, kt, :], in_=bk(b)[:, 0:CAP]), reads=[f'B{b}'], writes=[f'R{kt}'])
            for ht in range(NHT):
                ws = []
                for which, src in ((0, sg_), (1, su_)):
                    i = wgu_n[0] % 4
                    wgu_n[0] += 1
                    Wt = Wgu[i]
                    flat = src[ht].rearrange("p k n -> p (k n)")
                    for hf_ in range(2):
                        pc, pk = piece4(flat[:, hf_ * 1024:(hf_ + 1) * 1024], 1024)
                        cast(Wt[:, hf_ * 8:(hf_ + 1) * 8, :].rearrange("p k n -> p (k n)"), pc, pk, f'Wgu{i}')
                    ws.append((Wt, f'Wgu{i}'))
                par = ht % 2
                bg, bu = 2 + 2 * par, 3 + 2 * par
                for (Wt, wkk), b in zip(ws, (bg, bu)):
                    for kt in range(16):
                        S.op('pe', lambda e: e.matmul(bk(b)[:, 0:CAP], lhsT=Wt[:, kt, :], rhs=xgT[:, kt, :], start=(kt == 0), stop=(kt == 15)),
                             reads=[wkk, f'R{kt}'], writes=[f'B{b}'])
                S.op('act', lambda e: e.activation(out=sgt, in_=bk(bg)[:, 0:CAP], func=AF.Silu), reads=[f'B{bg}'], writes=['sgt'])
                S.op('dve', lambda e: e.tensor_tensor(out=hbT[:, ht, :], in0=sgt, in1=bk(bu)[:, 0:CAP], op=ALU.mult),
                     reads=['sgt', f'B{bu}'], writes=[f'hb{ht}'])
            for stt in range(NST):
                pb = bkbf(1)
                for tt in range(8):
                    S.op('pe', lambda e: e.transpose(out=pb[:, tt * 128:(tt + 1) * 128], in_=Pm[:, tt, stt * 128:(stt + 1) * 128], identity=identb),
                         reads=[f'P{tt}', 'identb'], writes=['B1'])
                S.op('act', lambda e: e.copy(out=PT[:, stt, :], in_=pb), reads=['B1'], writes=[f'R{2 * stt}', f'R{2 * stt + 1}'])
            for c8 in range(8):
                i = wd_n[0] % 2
                wd_n[0] += 1
                Wd = Wdb[i]
                for (h0, h1) in ((0, 4), (4, 8), (8, 11)):
                    nh = h1 - h0
                    pc, pk = piece4(sd_[c8][:, h0:h1, :].rearrange("p h n -> p (h n)"), nh * 256)
                    for hh in range(nh):
                        eng = ('pool', 'dve')[(hh + h0) % 2]
                        S.op(eng, lambda e: e.tensor_tensor(out=Wd[:, h0 + hh, :], in0=pc[:, hh * 256:(hh + 1) * 256],
                                                            in1=gt2bc[:, c8 * 256:(c8 + 1) * 256], op=ALU.mult),
                             reads=[pk, 'gt2bc'], writes=[f'Wd{i}'])
                j = yb_n[0] % 2
                yb_n[0] += 1
                yb = ybb[j]
                for stt in range(NST):
                    db = 6 if stt % 2 == 0 else 2
                    osl = bk(db)[:, 0:256]
                    for ht in range(NHT):
                        S.op('pe', lambda e: e.matmul(osl, lhsT=hbT[:, ht, stt * 128:(stt + 1) * 128], rhs=Wd[:, ht, :],
                                                      start=(ht == 0), stop=(ht == NHT - 1)), reads=[f'hb{ht}', f'Wd{i}'], writes=[f'B{db}'])
                    S.op('act', lambda e: e.copy(out=yb[:, stt, :], in_=osl), reads=[f'B{db}'], writes=[f'yb{j}'])
                for tt in range(8):
                    cb = 7 if tt % 2 == 0 else 3
                    osl = bk(cb)[:, 0:256]
                    for stt in range(NST):
                        S.op('pe', lambda e: e.matmul(osl, lhsT=PT[:, stt, tt * 128:(tt + 1) * 128], rhs=yb[:, stt, :],
                                                      start=(stt == 0), stop=(stt == NST - 1)),
                             reads=[f'R{2 * stt}', f'R{2 * stt + 1}', f'yb{j}'], writes=[f'B{cb}'])
                    S.op('dve' if tt % 2 == 0 else 'pool' if False else 'dve',
                         lambda e: e.scalar_tensor_tensor(out=x1[:, tt, c8 * 256:(c8 + 1) * 256], in0=osl, scalar=Gt[:, tt, e_:e_ + 1],
                                                          in1=x1[:, tt, c8 * 256:(c8 + 1) * 256], op0=ALU.mult, op1=ALU.add),
                         reads=[f'B{cb}', 'G', f'x1_{tt}'], writes=[f'x1_{tt}'])

        S.barrier()
        A.top = MO
        o_fg = A.alloc(8192); fgt = A.f32(o_fg, 2048)
        o_z = A.alloc(8192); zt = A.f32(o_z, 2048)
        o_ot = [A.alloc(8192) for _ in range(2)]
        ot = [A.f32(o, 2048) for o in o_ot]
        o_ss2 = A.alloc(64); ss2 = A.f32(o_ss2, 16)
        S.dma('d_fg', fgt, fg[:, :], writes=['fg'])
        out_t = out.rearrange("(n p) f -> n p f", p=128)
        for t in range(NT_OWN):
            ss = ss2[:, t:t + 1]
            S.op('act', lambda e: e.activation(out=zt, in_=x1[:, t, :], func=AF.Square, accum_out=ss), reads=[f'x1_{t}'], writes=['zt', f'fss{t}'])
            S.op('dve', lambda e: e.tensor_scalar(out=ss, in0=ss, scalar1=1.0 / D, scalar2=EPS, op0=ALU.mult, op1=ALU.add), reads=[f'fss{t}'], writes=[f'fss{t}'])
            S.op('act', lambda e: e.activation(out=ss, in_=ss, func=AF.Ln), reads=[f'fss{t}'], writes=[f'fss{t}'])
            S.op('act', lambda e: e.activation(out=ss, in_=ss, func=AF.Exp, scale=-0.5), reads=[f'fss{t}'], writes=[f'fss{t}'])
            S.op('dve', lambda e: e.scalar_tensor_tensor(out=ot[t % 2], in0=x1[:, t, :], scalar=ss, in1=fgt, op0=ALU.mult, op1=ALU.mult),
                 reads=[f'x1_{t}', f'fss{t}', 'fg'], writes=[f'ot{t % 2}'])
            S.dma(f'd_out{t % 2}', out_t[t], ot[t % 2], reads=[f'ot{t % 2}'], writes=[f'out{t}'])
        S.finish('sp')
    return nc


def _layouts(inp, ne=NE):
    f = np.float32
    c = lambda a: np.ascontiguousarray(a, dtype=f)
    w_in = inp['w_in'][0]
    sh = {}
    sh['wada'] = c(inp['w_ada'][0].reshape(16, 128, 6, D).transpose(2, 0, 1, 3))
    sh['bada2'] = c(np.stack([inp['b_ada'][0], inp['b_ada'][0]]))
    sh['g1_2'] = c(np.stack([inp['norm1_g'][0]] * 2)); sh['g2_2'] = c(np.stack([inp['norm2_g'][0]] * 2))
    kvg = np.concatenate([w_in[:, 3584:4096], w_in[:, 4096:5120], w_in[:, 6144:6160]], axis=1)
    sh['wkvg'] = c(kvg.reshape(16, 128, 1552))
    chunks = [w_in[:, 3072:3584], w_in[:, 3584:4096], w_in[:, 4096:4608], w_in[:, 4608:5120], w_in[:, 5120:5632], w_in[:, 5632:6144]]
    sh['wtok'] = c(np.stack([ch.reshape(4, 4, 128, 512).transpose(0, 2, 1, 3) for ch in chunks]))
    sh['wg16'] = c(w_in[:, 6144:6160].reshape(16, 128, 16).transpose(1, 0, 2))
    wc = np.stack([np.stack([w_in[:, part * 1024 + ct * 128: part * 1024 + (ct + 1) * 128].reshape(16, 128, 128).transpose(1, 0, 2)
                             for part in range(3)]) for ct in range(8)])
    sh['wconv'] = c(wc)
    sh['cw'] = c(inp['conv_w'][0].reshape(3, 8, 128).transpose(2, 1, 0))
    sh['gb'] = c(np.broadcast_to(inp['gate_b'][0].reshape(1, 16), (128, 16)))
    sh['hg'] = c(np.broadcast_to(inp['head_g'][0].reshape(1, 1024), (128, 1024)))
    wo = inp['w_out'][0]
    sh['wout'] = c(np.stack([wo[:, ch * 512:(ch + 1) * 512].reshape(4, 4, 128, 512).transpose(0, 2, 1, 3) for ch in range(4)]))
    sh['wr'] = c(inp['w_router'][0].reshape(16, 128, 64).transpose(1, 0, 2))
    sh['br'] = c(np.broadcast_to(inp['b_router'][0].reshape(1, 64), (128, 64)))
    sh['weg'] = c(inp['we_gate'][0][:ne].reshape(ne, 16, 128, NHT, 128).transpose(0, 3, 2, 1, 4))
    sh['weu'] = c(inp['we_up'][0][:ne].reshape(ne, 16, 128, NHT, 128).transpose(0, 3, 2, 1, 4))
    sh['wed'] = c(inp['we_down'][0][:ne].reshape(ne, NHT, 128, 8, 256).transpose(0, 3, 2, 1, 4))
    sh['wsg'] = c(inp['ws_gate'][0].reshape(16, 128, NHT, 128).transpose(2, 1, 0, 3))
    sh['wsu'] = c(inp['ws_up'][0].reshape(16, 128, NHT, 128).transpose(2, 1, 0, 3))
    sh['wsd'] = c(inp['ws_down'][0].reshape(NHT, 128, 8, 256).transpose(2, 1, 0, 3))
    sh['fg'] = c(np.broadcast_to(inp['final_g'].reshape(1, D), (128, D)))
    sp = np.full((128, 8, 2), -1.0, f)
    for t in range(8):
        sp[:, t, 0 if t < 4 else 1] = (t % 4) * 128 + np.arange(128)
    sh['shpos'] = sp
    per = []
    for core in range(8):
        b, seg = core // 4, core % 4
        m = dict(sh)
        m['xb'] = c(inp['x'][b]); m['xo'] = c(inp['x'][b, seg * 1024:(seg + 1) * 1024]); m['cx'] = c(inp['ctx'][b])
        m['cT'] = c(np.stack([inp['c'][b].reshape(16, 128).T, inp['c_ctx'].reshape(16, 128).T], axis=-1))
        fl = np.zeros((128, 68), f)
        fl[:, seg * 8] = 1.0
        fl[:, 33 + (24 - seg * 8)] = 1.0
        m['flags'] = fl
        per.append(m)
    return per


_NC_CACHE = {}


def kernel(**inputs):
    inp = {k: np.asarray(v) for k, v in inputs.items()}
    if 'full' not in _NC_CACHE:
        _NC_CACHE['full'] = build_nc('full')
    nc = _NC_CACHE['full']
    in_maps = _layouts(inp)
    res = run_bass_kernel_spmd(nc, in_maps, core_ids=list(range(8)))
    outs = [np.asarray(r["out"], dtype=np.float32) for r in res.results]
    full = np.zeros((2, 4096, D), np.float32)
    for core in range(8):
        b, seg = core // 4, core % 4
        full[b, seg * 1024:(seg + 1) * 1024] = outs[core]
    return full
```

```python
import numpy as np
from contextlib import ExitStack
import concourse.bass as bass
import concourse.mybir as mybir
from concourse.bass_utils import run_bass_kernel_spmd

F32 = mybir.dt.float32
BF16 = mybir.dt.bfloat16
I32 = mybir.dt.int32
AF = mybir.ActivationFunctionType
ALU = mybir.AluOpType
AX = mybir.AxisListType

D = 2048
NT_OWN = 8
NT_B = 32
NE = 64
CAP = 512
NST = CAP // 128
EPS = 1e-6
QSCALE = 128 ** -0.5
RSCALE = 2.446
DH = 1408
NHT = 11
NWD = 1
ARENA_BYTES = 210432


class Sched:
    def __init__(self, nc, stack):
        self.nc = nc
        self.stack = stack
        self.e = {'pe': nc.tensor, 'act': nc.scalar, 'dve': nc.vector, 'pool': nc.gpsimd, 'sp': nc.sync}
        self.sem = {k: stack.enter_context(nc.semaphore(f"s_{k}")) for k in self.e}
        self.cnt = {k: 0 for k in self.e}
        self.waited = {}
        self.st = {}
        self.dsems = {}
        self.dsem_by_name = {}
        self.epoch = 1

    def _state(self, k):
        s = self.st.get(k)
        if s is None:
            s = self.st[k] = {'w': None, 'r': []}
        return s

    def _deps(self, reads, writes):
        deps = []
        for k in reads:
            s = self._state(k)
            if s['w'] is not None:
                deps.append(s['w'])
        for k in writes:
            s = self._state(k)
            if s['w'] is not None:
                deps.append(s['w'])
            deps.extend(s['r'])
        return deps

    def _emit_waits(self, eng, deps, is_dma):
        need = {}
        for t in deps:
            kind, who, val = t
            if kind == 'e' and who == eng and not is_dma and eng == 'pe':
                continue
            key = (kind, who if kind == 'e' else id(who))
            if key not in need or need[key][1] < val:
                need[key] = (who, val, kind)
        for key, (who, val, kind) in need.items():
            wk = (eng, key)
            if self.waited.get(wk, 0) >= val:
                continue
            self.waited[wk] = val
            sem = self.sem[who] if kind == 'e' else who
            self.e[eng].wait_ge(sem, val)

    def _update(self, tok, reads, writes):
        for k in reads:
            s = self._state(k)
            r = s['r']
            r[:] = [x for x in r if not (x[0] == tok[0] and (x[1] is tok[1] or x[1] == tok[1]))]
            r.append(tok)
        for k in writes:
            s = self._state(k)
            s['w'] = tok
            s['r'] = []

    def op(self, eng, fn, reads=(), writes=()):
        deps = self._deps(reads, writes)
        self._emit_waits(eng, deps, False)
        inst = fn(self.e[eng])
        inst.then_inc(self.sem[eng], 1)
        self.cnt[eng] += 1
        self._update(('e', eng, self.cnt[eng]), reads, writes)

    def dsem(self, name):
        s = self.dsem_by_name.get(name)
        if s is None:
            s = self.stack.enter_context(self.nc.semaphore(name))
            self.dsem_by_name[name] = s
            self.dsems[id(s)] = [s, 0]
        return s

    def dma(self, semname, out, in_, reads=(), writes=(), eng='sp'):
        dsem = self.dsem(semname)
        deps = self._deps(reads, writes)
        self._emit_waits(eng, deps, True)
        self.e[eng].dma_start(out=out, in_=in_).then_inc(dsem, 16)
        rec = self.dsems[id(dsem)]
        rec[1] += 16
        self._update(('d', dsem, rec[1]), reads, writes)

    def barrier(self, new_epoch=False):
        for eng in self.e:
            for other in self.e:
                if (other != eng or eng in ('act', 'dve', 'pool')) and self.cnt[other] > 0:
                    wk = (eng, ('e', other))
                    if self.waited.get(wk, 0) < self.cnt[other]:
                        self.waited[wk] = self.cnt[other]
                        self.e[eng].wait_ge(self.sem[other], self.cnt[other])
            for sid, (s, v) in self.dsems.items():
                if v > 0:
                    wk = (eng, ('d', sid))
                    if self.waited.get(wk, 0) < v:
                        self.waited[wk] = v
                        self.e[eng].wait_ge(s, v)
        self.st = {}
        if new_epoch:
            for k in self.e:
                self.sem[k] = self.stack.enter_context(self.nc.semaphore(f"s_{k}_{self.epoch}"))
                self.cnt[k] = 0
            self.epoch += 1
            self.waited = {wk: v for wk, v in self.waited.items() if wk[1][0] != 'e'}

    def finish(self, eng='sp'):
        deps = []
        for k, s in self.st.items():
            if s['w'] is not None:
                deps.append(s['w'])
            deps.extend(s['r'])
        self._emit_waits(eng, deps, True)


class Arena:
    def __init__(self, ap):
        self.ap = ap
        self.top = 0

    def alloc(self, nbytes):
        off = (self.top + 31) // 32 * 32
        self.top = off + nbytes
        assert self.top <= ARENA_BYTES, f"arena overflow {self.top}"
        return off

    def bf(self, off, n, parts=128):
        return self.ap[0:parts, off // 2: off // 2 + n]

    def f32(self, off, n, parts=128):
        return self.ap[0:parts, off // 2: off // 2 + 2 * n].bitcast(F32)

    def i32(self, off, n, parts=128):
        return self.ap[0:parts, off // 2: off // 2 + 2 * n].bitcast(I32)


def build_nc(stage='full'):
    nc = bass.Bass("TRN2", target_bir_lowering=False)

    def din(name, shape):
        return nc.dram_tensor(name, list(shape), F32, kind="ExternalInput").ap()

    xb = din("xb", [4096, D]); xo = din("xo", [1024, D]); cx = din("cx", [256, D])
    cT = din("cT", [128, 16, 2])
    wada = din("wada", [6, 16, 128, D]); bada2 = din("bada2", [2, 6 * D])
    g1_2 = din("g1_2", [2, D]); g2_2 = din("g2_2", [2, D])
    wkvg = din("wkvg", [16, 128, 1552])
    wtok = din("wtok", [6, 4, 128, 4, 512]); wg16 = din("wg16", [128, 16, 16])
    wconv = din("wconv", [8, 3, 128, 16, 128]); cw = din("cw", [128, 8, 3])
    gb = din("gb", [128, 16]); hg = din("hg", [128, 1024])
    wout = din("wout", [4, 4, 128, 4, 512])
    wr = din("wr", [128, 16, 64]); br = din("br", [128, 64])
    NEd = NE if stage == 'full' else 1
    STAGE = stage
    weg = din("weg", [NEd, NHT, 128, 16, 128]); weu = din("weu", [NEd, NHT, 128, 16, 128])
    wed = din("wed", [NEd, 4, 128, NHT, 512])
    wsg = din("wsg", [NHT, 128, 16, 128]); wsu = din("wsu", [NHT, 128, 16, 128]); wsd = din("wsd", [4, 128, NHT, 512])
    fg = din("fg", [128, D]); flags = din("flags", [128, 68]); shpos = din("shpos", [128, 8, 2])
    out = nc.dram_tensor("out", [1024, D], F32, kind="ExternalOutput").ap()

    with ExitStack() as st:
        S = Sched(nc, st)
        arena_t = st.enter_context(nc.sbuf_tensor("arena", [128, ARENA_BYTES // 2], BF16))
        A = Arena(arena_t)
        banks = [st.enter_context(nc.psum_tensor(f"bank{i}", [128, 512], F32)) for i in range(8)]

        def bk(i):
            return banks[i]

        def bkbf(i):
            return banks[i][:, :].bitcast(BF16)

        o_identb = A.alloc(256); identb = A.bf(o_identb, 128)
        o_iota = A.alloc(2048); iota = A.f32(o_iota, 512)
        o_stage = A.alloc(16384)
        PERSIST = A.top
        o_identf = A.alloc(512); identf = A.f32(o_identf, 128)
        o_Ub = A.alloc(256); Ub = A.bf(o_Ub, 128)
        o_Lb = A.alloc(256); Lb = A.bf(o_Lb, 128)
        o_Uf = A.alloc(512); Uf = A.f32(o_Uf, 128)
        o_Lf = A.alloc(512); Lf = A.f32(o_Lf, 128)
        o_onesf = A.alloc(512); onesf = A.f32(o_onesf, 128)
        o_onesb = A.alloc(256); onesb = A.bf(o_onesb, 128)
        o_sel0 = A.alloc(512); sel0 = A.f32(o_sel0, 128)
        o_sel1 = A.alloc(512); sel1 = A.f32(o_sel1, 128)
        o_flags = A.alloc(68 * 4); flg = A.f32(o_flags, 68)
        o_gb = A.alloc(64); gbt = A.f32(o_gb, 16)
        o_tmpi = A.alloc(2048); tmpi = A.i32(o_tmpi, 512)
        o_gs = A.alloc(16 * 4 * 8); gsm = A.f32(o_gs, 128)
        MIXBASE = A.top
        M = MIXBASE

        def tri(ap_, pattern, chm, cmp, key):
            S.op('pool', lambda e: e.memset(ap_, 1.0), writes=[key])
            S.op('pool', lambda e: e.affine_select(out=ap_, in_=ap_, pattern=pattern, compare_op=cmp, fill=0.0,
                                                   base=0, channel_multiplier=chm), reads=[key], writes=[key])

        tri(identb, [[-1, 128]], 1, ALU.is_equal, 'identb')
        tri(identf, [[-1, 128]], 1, ALU.is_equal, 'identf')
        tri(Ub, [[1, 128]], -1, ALU.is_ge, 'Ub')
        tri(Uf, [[1, 128]], -1, ALU.is_ge, 'Uf')
        tri(Lb, [[-1, 128]], 1, ALU.is_ge, 'Lb')
        tri(Lf, [[-1, 128]], 1, ALU.is_ge, 'Lf')
        S.op('pool', lambda e: e.memset(onesf, 1.0), writes=['onesf'])
        S.op('pool', lambda e: e.memset(onesb, 1.0), writes=['onesb'])
        S.op('pool', lambda e: e.memset(sel0, 1.0), writes=['sel0'])
        S.op('pool', lambda e: e.affine_select(out=sel0, in_=sel0, pattern=[[0, 128]], compare_op=ALU.is_equal, fill=0.0,
                                               base=0, channel_multiplier=1), reads=['sel0'], writes=['sel0'])
        S.op('pool', lambda e: e.memset(sel1, 1.0), writes=['sel1'])
        S.op('pool', lambda e: e.affine_select(out=sel1, in_=sel1, pattern=[[0, 128]], compare_op=ALU.is_equal, fill=0.0,
                                               base=-1, channel_multiplier=1), reads=['sel1'], writes=['sel1'])
        S.op('pool', lambda e: e.iota(tmpi, pattern=[[1, 512]], base=0, channel_multiplier=0), writes=['tmpi'])
        S.op('pool', lambda e: e.tensor_copy(out=iota, in_=tmpi), reads=['tmpi'], writes=['iota'])
        S.dma('d_flags', flg, flags[:, :], writes=['flags'])
        S.dma('d_gb', gbt, gb[:, :], writes=['gb'])

        stage_n = [0]

        def stage_piece(src_ap, nelem, nslots=2, slot_elems=2048):
            i = stage_n[0] % nslots
            stage_n[0] += 1
            key = f"stg{nslots}_{i}"
            ap_ = A.f32(o_stage + i * slot_elems * 4, nelem)
            S.dma(f"d_{key}", ap_, src_ap, writes=[key])
            return ap_, key

        o_row = None

        def mod_vec(j, s2, rowbuf, key):
            for kt in range(16):
                pc, pk = stage_piece(wada[j, kt], 2048)
                for c4 in range(4):
                    S.op('pe', lambda e: e.matmul(bk(c4)[0:2, :], lhsT=s2[:, kt, :], rhs=pc[:, c4 * 512:(c4 + 1) * 512],
                                                  start=(kt == 0), stop=(kt == 15)),
                         reads=[pk, 's2'], writes=[f'B{c4}'])
            for c4 in range(4):
                S.op('dve', lambda e: e.tensor_copy(out=rowbuf[0:2, c4 * 512:(c4 + 1) * 512], in_=bk(c4)[0:2, :]),
                     reads=[f'B{c4}'], writes=[key])

        def bcast(dst, dkey, row, rkey, sel, skey):
            for c4 in range(4):
                S.op('pe', lambda e: e.matmul(bk(4 + c4)[:, :], lhsT=sel[0:2, :], rhs=row[0:2, c4 * 512:(c4 + 1) * 512],
                                              start=True, stop=True), reads=[rkey, skey], writes=[f'B{4 + c4}'])
                S.op('act', lambda e: e.copy(out=dst[:, c4 * 512:(c4 + 1) * 512], in_=bk(4 + c4)[:, :]),
                     reads=[f'B{4 + c4}'], writes=[dkey])

        A.top = M
        o_bc = [A.alloc(8192) for _ in range(4)]
        bc = [A.f32(o, 2048) for o in o_bc]
        tmpl = {}

        def ada_temps(base, gsrc):
            A.top = base
            tmpl['rows'] = [A.f32(A.alloc(8192), 2048) for _ in range(2)]
            tmpl['b2'] = A.f32(A.alloc(8192), 2048)
            tmpl['g12'] = A.f32(A.alloc(8192), 2048)
            tmpl['s2'] = A.f32(A.alloc(128), 32).rearrange("p (k r) -> p k r", r=2)
            S.dma('d_s2', tmpl['s2'], cT[:, :, :], writes=['s2'])
            S.dma('d_g12', tmpl['g12'][0:2, :], gsrc[:, :], writes=['g12'])
            S.op('act', lambda e: e.activation(out=tmpl['s2'], in_=tmpl['s2'], func=AF.Silu), reads=['s2'], writes=['s2'])

        ada_temps(M + 32768, g1_2)
        rows = tmpl['rows']; g12 = tmpl['g12']

        def mod_row(j, rowbuf, key):
            b2 = tmpl['b2']
            S.dma('d_b2', b2[0:2, :], bada2[:, j * D:(j + 1) * D], reads=[], writes=['b2'])
            mod_vec(j, tmpl['s2'], rowbuf, key)
            S.op('dve', lambda e: e.tensor_tensor(out=rowbuf[0:2, :], in0=rowbuf[0:2, :], in1=b2[0:2, :], op=ALU.add),
                 reads=[key, 'b2'], writes=[key])

        mod_row(0, rows[0], 'row0')
        mod_row(1, rows[1], 'row1')
        S.op('dve', lambda e: e.scalar_tensor_tensor(out=rows[1][0:2, :], in0=rows[1][0:2, :], scalar=1.0, in1=g12[0:2, :],
                                                     op0=ALU.add, op1=ALU.mult), reads=['row1', 'g12'], writes=['row1'])
        bcast(bc[0], 'bc0', rows[1], 'row1', sel0, 'sel0')
        bcast(bc[1], 'bc1', rows[0], 'row0', sel0, 'sel0')
        bcast(bc[2], 'bc2', rows[1], 'row1', sel1, 'sel1')
        bcast(bc[3], 'bc3', rows[0], 'row0', sel1, 'sel1')

        if stage == 'ada':
            out_t = out.rearrange("(n p) f -> n p f", p=128)
            for t in range(4):
                S.dma(f'd_out{t}', out_t[t], bc[t], reads=[f'bc{t}'], writes=[f'out{t}'])
            S.finish('sp')
            return nc
        S.barrier()
        A.top = M + 32768
        SCANBASE = A.top
        o_t1 = A.alloc(8192); t1 = A.f32(o_t1, 2048)
        o_ss = A.alloc(64); ssb = A.f32(o_ss, 16)
        o_xn = [A.alloc(4096) for _ in range(2)]
        xnb = [A.bf(o, 2048) for o in o_xn]
        nm_n = [0]

        def norm_mod(xp, xkey, gm, gmkey, sh, shkey, dst, dkey):
            i = nm_n[0] % 8
            nm_n[0] += 1
            ss = ssb[:, i:i + 1]
            sk = f'ss{i}'
            S.op('act', lambda e: e.activation(out=t1, in_=xp, func=AF.Square, accum_out=ss), reads=[xkey], writes=['t1', sk])
            S.op('dve', lambda e: e.tensor_scalar(out=ss, in0=ss, scalar1=1.0 / D, scalar2=EPS, op0=ALU.mult, op1=ALU.add),
                 reads=[sk], writes=[sk])
            S.op('act', lambda e: e.activation(out=ss, in_=ss, func=AF.Ln), reads=[sk], writes=[sk])
            S.op('act', lambda e: e.activation(out=ss, in_=ss, func=AF.Exp, scale=-0.5), reads=[sk], writes=[sk])
            S.op('dve', lambda e: e.scalar_tensor_tensor(out=t1, in0=xp, scalar=ss, in1=gm, op0=ALU.mult, op1=ALU.mult),
                 reads=[xkey, sk, gmkey], writes=['t1'])
            S.op('pool', lambda e: e.tensor_tensor(out=dst, in0=t1, in1=sh, op=ALU.add), reads=['t1', shkey], writes=[dkey])

        def transpose16(src, skey, dst3, dkeys, b0, b1, evac='act'):
            for half, b in ((0, b0), (1, b1)):
                pb = bkbf(b)
                for k8 in range(8):
                    kt = half * 8 + k8
                    S.op('pe', lambda e: e.transpose(out=pb[:, k8 * 128:(k8 + 1) * 128], in_=src[:, kt * 128:(kt + 1) * 128],
                                                     identity=identb), reads=[skey, 'identb'], writes=[f'B{b}'])
                dk = dkeys if isinstance(dkeys, list) else [dkeys]
                wk = dk[half * 8:(half + 1) * 8] if len(dk) == 16 else dk
                eng = evac if half == 0 else ('dve' if evac == 'act' else 'act')
                if eng == 'act':
                    S.op('act', lambda e: e.copy(out=dst3[:, half * 8:(half + 1) * 8, :],
                                                 in_=pb.rearrange("p (k t) -> p k t", t=128)), reads=[f'B{b}'], writes=wk)
                else:
                    S.op('dve', lambda e: e.tensor_copy(out=dst3[:, half * 8:(half + 1) * 8, :],
                                                        in_=pb.rearrange("p (k t) -> p k t", t=128)), reads=[f'B{b}'], writes=wk)

        o_wk = A.alloc(16 * 512 * 2); Wk = A.bf(o_wk, 16 * 512).rearrange("p (k n) -> p k n", n=512)
        o_wv = A.alloc(16 * 1024 * 2); Wv = A.bf(o_wv, 16 * 1024).rearrange("p (k n) -> p k n", n=1024)
        o_wg = A.alloc(16 * 16 * 2); Wg16 = A.bf(o_wg, 256).rearrange("p (k n) -> p k n", n=16)
        for kt in range(16):
            pc, pk = stage_piece(wkvg[kt], 1552)
            S.op('act', lambda e: e.copy(out=Wk[:, kt, :], in_=pc[:, 0:512]), reads=[pk], writes=['Wk'])
            S.op('dve', lambda e: e.tensor_copy(out=Wv[:, kt, :], in_=pc[:, 512:1536]), reads=[pk], writes=['Wv'])
            S.op('pool', lambda e: e.tensor_copy(out=Wg16[:, kt, :], in_=pc[:, 1536:1552]), reads=[pk], writes=['Wg16'])
        o_xnT = [A.alloc(4096) for _ in range(2)]
        xnT = [A.bf(o, 2048).rearrange("p (k t) -> p k t", t=128) for o in o_xnT]
        o_CT = A.alloc(2 * 4 * 256 * 4); CT = A.f32(o_CT, 2048).rearrange("p (d h v) -> p d h v", d=2, h=4)
        o_nst = A.alloc(32); nst = A.f32(o_nst, 8).rearrange("p (d h) -> p d h", d=2)
        o_CS = A.alloc(2 * 4 * 256 * 4); CSv = A.f32(o_CS, 2048).rearrange("p (d h v) -> p d h v", d=2, h=4)
        o_ns = A.alloc(32); nsv = A.f32(o_ns, 8).rearrange("p (d h) -> p d h", d=2)
        o_ks = [A.alloc(1024) for _ in range(2)]
        ksb = [A.bf(o, 512).rearrange("p (h d) -> p h d", h=4) for o in o_ks]
        o_v1 = [A.alloc(2048) for _ in range(2)]
        v1b = [A.bf(o, 1024).rearrange("p (h v) -> p h v", h=4) for o in o_v1]
        for z, zk in ((CT, 'CT'), (CSv, 'CS')):
            S.op('pool', lambda e: e.memset(z.rearrange("p d h v -> p (d h v)"), 0.0), writes=[zk + '0', zk + '1'])
        S.op('pool', lambda e: e.memset(nst.rearrange("p d h -> p (d h)"), 0.0), writes=['n0', 'n1'])
        S.op('pool', lambda e: e.memset(nsv.rearrange("p d h -> p (d h)"), 0.0), writes=['ns0', 'ns1'])

        gs_n = [0]

        def gates_dir(gpre, gkey, d):
            i = gs_n[0] % 2
            gs_n[0] += 1
            base = i * 64
            sp = gsm[:, base + 0:base + 4]; wq = gsm[:, base + 4:base + 8]; wk_ = gsm[:, base + 8:base + 12]
            aa = gsm[:, base + 12:base + 16]; tmp = gsm[:, base + 16:base + 20]
            k_ = f'gs{i}'
            ipre = gpre[:, d * 8 + 0:d * 8 + 4]
            fpre = gpre[:, d * 8 + 4:d * 8 + 8]
            S.op('act', lambda e: e.activation(out=sp, in_=fpre, func=AF.Exp, scale=-1.0), reads=[gkey], writes=[k_])
            S.op('dve', lambda e: e.tensor_scalar(out=sp, in0=sp, scalar1=1.0, scalar2=None, op0=ALU.add), reads=[k_], writes=[k_])
            S.op('act', lambda e: e.activation(out=sp, in_=sp, func=AF.Ln), reads=[k_], writes=[k_])
            tri_ = Uf if d == 0 else Lf
            S.op('pe', lambda e: e.matmul(bk(7)[:, 0:4], lhsT=tri_, rhs=sp, start=True, stop=True), reads=[k_, 'Uf', 'Lf'], writes=['B7'])
            S.op('pe', lambda e: e.matmul(bk(7)[:, 4:8], lhsT=onesf, rhs=sp, start=True, stop=True), reads=[k_, 'onesf'], writes=['B7'])
            S.op('act', lambda e: e.activation(out=wq, in_=bk(7)[:, 0:4], func=AF.Exp, scale=-1.0), reads=['B7'], writes=[k_])
            S.op('dve', lambda e: e.tensor_tensor(out=tmp, in0=bk(7)[:, 0:4], in1=ipre, op=ALU.add), reads=['B7', gkey], writes=[k_])
            S.op('act', lambda e: e.activation(out=aa, in_=bk(7)[:, 4:8], func=AF.Exp, scale=-1.0), reads=['B7'], writes=[k_])
            S.op('act', lambda e: e.activation(out=wk_, in_=tmp, func=AF.Exp), reads=[k_], writes=[k_])
            return wq, wk_, aa, k_

        def state_update(d, ks, kskey, v1, v1key, aa, akey, flag_idx):
            for h in range(4):
                b = 3 + h // 2
                S.op('pe', lambda e: e.matmul(bk(b)[:, (h % 2) * 256:(h % 2) * 256 + 256], lhsT=ks[:, h, :], rhs=v1[:, h, :],
                                              start=True, stop=True), reads=[kskey, v1key], writes=[f'B{b}'])
            for h in range(4):
                S.op('pe', lambda e: e.matmul(bk(7)[:, 8 + h:9 + h], lhsT=ks[:, h, :], rhs=onesb[:, 0:1], start=True, stop=True),
                     reads=[kskey, 'onesb'], writes=['B7'])
            ck = f'CT{d}'
            for hp in range(2):
                S.op('dve', lambda e: e.tensor_tensor(out=CT[:, d, 2 * hp:2 * hp + 2, :],
                                                      in0=CT[:, d, 2 * hp:2 * hp + 2, :],
                                                      in1=bk(3 + hp)[:, :].rearrange("p (h v) -> p h v", h=2), op=ALU.add),
                     reads=[f'B{3 + hp}', ck], writes=[ck])
            for h in range(4):
                S.op('act', lambda e: e.activation(out=CT[:, d, h, :], in_=CT[:, d, h, :], func=AF.Copy, scale=aa[:, h:h + 1]),
                     reads=[ck, akey], writes=[ck])
            nk = f'n{d}'
            S.op('dve', lambda e: e.tensor_tensor(out=nst[:, d, :], in0=nst[:, d, :], in1=bk(7)[:, 8:12], op=ALU.add),
                 reads=['B7', nk], writes=[nk])
            S.op('dve', lambda e: e.tensor_tensor(out=nst[:, d, :], in0=nst[:, d, :], in1=aa, op=ALU.mult), reads=[nk, akey], writes=[nk])
            if flag_idx is not None:
                fl = flg[:, flag_idx:flag_idx + 1]
                S.op('dve', lambda e: e.scalar_tensor_tensor(out=CSv[:, d].rearrange("p h v -> p (h v)"),
                                                              in0=CT[:, d].rearrange("p h v -> p (h v)"), scalar=fl,
                                                              in1=CSv[:, d].rearrange("p h v -> p (h v)"), op0=ALU.mult, op1=ALU.add),
                     reads=[ck, 'flags', f'CS{d}'], writes=[f'CS{d}'])
                S.op('dve', lambda e: e.scalar_tensor_tensor(out=nsv[:, d, :], in0=nst[:, d, :], scalar=fl, in1=nsv[:, d, :],
                                                              op0=ALU.mult, op1=ALU.add), reads=[nk, 'flags', f'ns{d}'], writes=[f'ns{d}'])

        sc_n = [0]

        def scan_tile(src_rows, gm, gmk, sh, shk, d, flag_idx):
            i = sc_n[0] % 2
            sc_n[0] += 1
            xp, xk = stage_piece(src_rows, 2048)
            norm_mod(xp, xk, gm, gmk, sh, shk, xnb[i], f'xnb{i}')
            transpose16(xnb[i], f'xnb{i}', xnT[i], f'xnT{i}', 5, 6)
            xt = xnT[i]; xtk = f'xnT{i}'
            for kt in range(16):
                S.op('pe', lambda e: e.matmul(bk(0)[:, :], lhsT=xt[:, kt, :], rhs=Wk[:, kt, :], start=(kt == 0), stop=(kt == 15)),
                     reads=[xtk, 'Wk'], writes=['B0'])
            for vh in range(2):
                for kt in range(16):
                    S.op('pe', lambda e: e.matmul(bk(1 + vh)[:, :], lhsT=xt[:, kt, :], rhs=Wv[:, kt, vh * 512:(vh + 1) * 512],
                                                  start=(kt == 0), stop=(kt == 15)), reads=[xtk, 'Wv'], writes=[f'B{1 + vh}'])
            for kt in range(16):
                S.op('pe', lambda e: e.matmul(bk(7)[:, 16:32], lhsT=xt[:, kt, :], rhs=Wg16[:, kt, :], start=(kt == 0), stop=(kt == 15)),
                     reads=[xtk, 'Wg16'], writes=['B7'])
            gp = gsm[:, 40 + i * 16 - 40 * 0: 40 + i * 16 + 16] if False else gsm[:, 96 + i * 16:96 + i * 16 + 16]
            gk = f'gpre{i}'
            S.op('dve', lambda e: e.tensor_tensor(out=gp, in0=bk(7)[:, 16:32], in1=gbt, op=ALU.add), reads=['B7', 'gb'], writes=[gk])
            wq, wk_, aa, gsk = gates_dir(gp, gk, d)
            ks = ksb[i]; v1 = v1b[i]
            for h in range(4):
                S.op('dve', lambda e: e.tensor_scalar(out=ks[:, h, :], in0=bk(0)[:, h * 128:(h + 1) * 128], scalar1=wk_[:, h:h + 1],
                                                      scalar2=None, op0=ALU.mult), reads=['B0', gsk], writes=[f'ks{i}'])
            for vh in range(2):
                S.op('act', lambda e: e.copy(out=v1[:, 2 * vh:2 * vh + 2, :], in_=bk(1 + vh)[:, :].rearrange("p (h v) -> p h v", h=2)),
                     reads=[f'B{1 + vh}'], writes=[f'v1{i}'])
            state_update(d, ks, f'ks{i}', v1, f'v1{i}', aa, gsk, flag_idx)

        ctx_t = cx.rearrange("(n p) f -> n p f", p=128)
        xb_t = xb.rearrange("(n p) f -> n p f", p=128)
        fwd = [(ctx_t[0], 2, 3, None), (ctx_t[1], 2, 3, 0)] + [(xb_t[j], 0, 1, j + 1) for j in range(NT_B)]
        bwd = [(ctx_t[1], 2, 3, None), (ctx_t[0], 2, 3, 33)] + [(xb_t[NT_B - 1 - j], 0, 1, 34 + j) for j in range(NT_B)]
        if stage.startswith('scan'):
            nst_ = int(stage[4:])
            out_t = out.rearrange("(n p) f -> n p f", p=128)
            if nst_ == 0:
                xp, xk = stage_piece(ctx_t[0], 2048)
                norm_mod(xp, xk, bc[2], 'bc2', bc[3], 'bc3', xnb[0], 'xnb0')
                transpose16(xnb[0], 'xnb0', xnT[0], 'xnT0', 5, 6)
                S.op('dve', lambda e: e.tensor_copy(out=t1, in_=xnT[0].rearrange("p k t -> p (k t)")), reads=['xnT0'], writes=['t1'])
                S.dma('d_out0', out_t[0], t1, reads=['t1'], writes=['out0'])
            else:
                for stp in range(nst_):
                    for d, lst in ((0, fwd), (1, bwd)):
                        src, gi, si, fi = lst[stp]
                        scan_tile(src, bc[gi], f'bc{gi}', bc[si], f'bc{si}', d, 0)
                S.dma('d_out0', out_t[0], CT.rearrange("p d h v -> p (d h v)"), reads=['CT0', 'CT1'], writes=['out0'])
            S.finish('sp')
            return nc
        for stp in range(len(fwd)):
            for d, lst in ((0, fwd), (1, bwd)):
                src, gi, si, fi = lst[stp]
                scan_tile(src, bc[gi], f'bc{gi}', bc[si], f'bc{si}', d, fi)

        S.barrier()
        A.top = M + 16384
        o_wg2 = A.alloc(512); Wg16b = A.bf(o_wg2, 256).rearrange("p (k n) -> p k n", n=16)
        o_CTs = A.alloc(8192); CT2 = A.f32(o_CTs, 2048).rearrange("p (d h v) -> p d h v", d=2, h=4)
        o_n2 = A.alloc(32); n2 = A.f32(o_n2, 8).rearrange("p (d h) -> p d h", d=2)
        o_cw = A.alloc(96); cwt = A.f32(o_cw, 24).rearrange("p (c j) -> p c j", j=3)
        o_hg = A.alloc(4096); hgt = A.f32(o_hg, 1024)
        assert A.top <= M + 32768
        S.op('dve', lambda e: e.tensor_copy(out=CT2.rearrange("p d h v -> p (d h v)"), in_=CSv.rearrange("p d h v -> p (d h v)")),
             writes=['CT0', 'CT1'])
        S.op('dve', lambda e: e.tensor_copy(out=n2.rearrange("p d h -> p (d h)"), in_=nsv.rearrange("p d h -> p (d h)")), writes=['n0', 'n1'])
        S.op('dve', lambda e: e.tensor_copy(out=Wg16b.rearrange("p k n -> p (k n)"), in_=Wg16.rearrange("p k n -> p (k n)")), writes=['Wg16'])
        S.barrier()
        CTo, no_ = CT2, n2
        A.top = o_xn[1] + 4096
        OWN = A.top
        o_q = A.alloc(8 * 512 * 2); qst = A.bf(o_q, 4096).rearrange("p (t n) -> p t n", t=8)
        o_k = A.alloc(8 * 512 * 2); kst = A.bf(o_k, 4096).rearrange("p (t n) -> p t n", t=8)
        o_v = A.alloc(8 * 1024 * 2); vst = A.bf(o_v, 8192).rearrange("p (t n) -> p t n", t=8)
        o_so = A.alloc(8 * 1024 * 2); sgo = A.bf(o_so, 8192).rearrange("p (t n) -> p t n", t=8)
        o_gp = A.alloc(8 * 16 * 4); gpst = A.f32(o_gp, 128).rearrange("p (t n) -> p t n", t=8)
        o_cv = A.alloc(8 * 1024 * 2); convT = A.bf(o_cv, 8192).rearrange("p (c t) -> p c t", c=8)
        S.dma('d_cw', cwt, cw[:, :, :], writes=['cw'])
        S.dma('d_hg', hgt, hg[:, :], writes=['hg'])
        O1 = A.top
        o_xT = A.alloc(16 * 1024 * 2); xTo = A.bf(o_xT, 16384).rearrange("p (k t) -> p k t", k=16)
        o_wc = [A.alloc(16 * 512 * 2) for _ in range(2)]
        Wc = [A.bf(o, 8192).rearrange("p (k n) -> p k n", k=16) for o in o_wc]

        xo_t = xo.rearrange("(n p) f -> n p f", p=128)
        for t in range(NT_OWN):
            xp, xk = stage_piece(xo_t[t], 2048)
            i = t % 2
            norm_mod(xp, xk, bc[0], 'bc0', bc[1], 'bc1', xnb[i], f'xnb{i}')
            transpose16(xnb[i], f'xnb{i}', xTo[:, :, t * 128:(t + 1) * 128], f'xTo{t}', 5, 6)

        for t in range(NT_OWN):
            for kt in range(16):
                S.op('pe', lambda e: e.matmul(bk(7)[:, 16:32], lhsT=xTo[:, kt, t * 128:(t + 1) * 128], rhs=Wg16b[:, kt, :],
                                              start=(kt == 0), stop=(kt == 15)), reads=[f'xTo{t}', 'Wg16'], writes=['B7'])
            S.op('dve', lambda e: e.tensor_tensor(out=gpst[:, t, :], in0=bk(7)[:, 16:32], in1=gbt, op=ALU.add),
                 reads=['B7', 'gb'], writes=[f'gpst{t}'])

        wc_n = [0]

        def load_wchunk(src4):
            i = wc_n[0] % 2
            wc_n[0] += 1
            W = Wc[i]
            for pi in range(4):
                pc, pk = stage_piece(src4[pi].rearrange("p k n -> p (k n)"), 2048)
                eng = ('act', 'dve', 'pool', 'dve')[pi]
                if eng == 'act':
                    S.op('act', lambda e: e.copy(out=W[:, pi * 4:(pi + 1) * 4, :].rearrange("p k n -> p (k n)"), in_=pc), reads=[pk], writes=[f'Wc{i}'])
                else:
                    S.op(eng, lambda e: e.tensor_copy(out=W[:, pi * 4:(pi + 1) * 4, :].rearrange("p k n -> p (k n)"), in_=pc), reads=[pk], writes=[f'Wc{i}'])
            return W, f'Wc{i}'

        for ch in range(6):
            W, wkk = load_wchunk(wtok[ch])
            for t in range(NT_OWN):
                b = t % 2
                for kt in range(16):
                    S.op('pe', lambda e: e.matmul(bk(b)[:, :], lhsT=xTo[:, kt, t * 128:(t + 1) * 128], rhs=W[:, kt, :],
                                                  start=(kt == 0), stop=(kt == 15)), reads=[f'xTo{t}', wkk], writes=[f'B{b}'])
                if ch == 0:
                    S.op('act', lambda e: e.copy(out=qst[:, t, :], in_=bk(b)[:, :]), reads=[f'B{b}'], writes=[f'q{t}'])
                elif ch == 1:
                    S.op('dve', lambda e: e.tensor_copy(out=kst[:, t, :], in_=bk(b)[:, :]), reads=[f'B{b}'], writes=[f'k{t}'])
                elif ch in (2, 3):
                    S.op('act', lambda e: e.copy(out=vst[:, t, (ch - 2) * 512:(ch - 1) * 512], in_=bk(b)[:, :]), reads=[f'B{b}'], writes=[f'v{t}'])
                else:
                    S.op('act', lambda e: e.activation(out=sgo[:, t, (ch - 4) * 512:(ch - 3) * 512], in_=bk(b)[:, :], func=AF.Sigmoid),
                         reads=[f'B{b}'], writes=[f'so{t}'])

        if stage == 'own1a':
            S.barrier()
            out_t = out.rearrange("(n p) f -> n p f", p=128)
            S.dma('d_out0', out_t[0][:, 0:128], gpst.rearrange("p t n -> p (t n)"), writes=['out0'])
            S.finish('sp')
            return nc
        S.barrier()
        A.top = o_wc[0]
        o_cvt = A.alloc(3 * 2048); cvt = [A.f32(o_cvt + i * 2048, 512) for i in range(3)]
        o_wcv = A.alloc(3 * 4096); Wcv = [A.bf(o_wcv + i * 4096, 2048).rearrange("p (k n) -> p k n", k=16) for i in range(3)]
        for ct in range(8):
            for part in range(3):
                pc, pk = stage_piece(wconv[ct, part].rearrange("p k n -> p (k n)"), 2048)
                if part == 0:
                    S.op('act', lambda e: e.copy(out=Wcv[part].rearrange("p k n -> p (k n)"), in_=pc), reads=[pk], writes=[f'Wcv{part}'])
                else:
                    S.op('dve' if part == 1 else 'pool', lambda e: e.tensor_copy(out=Wcv[part].rearrange("p k n -> p (k n)"), in_=pc),
                         reads=[pk], writes=[f'Wcv{part}'])
            for th in range(2):
                for part in range(3):
                    b = 2 + part
                    for kt in range(16):
                        S.op('pe', lambda e: e.matmul(bk(b)[:, :], lhsT=Wcv[part][:, kt, :], rhs=xTo[:, kt, th * 512:(th + 1) * 512],
                                                      start=(kt == 0), stop=(kt == 15)),
                             reads=[f'Wcv{part}'] + [f'xTo{th * 4 + q}' for q in range(4)], writes=[f'B{b}'])
                cs, u, y = cvt
                S.op('act', lambda e: e.copy(out=cs, in_=bk(3)[:, :]), reads=['B3'], writes=['cv_c'])
                S.op('dve', lambda e: e.tensor_tensor(out=u, in0=cs, in1=bk(4)[:, :], op=ALU.mult), reads=['cv_c', 'B4'], writes=['cv_u'])
                u3 = u.rearrange("p (r w) -> p r w", w=64); y3 = y.rearrange("p (r w) -> p r w", w=64)
                S.op('act', lambda e: e.activation(out=y, in_=u, func=AF.Copy, scale=cwt[:, ct, 1:2]), reads=['cv_u', 'cw'], writes=['cv_y'])
                S.op('dve', lambda e: e.scalar_tensor_tensor(out=y3[:, :, 1:64], in0=u3[:, :, 0:63], scalar=cwt[:, ct, 0:1], in1=y3[:, :, 1:64],
                                                             op0=ALU.mult, op1=ALU.add), reads=['cv_u', 'cv_y', 'cw'], writes=['cv_y'])
                S.op('dve', lambda e: e.scalar_tensor_tensor(out=y3[:, :, 0:63], in0=u3[:, :, 1:64], scalar=cwt[:, ct, 2:3], in1=y3[:, :, 0:63],
                                                             op0=ALU.mult, op1=ALU.add), reads=['cv_u', 'cv_y', 'cw'], writes=['cv_y'])
                S.op('dve', lambda e: e.tensor_tensor(out=convT[:, ct, th * 512:(th + 1) * 512], in0=y, in1=bk(2)[:, :], op=ALU.mult),
                     reads=['cv_y', 'B2'], writes=[f'convT{th}'])

        if stage == 'own1b':
            S.barrier()
            out_t = out.rearrange("(n p) f -> n p f", p=128)
            S.dma('d_out0', out_t[0][:, 0:128], gpst.rearrange("p t n -> p (t n)"), writes=['out0'])
            S.finish('sp')
            return nc
        S.barrier()
        A.top = O1
        o_hf = A.alloc(8 * 1024 * 4); hf = A.f32(o_hf, 8192).rearrange("p (t n) -> p t n", t=8)
        o_mx = A.alloc(8 * 1024 * 2); mxT = A.bf(o_mx, 8192).rearrange("p (c t) -> p c t", c=8)
        o_qs = A.alloc(1024); qsb = A.bf(o_qs, 512).rearrange("p (h d) -> p h d", h=4)
        o_ks2 = A.alloc(1024); ks2 = A.bf(o_ks2, 512).rearrange("p (h d) -> p h d", h=4)
        o_qT = A.alloc(1024); qsT = A.bf(o_qT, 512).rearrange("p (h d) -> p h d", h=4)
        o_kT = A.alloc(1024); ksT = A.bf(o_kT, 512).rearrange("p (h d) -> p h d", h=4)
        o_sq = A.alloc(1024); sqk = A.bf(o_sq, 512).rearrange("p (h d) -> p h d", h=4)
        o_CTb = A.alloc(2048); CTb = A.bf(o_CTb, 1024).rearrange("p (h v) -> p h v", h=4)
        o_nb = A.alloc(32); nbb = A.bf(o_nb, 16)
        o_dd = A.alloc(64); ddt = A.f32(o_dd, 16)
        A.top = M + 32768
        o_hs = A.alloc(4096); hs = A.f32(o_hs, 1024)
        o_hq = A.alloc(4096); hq = A.f32(o_hq, 1024)
        o_mb = A.alloc(2048); mxb = A.bf(o_mb, 1024)
        CT = CTo; nst = no_

        class _Stop(Exception):
            pass

        def ckpt(i):
            if stage == f'o2_{i}':
                raise _Stop()

        def own_tile(t, d):
            gk = f'gpst{t}'
            wq, wk_, aa, gsk = gates_dir(gpst[:, t, :], gk, d)
            for h in range(4):
                S.op('dve', lambda e: e.tensor_scalar(out=qsb[:, h, :], in0=qst[:, t, h * 128:(h + 1) * 128], scalar1=wq[:, h:h + 1],
                                                      scalar2=QSCALE, op0=ALU.mult, op1=ALU.mult), reads=[f'q{t}', gsk], writes=['qsb'])
                S.op('pool', lambda e: e.tensor_scalar(out=ks2[:, h, :], in0=kst[:, t, h * 128:(h + 1) * 128], scalar1=wk_[:, h:h + 1],
                                                       scalar2=None, op0=ALU.mult), reads=[f'k{t}', gsk], writes=['ks2'])
            ckpt(1)
            pb = bkbf(5)
            pb6 = bkbf(6)
            for h in range(4):
                S.op('pe', lambda e: e.transpose(out=pb[:, h * 128:(h + 1) * 128], in_=qsb[:, h, :], identity=identb),
                     reads=['qsb', 'identb'], writes=['B5'])
                S.op('pe', lambda e: e.transpose(out=pb6[:, h * 128:(h + 1) * 128], in_=ks2[:, h, :], identity=identb),
                     reads=['ks2', 'identb'], writes=['B6'])
            S.op('act', lambda e: e.copy(out=qsT.rearrange("p h d -> p (h d)"), in_=pb[:, 0:512]), reads=['B5'], writes=['qsT'])
            S.op('dve', lambda e: e.tensor_copy(out=ksT.rearrange("p h d -> p (h d)"), in_=pb6[:, 0:512]), reads=['B6'], writes=['ksT'])
            ckpt(2)
            ck = f'CT{d}'; nk = f'n{d}'
            S.op('act', lambda e: e.copy(out=CTb, in_=CT[:, d]), reads=[ck], writes=['CTb'])
            S.op('dve', lambda e: e.tensor_copy(out=nbb[:, 0:4], in_=nst[:, d, :]), reads=[nk], writes=['nbb'])
            for h in range(4):
                S.op('pe', lambda e: e.matmul(bk(6)[:, h * 128:(h + 1) * 128], lhsT=ksT[:, h, :], rhs=qsT[:, h, :], start=True, stop=True),
                     reads=['ksT', 'qsT'], writes=['B6'])
            ckpt(3)
            msk = Ub if d == 0 else Lb
            for h in range(4):
                S.op('dve', lambda e: e.tensor_tensor(out=sqk[:, h, :], in0=bk(6)[:, h * 128:(h + 1) * 128], in1=msk, op=ALU.mult),
                     reads=['B6', 'Ub', 'Lb'], writes=['sqk'])
            ckpt(4)
            vv = vst[:, t, :].rearrange("p (h v) -> p h v", h=4)
            for h in range(4):
                b = h // 2
                osl = bk(b)[:, (h % 2) * 256:(h % 2) * 256 + 256]
                S.op('pe', lambda e: e.matmul(osl, lhsT=sqk[:, h, :], rhs=vv[:, h, :], start=True, stop=False),
                     reads=['sqk', f'v{t}'], writes=[f'B{b}'])
                S.op('pe', lambda e: e.matmul(osl, lhsT=qsT[:, h, :], rhs=CTb[:, h, :], start=False, stop=True),
                     reads=['qsT', 'CTb'], writes=[f'B{b}'])
                S.op('pe', lambda e: e.matmul(bk(7)[:, 32 + h:33 + h], lhsT=sqk[:, h, :], rhs=onesb[:, 0:1], start=True, stop=False),
                     reads=['sqk', 'onesb'], writes=['B7'])
                S.op('pe', lambda e: e.matmul(bk(7)[:, 32 + h:33 + h], lhsT=qsT[:, h, :], rhs=nbb[:, h:h + 1], start=False, stop=True),
                     reads=['qsT', 'nbb'], writes=['B7'])
            ckpt(5)
            dd = ddt[:, 0:4]
            S.op('act', lambda e: e.activation(out=dd, in_=bk(7)[:, 32:36], func=AF.Abs), reads=['B7'], writes=['dd'])
            S.op('dve', lambda e: e.tensor_scalar(out=dd, in0=dd, scalar1=1.0, scalar2=None, op0=ALU.max), reads=['dd'], writes=['dd'])
            S.op('dve', lambda e: e.reciprocal(out=dd, in_=dd), reads=['dd'], writes=['dd'])
            for h in range(4):
                b = h // 2
                osl = bk(b)[:, (h % 2) * 256:(h % 2) * 256 + 256]
                if d == 0:
                    S.op('act', lambda e: e.activation(out=hf[:, t, h * 256:(h + 1) * 256], in_=osl, func=AF.Copy, scale=dd[:, h:h + 1]),
                         reads=[f'B{b}', 'dd'], writes=[f'hf{t}'])
                else:
                    S.op('dve', lambda e: e.scalar_tensor_tensor(out=hs[:, h * 256:(h + 1) * 256], in0=osl, scalar=dd[:, h:h + 1],
                                                                 in1=hf[:, t, h * 256:(h + 1) * 256], op0=ALU.mult, op1=ALU.add),
                         reads=[f'B{b}', 'dd', f'hf{t}'], writes=['hs'])
            ckpt(6)
            state_update(d, ks2, 'ks2', vv, f'v{t}', aa, gsk, None)
            ckpt(7)
            if d == 1:
                ssq = ddt[:, 4:8]
                S.op('pool', lambda e: e.tensor_tensor(out=hq, in0=hs, in1=hs, op=ALU.mult), reads=['hs'], writes=['hq'])
                S.op('dve', lambda e: e.tensor_reduce(out=ssq, in_=hq.rearrange("p (h v) -> p h v", h=4), axis=AX.X, op=ALU.add),
                     reads=['hq'], writes=['ssq'])
                S.op('dve', lambda e: e.tensor_scalar(out=ssq, in0=ssq, scalar1=1.0 / 256, scalar2=EPS, op0=ALU.mult, op1=ALU.add),
                     reads=['ssq'], writes=['ssq'])
                S.op('act', lambda e: e.activation(out=ssq, in_=ssq, func=AF.Ln), reads=['ssq'], writes=['ssq'])
                S.op('act', lambda e: e.activation(out=ssq, in_=ssq, func=AF.Exp, scale=-0.5), reads=['ssq'], writes=['ssq'])
                for h in range(4):
                    S.op('dve', lambda e: e.scalar_tensor_tensor(out=hq[:, h * 256:(h + 1) * 256], in0=hs[:, h * 256:(h + 1) * 256],
                                                                  scalar=ssq[:, h:h + 1], in1=hgt[:, h * 256:(h + 1) * 256],
                                                                  op0=ALU.mult, op1=ALU.mult), reads=['hs', 'ssq', 'hg'], writes=['hq'])
                S.op('dve', lambda e: e.tensor_tensor(out=mxb, in0=hq, in1=sgo[:, t, :], op=ALU.mult), reads=['hq', f'so{t}'], writes=['mxb'])
                pb2 = bkbf(5)
                for c8 in range(8):
                    S.op('pe', lambda e: e.transpose(out=pb2[:, c8 * 128:(c8 + 1) * 128], in_=mxb[:, c8 * 128:(c8 + 1) * 128], identity=identb),
                         reads=['mxb', 'identb'], writes=['B5'])
                S.op('act', lambda e: e.copy(out=mxT[:, :, t * 128:(t + 1) * 128], in_=pb2.rearrange("p (c t) -> p c t", t=128)),
                     reads=['B5'], writes=[f'mxT{t}'])

        try:
            for t in range(NT_OWN):
                own_tile(t, 0)
            ckpt(8)
            for t in reversed(range(NT_OWN)):
                own_tile(t, 1)
                ckpt(9)
        except _Stop:
            S.barrier()
            out_t = out.rearrange("(n p) f -> n p f", p=128)
            S.dma('d_out0', out_t[0][:, 0:1024], hf[:, 0, :], writes=['out0'])
            S.finish('sp')
            return nc

        if stage == 'own2':
            S.barrier()
            out_t = out.rearrange("(n p) f -> n p f", p=128)
            S.dma('d_out0', out_t[0][:, 0:1024], hf[:, 0, :], writes=['out0'])
            S.finish('sp')
            return nc
        S.barrier()
        A.top = O1
        o_bcx = [A.alloc(8192) for _ in range(3)]
        bcx = [A.f32(o, 2048) for o in o_bcx]
        o_gt2 = A.alloc(8192); gt2bc = A.f32(o_gt2, 2048)
        assert A.top <= o_mx
        A.top = o_mx + 16384
        o_tmp = A.alloc(2048); tmpc = A.f32(o_tmp, 512)
        ada_temps(OWN, g2_2)
        rows = tmpl['rows']; g12 = tmpl['g12']
        assert A.top <= o_cv
        mod_row(2, rows[0], 'row0')
        bcast(bcx[0], 'bcx0', rows[0], 'row0', sel0, 'sel0')
        mod_row(3, rows[0], 'row0')
        mod_row(4, rows[1], 'row1')
        S.op('dve', lambda e: e.scalar_tensor_tensor(out=rows[1][0:2, :], in0=rows[1][0:2, :], scalar=1.0, in1=g12[0:2, :],
                                                     op0=ALU.add, op1=ALU.mult), reads=['row1', 'g12'], writes=['row1'])
        bcast(bcx[1], 'bcx1', rows[1], 'row1', sel0, 'sel0')
        bcast(bcx[2], 'bcx2', rows[0], 'row0', sel0, 'sel0')
        mod_row(5, rows[0], 'row0')
        bcast(gt2bc, 'gt2bc', rows[0], 'row0', sel0, 'sel0')

        if stage == 'ada2':
            S.barrier()
            out_t = out.rearrange("(n p) f -> n p f", p=128)
            S.dma('d_out0', out_t[0], gt2bc, writes=['out0'])
            S.finish('sp')
            return nc
        S.barrier()
        o_x1 = M
        x1 = A.f32(o_x1, 8 * 2048).rearrange("p (t n) -> p t n", t=8)
        A.top = M + 65536
        o_wc2 = [A.alloc(16 * 512 * 2) for _ in range(2)]
        assert A.top <= o_cv, (A.top, o_cv)
        Wc[0] = A.bf(o_wc2[0], 8192).rearrange("p (k n) -> p k n", k=16)
        Wc[1] = A.bf(o_wc2[1], 8192).rearrange("p (k n) -> p k n", k=16)
        for t in range(NT_OWN):
            S.dma(f'd_x1_{t}', x1[:, t, :], xo_t[t], writes=[f'x1_{t}'])
        for ch in range(4):
            W, wkk = load_wchunk(wout[ch])
            for t in range(NT_OWN):
                b = t % 2
                for ft in range(16):
                    lh = convT[:, ft, t * 128:(t + 1) * 128] if ft < 8 else mxT[:, ft - 8, t * 128:(t + 1) * 128]
                    S.op('pe', lambda e: e.matmul(bk(b)[:, :], lhsT=lh, rhs=W[:, ft, :], start=(ft == 0), stop=(ft == 15)),
                         reads=[wkk, f'mxT{t}', 'convT0', 'convT1'], writes=[f'B{b}'])
                S.op('dve', lambda e: e.tensor_tensor(out=tmpc, in0=bk(b)[:, :], in1=bcx[0][:, ch * 512:(ch + 1) * 512], op=ALU.mult),
                     reads=[f'B{b}', 'bcx0'], writes=['tmpc'])
                S.op('pool', lambda e: e.tensor_tensor(out=x1[:, t, ch * 512:(ch + 1) * 512], in0=x1[:, t, ch * 512:(ch + 1) * 512], in1=tmpc,
                                                       op=ALU.add), reads=['tmpc', f'x1_{t}'], writes=[f'x1_{t}'])

        if stage == 'mixer':
            out_t = out.rearrange("(n p) f -> n p f", p=128)
            for t in range(NT_OWN):
                S.dma(f'd_out{t}', out_t[t], x1[:, t, :], reads=[f'x1_{t}'], writes=[f'out{t}'])
            S.finish('sp')
            return nc

        S.barrier()
        o_h2 = M + 65536
        h2tok = A.bf(o_h2, 8 * 2048).rearrange("p (t n) -> p t n", t=8)
        A.top = o_h2 + 32768
        o_t1 = A.alloc(8192); t1 = A.f32(o_t1, 2048)
        o_h2f = A.alloc(8192); h2f = A.f32(o_h2f, 2048)
        assert A.top <= O1 + 8192, (A.top, O1)
        A.top = o_mx
        o_ss = A.alloc(64); ssb = A.f32(o_ss, 16)
        o_h2T = A.alloc(8192); h2T = A.f32(o_h2T, 2048).rearrange("p (k t) -> p k t", t=128)
        o_wr = A.alloc(4096); wrt = A.f32(o_wr, 1024).rearrange("p (k n) -> p k n", n=64)
        o_br = A.alloc(256); brt = A.f32(o_br, 64)
        NV = NE + 2
        o_mask = A.alloc(8 * NV * 4); maskt = A.f32(o_mask, 8 * NV).rearrange("p (t n) -> p t n", t=8)
        o_G = A.alloc(8 * NV * 4); Gt = A.f32(o_G, 8 * NV).rearrange("p (t n) -> p t n", t=8)
        o_pos = A.alloc(8 * NV * 4); posm = A.f32(o_pos, 8 * NV).rearrange("p (t n) -> p t n", t=8)
        o_sc = A.alloc(1024); sct = A.f32(o_sc, 256)
        S.dma('d_wr', wrt, wr[:, :, :], writes=['wr'])
        S.dma('d_br', brt, br[:, :], writes=['br'])
        for t in range(NT_OWN):
            S.dma(f'd_shp{t}', posm[:, t, NE:NE + 2], shpos[:, t, :], writes=[f'posm{t}'])
        for t in range(NT_OWN):
            norm_mod(x1[:, t, :], f'x1_{t}', bcx[1], 'bcx1', bcx[2], 'bcx2', h2f, 'h2f')
            S.op('act', lambda e: e.copy(out=h2tok[:, t, :], in_=h2f), reads=['h2f'], writes=[f'h2tok{t}'])
            for g4 in range(4):
                for k4 in range(4):
                    kt = g4 * 4 + k4
                    S.op('pe', lambda e: e.transpose(out=bk(g4)[:, k4 * 128:(k4 + 1) * 128], in_=h2f[:, kt * 128:(kt + 1) * 128], identity=identf),
                         reads=['h2f', 'identf'], writes=[f'B{g4}'])
                S.op('act' if g4 % 2 == 0 else 'dve',
                     (lambda e: e.copy(out=h2T[:, g4 * 4:(g4 + 1) * 4, :], in_=bk(g4)[:, :].rearrange("p (k t) -> p k t", t=128))) if g4 % 2 == 0 else
                     (lambda e: e.tensor_copy(out=h2T[:, g4 * 4:(g4 + 1) * 4, :], in_=bk(g4)[:, :].rearrange("p (k t) -> p k t", t=128))),
                     reads=[f'B{g4}'], writes=['h2T'])
            for kt in range(16):
                S.op('pe', lambda e: e.matmul(bk(7)[:, 64:128], lhsT=h2T[:, kt, :], rhs=wrt[:, kt, :], start=(kt == 0), stop=(kt == 15)),
                     reads=['h2T', 'wr'], writes=['B7'])
            scr = sct[:, 0:64]; bia = sct[:, 64:128]; top8 = sct[:, 128:136]; den = sct[:, 136:137]
            S.op('act', lambda e: e.activation(out=scr, in_=bk(7)[:, 64:128], func=AF.Sigmoid), reads=['B7'], writes=['scr'])
            S.op('dve', lambda e: e.tensor_tensor(out=bia, in0=scr, in1=brt, op=ALU.add), reads=['scr', 'br'], writes=['bia'])
            S.op('dve', lambda e: e.max(out=top8, in_=bia), reads=['bia'], writes=['top8'])
            S.op('dve', lambda e: e.tensor_scalar(out=maskt[:, t, 0:NE], in0=bia, scalar1=top8[:, 5:6], scalar2=None, op0=ALU.is_ge),
                 reads=['bia', 'top8'], writes=[f'mask{t}'])
            S.op('dve', lambda e: e.tensor_tensor(out=scr, in0=scr, in1=maskt[:, t, 0:NE], op=ALU.mult), reads=['scr', f'mask{t}'], writes=['scr'])
            S.op('dve', lambda e: e.tensor_reduce(out=den, in_=scr, axis=AX.X, op=ALU.add), reads=['scr'], writes=['den'])
            S.op('dve', lambda e: e.reciprocal(out=den, in_=den), reads=['den'], writes=['den'])
            S.op('dve', lambda e: e.tensor_scalar(out=Gt[:, t, 0:NE], in0=scr, scalar1=den, scalar2=RSCALE, op0=ALU.mult, op1=ALU.mult),
                 reads=['scr', 'den'], writes=[f'G{t}'])
            S.op('pool', lambda e: e.memset(Gt[:, t, NE:NE + 2], 1.0), writes=[f'G{t}'])
        for t in range(NT_OWN):
            S.op('pe', lambda e: e.matmul(bk(6)[:, 0:NE], lhsT=Uf, rhs=maskt[:, t, 0:NE], start=True, stop=(t == 0)),
                 reads=['Uf', f'mask{t}'], writes=['B6'])
            for t2 in range(t):
                S.op('pe', lambda e: e.matmul(bk(6)[:, 0:NE], lhsT=onesf, rhs=maskt[:, t2, 0:NE], start=False, stop=(t2 == t - 1)),
                     reads=['onesf', f'mask{t2}'], writes=['B6'])
            S.op('dve', lambda e: e.tensor_tensor(out=posm[:, t, 0:NE], in0=bk(6)[:, 0:NE], in1=maskt[:, t, 0:NE], op=ALU.mult),
                 reads=['B6', f'mask{t}'], writes=[f'posm{t}'])
            S.op('dve', lambda e: e.tensor_scalar(out=posm[:, t, 0:NE], in0=posm[:, t, 0:NE], scalar1=-1.0, scalar2=None, op0=ALU.add),
                 reads=[f'posm{t}'], writes=[f'posm{t}'])

        S.barrier()
        MO = o_h2 + 32768
        A.top = MO
        o_gt2n = A.alloc(8192)
        o_Gn = A.alloc(8 * NV * 4); o_posn = A.alloc(8 * NV * 4)
        stg_f = A.f32(o_stage, 4096)
        S.op('dve', lambda e: e.tensor_copy(out=stg_f[:, 0:2048], in_=gt2bc), writes=['mv'])
        S.op('dve', lambda e: e.tensor_copy(out=stg_f[:, 2048:2048 + 8 * NV], in_=Gt.rearrange("p t n -> p (t n)")), writes=['mv'])
        S.op('dve', lambda e: e.tensor_copy(out=stg_f[:, 2048 + 8 * NV:2048 + 16 * NV], in_=posm.rearrange("p t n -> p (t n)")), writes=['mv'])
        S.barrier()
        gt2bc = A.f32(o_gt2n, 2048)
        Gt = A.f32(o_Gn, 8 * NV).rearrange("p (t n) -> p t n", t=8)
        posm = A.f32(o_posn, 8 * NV).rearrange("p (t n) -> p t n", t=8)
        S.op('dve', lambda e: e.tensor_copy(out=gt2bc, in_=stg_f[:, 0:2048]), writes=['gt2bc'])
        S.op('dve', lambda e: e.tensor_copy(out=Gt.rearrange("p t n -> p (t n)"), in_=stg_f[:, 2048:2048 + 8 * NV]), writes=['G'])
        S.op('dve', lambda e: e.tensor_copy(out=posm.rearrange("p t n -> p (t n)"), in_=stg_f[:, 2048 + 8 * NV:2048 + 16 * NV]), writes=['posm'])
        S.barrier()
        o_P = A.alloc(8 * CAP * 2); Pm = A.bf(o_P, 8 * CAP).rearrange("p (t c) -> p t c", t=8)
        o_R = A.alloc(16 * CAP * 2); xgT = A.bf(o_R, 16 * CAP).rearrange("p (k c) -> p k c", k=16)
        PT = A.bf(o_R, NST * 1024).rearrange("p (s t) -> p s t", s=NST)
        o_hb = A.alloc(NHT * CAP * 2); hbT = A.bf(o_hb, NHT * CAP).rearrange("p (h c) -> p h c", h=NHT)
        o_sg = A.alloc(CAP * 2); sgt = A.bf(o_sg, CAP)
        o_yb = [A.alloc(NST * 512 * 2) for _ in range(2)]
        ybb = [A.bf(o, NST * 512).rearrange("p (s n) -> p s n", s=NST) for o in o_yb]
        o_wgu = [A.alloc(4096) for _ in range(4)]
        Wgu = [A.bf(o, 2048).rearrange("p (k n) -> p k n", k=16) for o in o_wgu]
        o_wd = [A.alloc(NHT * 512 * 2) for _ in range(NWD)]
        Wdb = [A.bf(o, NHT * 512).rearrange("p (h n) -> p h n", h=NHT) for o in o_wd]
        print("MoE arena top", A.top, "of", ARENA_BYTES)
        stage_n[0] = 0

        def piece4(src, nelem):
            return stage_piece(src, nelem, nslots=4, slot_elems=1024)

        wgu_n = [0]; wd_n = [0]; yb_n = [0]
        cast_rr = [0]

        def cast(out_ap, in_ap, rk, wk):
            i = cast_rr[0] % 3
            cast_rr[0] += 1
            if i == 0:
                S.op('act', lambda e: e.copy(out=out_ap, in_=in_ap), reads=[rk], writes=[wk])
            elif i == 1:
                S.op('pool', lambda e: e.tensor_copy(out=out_ap, in_=in_ap), reads=[rk], writes=[wk])
            else:
                S.op('dve', lambda e: e.tensor_copy(out=out_ap, in_=in_ap), reads=[rk], writes=[wk])

        order = [NE, NE + 1] + list(range(NE))
        for e_ in order:
            if e_ < NE:
                sg_, su_, sd_ = weg[e_], weu[e_], wed[e_]
            else:
                sg_, su_, sd_ = wsg, wsu, wsd
            for tt in range(8):
                S.op('dve' if tt % 2 == 0 else 'pool',
                     lambda e: e.tensor_scalar(out=Pm[:, tt, :], in0=iota[:, 0:CAP], scalar1=posm[:, tt, e_:e_ + 1], scalar2=None, op0=ALU.is_equal),
                     reads=['iota', 'posm'], writes=[f'P{tt}'])
            for kt in range(16):
                b = kt % 2
                for tt in range(8):
                    S.op('pe', lambda e: e.matmul(bk(b)[:, 0:CAP], lhsT=h2tok[:, tt, kt * 128:(kt + 1) * 128], rhs=Pm[:, tt, :],
                                                  start=(tt == 0), stop=(tt == 7)), reads=[f'h2tok{tt}', f'P{tt}'], writes=[f'B{b}'])
                if kt % 2 == 0:
                    S.op('act', lambda e: e.copy(out=xgT[:, kt, :], in_=bk(b)[:, 0:CAP]), reads=[f'B{b}'], writes=[f'R{kt}'])
                else:
                    S.op('dve', lambda e: e.tensor_copy(out=xgT[:, kt, :], in_=bk(b)[:, 0:CAP]), reads=[f'B{b}'], writes=[f'R{kt}'])
            for ht in range(NHT):
                ws = []
                for which, src in ((0, sg_), (1, su_)):
                    i = wgu_n[0] % 4
                    wgu_n[0] += 1
                    Wt = Wgu[i]
                    flat = src[ht].rearrange("p k n -> p (k n)")
                    for hf_ in range(2):
                        pc, pk = piece4(flat[:, hf_ * 1024:(hf_ + 1) * 1024], 1024)
                        cast(Wt[:, hf_ * 8:(hf_ + 1) * 8, :].rearrange("p k n -> p (k n)"), pc, pk, f'Wgu{i}')
                    ws.append((Wt, f'Wgu{i}'))
                par = ht % 2
                bg, bu = 2 + 2 * par, 3 + 2 * par
                for (Wt, wkk), b in zip(ws, (bg, bu)):
                    for kt in range(16):
                        S.op('pe', lambda e: e.matmul(bk(b)[:, 0:CAP], lhsT=Wt[:, kt, :], rhs=xgT[:, kt, :], start=(kt == 0), stop=(kt == 15)),
                             reads=[wkk, f'R{kt}'], writes=[f'B{b}'])
                S.op('act', lambda e: e.activation(out=sgt, in_=bk(bg)[:, 0:CAP], func=AF.Silu), reads=[f'B{bg}'], writes=['sgt'])
                S.op('dve', lambda e: e.tensor_tensor(out=hbT[:, ht, :], in0=sgt, in1=bk(bu)[:, 0:CAP], op=ALU.mult),
                     reads=['sgt', f'B{bu}'], writes=[f'hb{ht}'])
            for stt in range(NST):
                pb = bkbf(1)
                for tt in range(8):
                    S.op('pe', lambda e: e.transpose(out=pb[:, tt * 128:(tt + 1) * 128], in_=Pm[:, tt, stt * 128:(stt + 1) * 128], identity=identb),
                         reads=[f'P{tt}', 'identb'], writes=['B1'])
                S.op('act', lambda e: e.copy(out=PT[:, stt, :], in_=pb), reads=['B1'], writes=[f'R{2 * stt}', f'R{2 * stt + 1}'])
            for c8 in range(4):
                i = wd_n[0] % NWD
                wd_n[0] += 1
                Wd = Wdb[i]
                for (h0, h1) in ((0, 2), (2, 4), (4, 6), (6, 8), (8, 10), (10, 11)):
                    nh = h1 - h0
                    pc, pk = piece4(sd_[c8][:, h0:h1, :].rearrange("p h n -> p (h n)"), nh * 512)
                    for hh in range(nh):
                        eng = ('pool', 'dve')[(hh + h0) % 2]
                        S.op(eng, lambda e: e.tensor_tensor(out=Wd[:, h0 + hh, :], in0=pc[:, hh * 512:(hh + 1) * 512],
                                                            in1=gt2bc[:, c8 * 512:(c8 + 1) * 512], op=ALU.mult),
                             reads=[pk, 'gt2bc'], writes=[f'Wd{i}'])
                j = yb_n[0] % 2
                yb_n[0] += 1
                yb = ybb[j]
                for stt in range(NST):
                    db = 6 if stt % 2 == 0 else 2
                    osl = bk(db)[:, 0:512]
                    for ht in range(NHT):
                        S.op('pe', lambda e: e.matmul(osl, lhsT=hbT[:, ht, stt * 128:(stt + 1) * 128], rhs=Wd[:, ht, :],
                                                      start=(ht == 0), stop=(ht == NHT - 1)), reads=[f'hb{ht}', f'Wd{i}'], writes=[f'B{db}'])
                    S.op('act', lambda e: e.copy(out=yb[:, stt, :], in_=osl), reads=[f'B{db}'], writes=[f'yb{j}'])
                for tt in range(8):
                    cb = 7 if tt % 2 == 0 else 3
                    osl = bk(cb)[:, 0:512]
                    for stt in range(NST):
                        S.op('pe', lambda e: e.matmul(osl, lhsT=PT[:, stt, tt * 128:(tt + 1) * 128], rhs=yb[:, stt, :],
                                                      start=(stt == 0), stop=(stt == NST - 1)),
                             reads=[f'R{2 * stt}', f'R{2 * stt + 1}', f'yb{j}'], writes=[f'B{cb}'])
                    S.op('dve' if tt % 2 == 0 else 'pool' if False else 'dve',
                         lambda e: e.scalar_tensor_tensor(out=x1[:, tt, c8 * 512:(c8 + 1) * 512], in0=osl, scalar=Gt[:, tt, e_:e_ + 1],
                                                          in1=x1[:, tt, c8 * 512:(c8 + 1) * 512], op0=ALU.mult, op1=ALU.add),
                         reads=[f'B{cb}', 'G', f'x1_{tt}'], writes=[f'x1_{tt}'])

        S.barrier()
        A.top = MO
        o_fg = A.alloc(8192); fgt = A.f32(o_fg, 2048)
        o_z = A.alloc(8192); zt = A.f32(o_z, 2048)
        o_ot = [A.alloc(8192) for _ in range(2)]
        ot = [A.f32(o, 2048) for o in o_ot]
        o_ss2 = A.alloc(64); ss2 = A.f32(o_ss2, 16)
        S.dma('d_fg', fgt, fg[:, :], writes=['fg'])
        out_t = out.rearrange("(n p) f -> n p f", p=128)
        for t in range(NT_OWN):
            ss = ss2[:, t:t + 1]
            S.op('act', lambda e: e.activation(out=zt, in_=x1[:, t, :], func=AF.Square, accum_out=ss), reads=[f'x1_{t}'], writes=['zt', f'fss{t}'])
            S.op('dve', lambda e: e.tensor_scalar(out=ss, in0=ss, scalar1=1.0 / D, scalar2=EPS, op0=ALU.mult, op1=ALU.add), reads=[f'fss{t}'], writes=[f'fss{t}'])
            S.op('act', lambda e: e.activation(out=ss, in_=ss, func=AF.Ln), reads=[f'fss{t}'], writes=[f'fss{t}'])
            S.op('act', lambda e: e.activation(out=ss, in_=ss, func=AF.Exp, scale=-0.5), reads=[f'fss{t}'], writes=[f'fss{t}'])
            S.op('dve', lambda e: e.scalar_tensor_tensor(out=ot[t % 2], in0=x1[:, t, :], scalar=ss, in1=fgt, op0=ALU.mult, op1=ALU.mult),
                 reads=[f'x1_{t}', f'fss{t}', 'fg'], writes=[f'ot{t % 2}'])
            S.dma(f'd_out{t % 2}', out_t[t], ot[t % 2], reads=[f'ot{t % 2}'], writes=[f'out{t}'])
        S.finish('sp')
    return nc


def _layouts(inp, ne=NE):
    f = np.float32
    c = lambda a: np.ascontiguousarray(a, dtype=f)
    w_in = inp['w_in'][0]
    sh = {}
    sh['wada'] = c(inp['w_ada'][0].reshape(16, 128, 6, D).transpose(2, 0, 1, 3))
    sh['bada2'] = c(np.stack([inp['b_ada'][0], inp['b_ada'][0]]))
    sh['g1_2'] = c(np.stack([inp['norm1_g'][0]] * 2)); sh['g2_2'] = c(np.stack([inp['norm2_g'][0]] * 2))
    kvg = np.concatenate([w_in[:, 3584:4096], w_in[:, 4096:5120], w_in[:, 6144:6160]], axis=1)
    sh['wkvg'] = c(kvg.reshape(16, 128, 1552))
    chunks = [w_in[:, 3072:3584], w_in[:, 3584:4096], w_in[:, 4096:4608], w_in[:, 4608:5120], w_in[:, 5120:5632], w_in[:, 5632:6144]]
    sh['wtok'] = c(np.stack([ch.reshape(4, 4, 128, 512).transpose(0, 2, 1, 3) for ch in chunks]))
    sh['wg16'] = c(w_in[:, 6144:6160].reshape(16, 128, 16).transpose(1, 0, 2))
    wc = np.stack([np.stack([w_in[:, part * 1024 + ct * 128: part * 1024 + (ct + 1) * 128].reshape(16, 128, 128).transpose(1, 0, 2)
                             for part in range(3)]) for ct in range(8)])
    sh['wconv'] = c(wc)
    sh['cw'] = c(inp['conv_w'][0].reshape(3, 8, 128).transpose(2, 1, 0))
    sh['gb'] = c(np.broadcast_to(inp['gate_b'][0].reshape(1, 16), (128, 16)))
    sh['hg'] = c(np.broadcast_to(inp['head_g'][0].reshape(1, 1024), (128, 1024)))
    wo = inp['w_out'][0]
    sh['wout'] = c(np.stack([wo[:, ch * 512:(ch + 1) * 512].reshape(4, 4, 128, 512).transpose(0, 2, 1, 3) for ch in range(4)]))
    sh['wr'] = c(inp['w_router'][0].reshape(16, 128, 64).transpose(1, 0, 2))
    sh['br'] = c(np.broadcast_to(inp['b_router'][0].reshape(1, 64), (128, 64)))
    sh['weg'] = c(inp['we_gate'][0][:ne].reshape(ne, 16, 128, NHT, 128).transpose(0, 3, 2, 1, 4))
    sh['weu'] = c(inp['we_up'][0][:ne].reshape(ne, 16, 128, NHT, 128).transpose(0, 3, 2, 1, 4))
    sh['wed'] = c(inp['we_down'][0][:ne].reshape(ne, NHT, 128, 4, 512).transpose(0, 3, 2, 1, 4))
    sh['wsg'] = c(inp['ws_gate'][0].reshape(16, 128, NHT, 128).transpose(2, 1, 0, 3))
    sh['wsu'] = c(inp['ws_up'][0].reshape(16, 128, NHT, 128).transpose(2, 1, 0, 3))
    sh['wsd'] = c(inp['ws_down'][0].reshape(NHT, 128, 4, 512).transpose(2, 1, 0, 3))
    sh['fg'] = c(np.broadcast_to(inp['final_g'].reshape(1, D), (128, D)))
    sp = np.full((128, 8, 2), -1.0, f)
    for t in range(8):
        sp[:, t, 0 if t < 4 else 1] = (t % 4) * 128 + np.arange(128)
    sh['shpos'] = sp
    per = []
    for core in range(8):
        b, seg = core // 4, core % 4
        m = dict(sh)
        m['xb'] = c(inp['x'][b]); m['xo'] = c(inp['x'][b, seg * 1024:(seg + 1) * 1024]); m['cx'] = c(inp['ctx'][b])
        m['cT'] = c(np.stack([inp['c'][b].reshape(16, 128).T, inp['c_ctx'].reshape(16, 128).T], axis=-1))
        fl = np.zeros((128, 68), f)
        fl[:, seg * 8] = 1.0
        fl[:, 33 + (24 - seg * 8)] = 1.0
        m['flags'] = fl
        per.append(m)
    return per


_NC_CACHE = {}


def kernel(**inputs):
    inp = {k: np.asarray(v) for k, v in inputs.items()}
    if 'full' not in _NC_CACHE:
        _NC_CACHE['full'] = build_nc('full')
    nc = _NC_CACHE['full']
    in_maps = _layouts(inp)
    res = run_bass_kernel_spmd(nc, in_maps, core_ids=list(range(8)))
    outs = [np.asarray(r["out"], dtype=np.float32) for r in res.results]
    full = np.zeros((2, 4096, D), np.float32)
    for core in range(8):
        b, seg = core // 4, core % 4
        full[b, seg * 1024:(seg + 1) * 1024] = outs[core]
    return full
```

```python
import numpy as np
from contextlib import ExitStack
import concourse.bass as bass
import concourse.mybir as mybir
from concourse.bass_utils import run_bass_kernel_spmd

F32 = mybir.dt.float32
BF16 = mybir.dt.bfloat16
I32 = mybir.dt.int32
AF = mybir.ActivationFunctionType
ALU = mybir.AluOpType
AX = mybir.AxisListType

D = 2048
NT_OWN = 8
NT_B = 32
NE = 64
CAP = 512
NST = CAP // 128
EPS = 1e-6
QSCALE = 128 ** -0.5
RSCALE = 2.446
DH = 1408
NHT = 11
NWD = 1
ARENA_BYTES = 210432


class Sched:
    def __init__(self, nc, stack):
        self.nc = nc
        self.stack = stack
        self.e = {'pe': nc.tensor, 'act': nc.scalar, 'dve': nc.vector, 'pool': nc.gpsimd, 'sp': nc.sync}
        self.sem = {k: stack.enter_context(nc.semaphore(f"s_{k}")) for k in self.e}
        self.cnt = {k: 0 for k in self.e}
        self.waited = {}
        self.st = {}
        self.dsems = {}
        self.dsem_by_name = {}
        self.epoch = 1

    def _state(self, k):
        s = self.st.get(k)
        if s is None:
            s = self.st[k] = {'w': None, 'r': []}
        return s

    def _deps(self, reads, writes):
        deps = []
        for k in reads:
            s = self._state(k)
            if s['w'] is not None:
                deps.append(s['w'])
        for k in writes:
            s = self._state(k)
            if s['w'] is not None:
                deps.append(s['w'])
            deps.extend(s['r'])
        return deps

    def _emit_waits(self, eng, deps, is_dma):
        need = {}
        for t in deps:
            kind, who, val = t
            if kind == 'e' and who == eng and not is_dma and eng == 'pe':
                continue
            key = (kind, who if kind == 'e' else id(who))
            if key not in need or need[key][1] < val:
                need[key] = (who, val, kind)
        for key, (who, val, kind) in need.items():
            wk = (eng, key)
            if self.waited.get(wk, 0) >= val:
                continue
            self.waited[wk] = val
            sem = self.sem[who] if kind == 'e' else who
            self.e[eng].wait_ge(sem, val)

    def _update(self, tok, reads, writes):
        for k in reads:
            s = self._state(k)
            r = s['r']
            r[:] = [x for x in r if not (x[0] == tok[0] and (x[1] is tok[1] or x[1] == tok[1]))]
            r.append(tok)
        for k in writes:
            s = self._state(k)
            s['w'] = tok
            s['r'] = []

    def op(self, eng, fn, reads=(), writes=(), signal=True):
        deps = self._deps(reads, writes)
        self._emit_waits(eng, deps, False)
        inst = fn(self.e[eng])
        if signal:
            inst.then_inc(self.sem[eng], 1)
            self.cnt[eng] += 1
            self._update(('e', eng, self.cnt[eng]), reads, writes)
        else:
            assert eng == 'pe'
            self._update(('e', eng, self.cnt[eng] + 1), reads, writes)

    def dsem(self, name):
        s = self.dsem_by_name.get(name)
        if s is None:
            s = self.stack.enter_context(self.nc.semaphore(name))
            self.dsem_by_name[name] = s
            self.dsems[id(s)] = [s, 0]
        return s

    def dma(self, semname, out, in_, reads=(), writes=(), eng='sp'):
        dsem = self.dsem(semname)
        deps = self._deps(reads, writes)
        self._emit_waits(eng, deps, True)
        self.e[eng].dma_start(out=out, in_=in_).then_inc(dsem, 16)
        rec = self.dsems[id(dsem)]
        rec[1] += 16
        self._update(('d', dsem, rec[1]), reads, writes)

    def barrier(self, new_epoch=False):
        for eng in self.e:
            for other in self.e:
                if (other != eng or eng in ('act', 'dve', 'pool')) and self.cnt[other] > 0:
                    wk = (eng, ('e', other))
                    if self.waited.get(wk, 0) < self.cnt[other]:
                        self.waited[wk] = self.cnt[other]
                        self.e[eng].wait_ge(self.sem[other], self.cnt[other])
            for sid, (s, v) in self.dsems.items():
                if v > 0:
                    wk = (eng, ('d', sid))
                    if self.waited.get(wk, 0) < v:
                        self.waited[wk] = v
                        self.e[eng].wait_ge(s, v)
        self.st = {}
        if new_epoch:
            for k in self.e:
                self.sem[k] = self.stack.enter_context(self.nc.semaphore(f"s_{k}_{self.epoch}"))
                self.cnt[k] = 0
            self.epoch += 1
            self.waited = {wk: v for wk, v in self.waited.items() if wk[1][0] != 'e'}

    def finish(self, eng='sp'):
        deps = []
        for k, s in self.st.items():
            if s['w'] is not None:
                deps.append(s['w'])
            deps.extend(s['r'])
        self._emit_waits(eng, deps, True)


class Arena:
    def __init__(self, ap):
        self.ap = ap
        self.top = 0

    def alloc(self, nbytes):
        off = (self.top + 31) // 32 * 32
        self.top = off + nbytes
        assert self.top <= ARENA_BYTES, f"arena overflow {self.top}"
        return off

    def bf(self, off, n, parts=128):
        return self.ap[0:parts, off // 2: off // 2 + n]

    def f32(self, off, n, parts=128):
        return self.ap[0:parts, off // 2: off // 2 + 2 * n].bitcast(F32)

    def i32(self, off, n, parts=128):
        return self.ap[0:parts, off // 2: off // 2 + 2 * n].bitcast(I32)


def build_nc(stage='full'):
    nc = bass.Bass("TRN2", target_bir_lowering=False)

    def din(name, shape):
        return nc.dram_tensor(name, list(shape), F32, kind="ExternalInput").ap()

    xb = din("xb", [4096, D]); xo = din("xo", [1024, D]); cx = din("cx", [256, D])
    cT = din("cT", [128, 16, 2])
    wada = din("wada", [6, 16, 128, D]); bada2 = din("bada2", [2, 6 * D])
    g1_2 = din("g1_2", [2, D]); g2_2 = din("g2_2", [2, D])
    wkvg = din("wkvg", [16, 128, 1552])
    wtok = din("wtok", [6, 4, 128, 4, 512]); wg16 = din("wg16", [128, 16, 16])
    wconv = din("wconv", [8, 3, 128, 16, 128]); cw = din("cw", [128, 8, 3])
    gb = din("gb", [128, 16]); hg = din("hg", [128, 1024])
    wout = din("wout", [4, 4, 128, 4, 512])
    wr = din("wr", [128, 16, 64]); br = din("br", [128, 64])
    NEd = NE if stage == 'full' else 1
    STAGE = stage
    weg = din("weg", [NEd, NHT, 128, 16, 128]); weu = din("weu", [NEd, NHT, 128, 16, 128])
    wed = din("wed", [NEd, 4, 128, NHT, 512])
    wsg = din("wsg", [NHT, 128, 16, 128]); wsu = din("wsu", [NHT, 128, 16, 128]); wsd = din("wsd", [4, 128, NHT, 512])
    fg = din("fg", [128, D]); flags = din("flags", [128, 68]); shpos = din("shpos", [128, 8, 2])
    out = nc.dram_tensor("out", [1024, D], F32, kind="ExternalOutput").ap()

    with ExitStack() as st:
        S = Sched(nc, st)
        arena_t = st.enter_context(nc.sbuf_tensor("arena", [128, ARENA_BYTES // 2], BF16))
        A = Arena(arena_t)
        banks = [st.enter_context(nc.psum_tensor(f"bank{i}", [128, 512], F32)) for i in range(8)]

        def bk(i):
            return banks[i]

        def bkbf(i):
            return banks[i][:, :].bitcast(BF16)

        o_identb = A.alloc(256); identb = A.bf(o_identb, 128)
        o_iota = A.alloc(2048); iota = A.f32(o_iota, 512)
        o_stage = A.alloc(16384)
        PERSIST = A.top
        o_identf = A.alloc(512); identf = A.f32(o_identf, 128)
        o_Ub = A.alloc(256); Ub = A.bf(o_Ub, 128)
        o_Lb = A.alloc(256); Lb = A.bf(o_Lb, 128)
        o_Uf = A.alloc(512); Uf = A.f32(o_Uf, 128)
        o_Lf = A.alloc(512); Lf = A.f32(o_Lf, 128)
        o_onesf = A.alloc(512); onesf = A.f32(o_onesf, 128)
        o_onesb = A.alloc(256); onesb = A.bf(o_onesb, 128)
        o_sel0 = A.alloc(512); sel0 = A.f32(o_sel0, 128)
        o_sel1 = A.alloc(512); sel1 = A.f32(o_sel1, 128)
        o_flags = A.alloc(68 * 4); flg = A.f32(o_flags, 68)
        o_gb = A.alloc(64); gbt = A.f32(o_gb, 16)
        o_tmpi = A.alloc(2048); tmpi = A.i32(o_tmpi, 512)
        o_gs = A.alloc(16 * 4 * 8); gsm = A.f32(o_gs, 128)
        MIXBASE = A.top
        M = MIXBASE

        def tri(ap_, pattern, chm, cmp, key):
            S.op('pool', lambda e: e.memset(ap_, 1.0), writes=[key])
            S.op('pool', lambda e: e.affine_select(out=ap_, in_=ap_, pattern=pattern, compare_op=cmp, fill=0.0,
                                                   base=0, channel_multiplier=chm), reads=[key], writes=[key])

        tri(identb, [[-1, 128]], 1, ALU.is_equal, 'identb')
        tri(identf, [[-1, 128]], 1, ALU.is_equal, 'identf')
        tri(Ub, [[1, 128]], -1, ALU.is_ge, 'Ub')
        tri(Uf, [[1, 128]], -1, ALU.is_ge, 'Uf')
        tri(Lb, [[-1, 128]], 1, ALU.is_ge, 'Lb')
        tri(Lf, [[-1, 128]], 1, ALU.is_ge, 'Lf')
        S.op('pool', lambda e: e.memset(onesf, 1.0), writes=['onesf'])
        S.op('pool', lambda e: e.memset(onesb, 1.0), writes=['onesb'])
        S.op('pool', lambda e: e.memset(sel0, 1.0), writes=['sel0'])
        S.op('pool', lambda e: e.affine_select(out=sel0, in_=sel0, pattern=[[0, 128]], compare_op=ALU.is_equal, fill=0.0,
                                               base=0, channel_multiplier=1), reads=['sel0'], writes=['sel0'])
        S.op('pool', lambda e: e.memset(sel1, 1.0), writes=['sel1'])
        S.op('pool', lambda e: e.affine_select(out=sel1, in_=sel1, pattern=[[0, 128]], compare_op=ALU.is_equal, fill=0.0,
                                               base=-1, channel_multiplier=1), reads=['sel1'], writes=['sel1'])
        S.op('pool', lambda e: e.iota(tmpi, pattern=[[1, 512]], base=0, channel_multiplier=0), writes=['tmpi'])
        S.op('pool', lambda e: e.tensor_copy(out=iota, in_=tmpi), reads=['tmpi'], writes=['iota'])
        S.dma('d_flags', flg, flags[:, :], writes=['flags'])
        S.dma('d_gb', gbt, gb[:, :], writes=['gb'])

        stage_n = [0]

        def stage_piece(src_ap, nelem, nslots=2, slot_elems=2048):
            i = stage_n[0] % nslots
            stage_n[0] += 1
            key = f"stg{nslots}_{i}"
            ap_ = A.f32(o_stage + i * slot_elems * 4, nelem)
            S.dma(f"d_{key}", ap_, src_ap, writes=[key])
            return ap_, key

        o_row = None

        def mod_vec(j, s2, rowbuf, key):
            for kt in range(16):
                pc, pk = stage_piece(wada[j, kt], 2048)
                for c4 in range(4):
                    S.op('pe', lambda e: e.matmul(bk(c4)[0:2, :], lhsT=s2[:, kt, :], rhs=pc[:, c4 * 512:(c4 + 1) * 512],
                                                  start=(kt == 0), stop=(kt == 15)),
                         reads=[pk, 's2'], writes=[f'B{c4}'])
            for c4 in range(4):
                S.op('dve', lambda e: e.tensor_copy(out=rowbuf[0:2, c4 * 512:(c4 + 1) * 512], in_=bk(c4)[0:2, :]),
                     reads=[f'B{c4}'], writes=[key])

        def bcast(dst, dkey, row, rkey, sel, skey):
            for c4 in range(4):
                S.op('pe', lambda e: e.matmul(bk(4 + c4)[:, :], lhsT=sel[0:2, :], rhs=row[0:2, c4 * 512:(c4 + 1) * 512],
                                              start=True, stop=True), reads=[rkey, skey], writes=[f'B{4 + c4}'])
                S.op('act', lambda e: e.copy(out=dst[:, c4 * 512:(c4 + 1) * 512], in_=bk(4 + c4)[:, :]),
                     reads=[f'B{4 + c4}'], writes=[dkey])

        A.top = M
        o_bc = [A.alloc(8192) for _ in range(4)]
        bc = [A.f32(o, 2048) for o in o_bc]
        tmpl = {}

        def ada_temps(base, gsrc):
            A.top = base
            tmpl['rows'] = [A.f32(A.alloc(8192), 2048) for _ in range(2)]
            tmpl['b2'] = A.f32(A.alloc(8192), 2048)
            tmpl['g12'] = A.f32(A.alloc(8192), 2048)
            tmpl['s2'] = A.f32(A.alloc(128), 32).rearrange("p (k r) -> p k r", r=2)
            S.dma('d_s2', tmpl['s2'], cT[:, :, :], writes=['s2'])
            S.dma('d_g12', tmpl['g12'][0:2, :], gsrc[:, :], writes=['g12'])
            S.op('act', lambda e: e.activation(out=tmpl['s2'], in_=tmpl['s2'], func=AF.Silu), reads=['s2'], writes=['s2'])

        ada_temps(M + 32768, g1_2)
        rows = tmpl['rows']; g12 = tmpl['g12']

        def mod_row(j, rowbuf, key):
            b2 = tmpl['b2']
            S.dma('d_b2', b2[0:2, :], bada2[:, j * D:(j + 1) * D], reads=[], writes=['b2'])
            mod_vec(j, tmpl['s2'], rowbuf, key)
            S.op('dve', lambda e: e.tensor_tensor(out=rowbuf[0:2, :], in0=rowbuf[0:2, :], in1=b2[0:2, :], op=ALU.add),
                 reads=[key, 'b2'], writes=[key])

        mod_row(0, rows[0], 'row0')
        mod_row(1, rows[1], 'row1')
        S.op('dve', lambda e: e.scalar_tensor_tensor(out=rows[1][0:2, :], in0=rows[1][0:2, :], scalar=1.0, in1=g12[0:2, :],
                                                     op0=ALU.add, op1=ALU.mult), reads=['row1', 'g12'], writes=['row1'])
        bcast(bc[0], 'bc0', rows[1], 'row1', sel0, 'sel0')
        bcast(bc[1], 'bc1', rows[0], 'row0', sel0, 'sel0')
        bcast(bc[2], 'bc2', rows[1], 'row1', sel1, 'sel1')
        bcast(bc[3], 'bc3', rows[0], 'row0', sel1, 'sel1')

        if stage == 'ada':
            out_t = out.rearrange("(n p) f -> n p f", p=128)
            for t in range(4):
                S.dma(f'd_out{t}', out_t[t], bc[t], reads=[f'bc{t}'], writes=[f'out{t}'])
            S.finish('sp')
            return nc
        S.barrier()
        A.top = M + 32768
        SCANBASE = A.top
        o_t1 = A.alloc(8192); t1 = A.f32(o_t1, 2048)
        o_ss = A.alloc(64); ssb = A.f32(o_ss, 16)
        o_xn = [A.alloc(4096) for _ in range(2)]
        xnb = [A.bf(o, 2048) for o in o_xn]
        nm_n = [0]

        def norm_mod(xp, xkey, gm, gmkey, sh, shkey, dst, dkey):
            i = nm_n[0] % 8
            nm_n[0] += 1
            ss = ssb[:, i:i + 1]
            sk = f'ss{i}'
            S.op('act', lambda e: e.activation(out=t1, in_=xp, func=AF.Square, accum_out=ss), reads=[xkey], writes=['t1', sk])
            S.op('dve', lambda e: e.tensor_scalar(out=ss, in0=ss, scalar1=1.0 / D, scalar2=EPS, op0=ALU.mult, op1=ALU.add),
                 reads=[sk], writes=[sk])
            S.op('act', lambda e: e.activation(out=ss, in_=ss, func=AF.Ln), reads=[sk], writes=[sk])
            S.op('act', lambda e: e.activation(out=ss, in_=ss, func=AF.Exp, scale=-0.5), reads=[sk], writes=[sk])
            S.op('dve', lambda e: e.scalar_tensor_tensor(out=t1, in0=xp, scalar=ss, in1=gm, op0=ALU.mult, op1=ALU.mult),
                 reads=[xkey, sk, gmkey], writes=['t1'])
            S.op('pool', lambda e: e.tensor_tensor(out=dst, in0=t1, in1=sh, op=ALU.add), reads=['t1', shkey], writes=[dkey])

        def transpose16(src, skey, dst3, dkeys, b0, b1, evac='act'):
            for half, b in ((0, b0), (1, b1)):
                pb = bkbf(b)
                for k8 in range(8):
                    kt = half * 8 + k8
                    S.op('pe', lambda e: e.transpose(out=pb[:, k8 * 128:(k8 + 1) * 128], in_=src[:, kt * 128:(kt + 1) * 128],
                                                     identity=identb), reads=[skey, 'identb'], writes=[f'B{b}'])
                dk = dkeys if isinstance(dkeys, list) else [dkeys]
                wk = dk[half * 8:(half + 1) * 8] if len(dk) == 16 else dk
                eng = evac if half == 0 else ('dve' if evac == 'act' else 'act')
                if eng == 'act':
                    S.op('act', lambda e: e.copy(out=dst3[:, half * 8:(half + 1) * 8, :],
                                                 in_=pb.rearrange("p (k t) -> p k t", t=128)), reads=[f'B{b}'], writes=wk)
                else:
                    S.op('dve', lambda e: e.tensor_copy(out=dst3[:, half * 8:(half + 1) * 8, :],
                                                        in_=pb.rearrange("p (k t) -> p k t", t=128)), reads=[f'B{b}'], writes=wk)

        o_wk = A.alloc(16 * 512 * 2); Wk = A.bf(o_wk, 16 * 512).rearrange("p (k n) -> p k n", n=512)
        o_wv = A.alloc(16 * 1024 * 2); Wv = A.bf(o_wv, 16 * 1024).rearrange("p (k n) -> p k n", n=1024)
        o_wg = A.alloc(16 * 16 * 2); Wg16 = A.bf(o_wg, 256).rearrange("p (k n) -> p k n", n=16)
        for kt in range(16):
            pc, pk = stage_piece(wkvg[kt], 1552)
            S.op('act', lambda e: e.copy(out=Wk[:, kt, :], in_=pc[:, 0:512]), reads=[pk], writes=['Wk'])
            S.op('dve', lambda e: e.tensor_copy(out=Wv[:, kt, :], in_=pc[:, 512:1536]), reads=[pk], writes=['Wv'])
            S.op('pool', lambda e: e.tensor_copy(out=Wg16[:, kt, :], in_=pc[:, 1536:1552]), reads=[pk], writes=['Wg16'])
        o_xnT = [A.alloc(4096) for _ in range(2)]
        xnT = [A.bf(o, 2048).rearrange("p (k t) -> p k t", t=128) for o in o_xnT]
        o_CT = A.alloc(2 * 4 * 256 * 4); CT = A.f32(o_CT, 2048).rearrange("p (d h v) -> p d h v", d=2, h=4)
        o_nst = A.alloc(32); nst = A.f32(o_nst, 8).rearrange("p (d h) -> p d h", d=2)
        o_CS = A.alloc(2 * 4 * 256 * 4); CSv = A.f32(o_CS, 2048).rearrange("p (d h v) -> p d h v", d=2, h=4)
        o_ns = A.alloc(32); nsv = A.f32(o_ns, 8).rearrange("p (d h) -> p d h", d=2)
        o_ks = [A.alloc(1024) for _ in range(2)]
        ksb = [A.bf(o, 512).rearrange("p (h d) -> p h d", h=4) for o in o_ks]
        o_v1 = [A.alloc(2048) for _ in range(2)]
        v1b = [A.bf(o, 1024).rearrange("p (h v) -> p h v", h=4) for o in o_v1]
        for z, zk in ((CT, 'CT'), (CSv, 'CS')):
            S.op('pool', lambda e: e.memset(z.rearrange("p d h v -> p (d h v)"), 0.0), writes=[zk + '0', zk + '1'])
        S.op('pool', lambda e: e.memset(nst.rearrange("p d h -> p (d h)"), 0.0), writes=['n0', 'n1'])
        S.op('pool', lambda e: e.memset(nsv.rearrange("p d h -> p (d h)"), 0.0), writes=['ns0', 'ns1'])

        gs_n = [0]

        def gates_dir(gpre, gkey, d):
            i = gs_n[0] % 2
            gs_n[0] += 1
            base = i * 64
            sp = gsm[:, base + 0:base + 4]; wq = gsm[:, base + 4:base + 8]; wk_ = gsm[:, base + 8:base + 12]
            aa = gsm[:, base + 12:base + 16]; tmp = gsm[:, base + 16:base + 20]
            k_ = f'gs{i}'
            ipre = gpre[:, d * 8 + 0:d * 8 + 4]
            fpre = gpre[:, d * 8 + 4:d * 8 + 8]
            S.op('act', lambda e: e.activation(out=sp, in_=fpre, func=AF.Exp, scale=-1.0), reads=[gkey], writes=[k_])
            S.op('dve', lambda e: e.tensor_scalar(out=sp, in0=sp, scalar1=1.0, scalar2=None, op0=ALU.add), reads=[k_], writes=[k_])
            S.op('act', lambda e: e.activation(out=sp, in_=sp, func=AF.Ln), reads=[k_], writes=[k_])
            tri_ = Uf if d == 0 else Lf
            S.op('pe', lambda e: e.matmul(bk(7)[:, 0:4], lhsT=tri_, rhs=sp, start=True, stop=True), reads=[k_, 'Uf', 'Lf'], writes=['B7'])
            S.op('pe', lambda e: e.matmul(bk(7)[:, 4:8], lhsT=onesf, rhs=sp, start=True, stop=True), reads=[k_, 'onesf'], writes=['B7'])
            S.op('act', lambda e: e.activation(out=wq, in_=bk(7)[:, 0:4], func=AF.Exp, scale=-1.0), reads=['B7'], writes=[k_])
            S.op('dve', lambda e: e.tensor_tensor(out=tmp, in0=bk(7)[:, 0:4], in1=ipre, op=ALU.add), reads=['B7', gkey], writes=[k_])
            S.op('act', lambda e: e.activation(out=aa, in_=bk(7)[:, 4:8], func=AF.Exp, scale=-1.0), reads=['B7'], writes=[k_])
            S.op('act', lambda e: e.activation(out=wk_, in_=tmp, func=AF.Exp), reads=[k_], writes=[k_])
            return wq, wk_, aa, k_

        def state_update(d, ks, kskey, v1, v1key, aa, akey, flag_idx):
            for h in range(4):
                b = 3 + h // 2
                S.op('pe', lambda e: e.matmul(bk(b)[:, (h % 2) * 256:(h % 2) * 256 + 256], lhsT=ks[:, h, :], rhs=v1[:, h, :],
                                              start=True, stop=True), reads=[kskey, v1key], writes=[f'B{b}'])
            for h in range(4):
                S.op('pe', lambda e: e.matmul(bk(7)[:, 8 + h:9 + h], lhsT=ks[:, h, :], rhs=onesb[:, 0:1], start=True, stop=True),
                     reads=[kskey, 'onesb'], writes=['B7'])
            ck = f'CT{d}'
            for hp in range(2):
                S.op('dve', lambda e: e.tensor_tensor(out=CT[:, d, 2 * hp:2 * hp + 2, :],
                                                      in0=CT[:, d, 2 * hp:2 * hp + 2, :],
                                                      in1=bk(3 + hp)[:, :].rearrange("p (h v) -> p h v", h=2), op=ALU.add),
                     reads=[f'B{3 + hp}', ck], writes=[ck])
            for h in range(4):
                S.op('act', lambda e: e.activation(out=CT[:, d, h, :], in_=CT[:, d, h, :], func=AF.Copy, scale=aa[:, h:h + 1]),
                     reads=[ck, akey], writes=[ck])
            nk = f'n{d}'
            S.op('dve', lambda e: e.tensor_tensor(out=nst[:, d, :], in0=nst[:, d, :], in1=bk(7)[:, 8:12], op=ALU.add),
                 reads=['B7', nk], writes=[nk])
            S.op('dve', lambda e: e.tensor_tensor(out=nst[:, d, :], in0=nst[:, d, :], in1=aa, op=ALU.mult), reads=[nk, akey], writes=[nk])
            if flag_idx is not None:
                fl = flg[:, flag_idx:flag_idx + 1]
                S.op('dve', lambda e: e.scalar_tensor_tensor(out=CSv[:, d].rearrange("p h v -> p (h v)"),
                                                              in0=CT[:, d].rearrange("p h v -> p (h v)"), scalar=fl,
                                                              in1=CSv[:, d].rearrange("p h v -> p (h v)"), op0=ALU.mult, op1=ALU.add),
                     reads=[ck, 'flags', f'CS{d}'], writes=[f'CS{d}'])
                S.op('dve', lambda e: e.scalar_tensor_tensor(out=nsv[:, d, :], in0=nst[:, d, :], scalar=fl, in1=nsv[:, d, :],
                                                              op0=ALU.mult, op1=ALU.add), reads=[nk, 'flags', f'ns{d}'], writes=[f'ns{d}'])

        sc_n = [0]

        def scan_tile(src_rows, gm, gmk, sh, shk, d, flag_idx):
            i = sc_n[0] % 2
            sc_n[0] += 1
            xp, xk = stage_piece(src_rows, 2048)
            norm_mod(xp, xk, gm, gmk, sh, shk, xnb[i], f'xnb{i}')
            transpose16(xnb[i], f'xnb{i}', xnT[i], f'xnT{i}', 5, 6)
            xt = xnT[i]; xtk = f'xnT{i}'
            for kt in range(16):
                S.op('pe', lambda e: e.matmul(bk(0)[:, :], lhsT=xt[:, kt, :], rhs=Wk[:, kt, :], start=(kt == 0), stop=(kt == 15)),
                     reads=[xtk, 'Wk'], writes=['B0'])
            for vh in range(2):
                for kt in range(16):
                    S.op('pe', lambda e: e.matmul(bk(1 + vh)[:, :], lhsT=xt[:, kt, :], rhs=Wv[:, kt, vh * 512:(vh + 1) * 512],
                                                  start=(kt == 0), stop=(kt == 15)), reads=[xtk, 'Wv'], writes=[f'B{1 + vh}'])
            for kt in range(16):
                S.op('pe', lambda e: e.matmul(bk(7)[:, 16:32], lhsT=xt[:, kt, :], rhs=Wg16[:, kt, :], start=(kt == 0), stop=(kt == 15)),
                     reads=[xtk, 'Wg16'], writes=['B7'])
            gp = gsm[:, 40 + i * 16 - 40 * 0: 40 + i * 16 + 16] if False else gsm[:, 96 + i * 16:96 + i * 16 + 16]
            gk = f'gpre{i}'
            S.op('dve', lambda e: e.tensor_tensor(out=gp, in0=bk(7)[:, 16:32], in1=gbt, op=ALU.add), reads=['B7', 'gb'], writes=[gk])
            wq, wk_, aa, gsk = gates_dir(gp, gk, d)
            ks = ksb[i]; v1 = v1b[i]
            for h in range(4):
                S.op('dve', lambda e: e.tensor_scalar(out=ks[:, h, :], in0=bk(0)[:, h * 128:(h + 1) * 128], scalar1=wk_[:, h:h + 1],
                                                      scalar2=None, op0=ALU.mult), reads=['B0', gsk], writes=[f'ks{i}'])
            for vh in range(2):
                S.op('act', lambda e: e.copy(out=v1[:, 2 * vh:2 * vh + 2, :], in_=bk(1 + vh)[:, :].rearrange("p (h v) -> p h v", h=2)),
                     reads=[f'B{1 + vh}'], writes=[f'v1{i}'])
            state_update(d, ks, f'ks{i}', v1, f'v1{i}', aa, gsk, flag_idx)

        ctx_t = cx.rearrange("(n p) f -> n p f", p=128)
        xb_t = xb.rearrange("(n p) f -> n p f", p=128)
        fwd = [(ctx_t[0], 2, 3, None), (ctx_t[1], 2, 3, 0)] + [(xb_t[j], 0, 1, j + 1) for j in range(NT_B)]
        bwd = [(ctx_t[1], 2, 3, None), (ctx_t[0], 2, 3, 33)] + [(xb_t[NT_B - 1 - j], 0, 1, 34 + j) for j in range(NT_B)]
        if stage.startswith('scan'):
            nst_ = int(stage[4:])
            out_t = out.rearrange("(n p) f -> n p f", p=128)
            if nst_ == 0:
                xp, xk = stage_piece(ctx_t[0], 2048)
                norm_mod(xp, xk, bc[2], 'bc2', bc[3], 'bc3', xnb[0], 'xnb0')
                transpose16(xnb[0], 'xnb0', xnT[0], 'xnT0', 5, 6)
                S.op('dve', lambda e: e.tensor_copy(out=t1, in_=xnT[0].rearrange("p k t -> p (k t)")), reads=['xnT0'], writes=['t1'])
                S.dma('d_out0', out_t[0], t1, reads=['t1'], writes=['out0'])
            else:
                for stp in range(nst_):
                    for d, lst in ((0, fwd), (1, bwd)):
                        src, gi, si, fi = lst[stp]
                        scan_tile(src, bc[gi], f'bc{gi}', bc[si], f'bc{si}', d, 0)
                S.dma('d_out0', out_t[0], CT.rearrange("p d h v -> p (d h v)"), reads=['CT0', 'CT1'], writes=['out0'])
            S.finish('sp')
            return nc
        for stp in range(len(fwd)):
            for d, lst in ((0, fwd), (1, bwd)):
                src, gi, si, fi = lst[stp]
                scan_tile(src, bc[gi], f'bc{gi}', bc[si], f'bc{si}', d, fi)

        S.barrier()
        A.top = M + 16384
        o_wg2 = A.alloc(512); Wg16b = A.bf(o_wg2, 256).rearrange("p (k n) -> p k n", n=16)
        o_CTs = A.alloc(8192); CT2 = A.f32(o_CTs, 2048).rearrange("p (d h v) -> p d h v", d=2, h=4)
        o_n2 = A.alloc(32); n2 = A.f32(o_n2, 8).rearrange("p (d h) -> p d h", d=2)
        o_cw = A.alloc(96); cwt = A.f32(o_cw, 24).rearrange("p (c j) -> p c j", j=3)
        o_hg = A.alloc(4096); hgt = A.f32(o_hg, 1024)
        assert A.top <= M + 32768
        S.op('dve', lambda e: e.tensor_copy(out=CT2.rearrange("p d h v -> p (d h v)"), in_=CSv.rearrange("p d h v -> p (d h v)")),
             writes=['CT0', 'CT1'])
        S.op('dve', lambda e: e.tensor_copy(out=n2.rearrange("p d h -> p (d h)"), in_=nsv.rearrange("p d h -> p (d h)")), writes=['n0', 'n1'])
        S.op('dve', lambda e: e.tensor_copy(out=Wg16b.rearrange("p k n -> p (k n)"), in_=Wg16.rearrange("p k n -> p (k n)")), writes=['Wg16'])
        S.barrier()
        CTo, no_ = CT2, n2
        A.top = o_xn[1] + 4096
        OWN = A.top
        o_q = A.alloc(8 * 512 * 2); qst = A.bf(o_q, 4096).rearrange("p (t n) -> p t n", t=8)
        o_k = A.alloc(8 * 512 * 2); kst = A.bf(o_k, 4096).rearrange("p (t n) -> p t n", t=8)
        o_v = A.alloc(8 * 1024 * 2); vst = A.bf(o_v, 8192).rearrange("p (t n) -> p t n", t=8)
        o_so = A.alloc(8 * 1024 * 2); sgo = A.bf(o_so, 8192).rearrange("p (t n) -> p t n", t=8)
        o_gp = A.alloc(8 * 16 * 4); gpst = A.f32(o_gp, 128).rearrange("p (t n) -> p t n", t=8)
        o_cv = A.alloc(8 * 1024 * 2); convT = A.bf(o_cv, 8192).rearrange("p (c t) -> p c t", c=8)
        S.dma('d_cw', cwt, cw[:, :, :], writes=['cw'])
        S.dma('d_hg', hgt, hg[:, :], writes=['hg'])
        O1 = A.top
        o_xT = A.alloc(16 * 1024 * 2); xTo = A.bf(o_xT, 16384).rearrange("p (k t) -> p k t", k=16)
        o_wc = [A.alloc(16 * 512 * 2) for _ in range(2)]
        Wc = [A.bf(o, 8192).rearrange("p (k n) -> p k n", k=16) for o in o_wc]

        xo_t = xo.rearrange("(n p) f -> n p f", p=128)
        for t in range(NT_OWN):
            xp, xk = stage_piece(xo_t[t], 2048)
            i = t % 2
            norm_mod(xp, xk, bc[0], 'bc0', bc[1], 'bc1', xnb[i], f'xnb{i}')
            transpose16(xnb[i], f'xnb{i}', xTo[:, :, t * 128:(t + 1) * 128], f'xTo{t}', 5, 6)

        for t in range(NT_OWN):
            for kt in range(16):
                S.op('pe', lambda e: e.matmul(bk(7)[:, 16:32], lhsT=xTo[:, kt, t * 128:(t + 1) * 128], rhs=Wg16b[:, kt, :],
                                              start=(kt == 0), stop=(kt == 15)), reads=[f'xTo{t}', 'Wg16'], writes=['B7'])
            S.op('dve', lambda e: e.tensor_tensor(out=gpst[:, t, :], in0=bk(7)[:, 16:32], in1=gbt, op=ALU.add),
                 reads=['B7', 'gb'], writes=[f'gpst{t}'])

        wc_n = [0]

        def load_wchunk(src4):
            i = wc_n[0] % 2
            wc_n[0] += 1
            W = Wc[i]
            for pi in range(4):
                pc, pk = stage_piece(src4[pi].rearrange("p k n -> p (k n)"), 2048)
                eng = ('act', 'dve', 'pool', 'dve')[pi]
                if eng == 'act':
                    S.op('act', lambda e: e.copy(out=W[:, pi * 4:(pi + 1) * 4, :].rearrange("p k n -> p (k n)"), in_=pc), reads=[pk], writes=[f'Wc{i}'])
                else:
                    S.op(eng, lambda e: e.tensor_copy(out=W[:, pi * 4:(pi + 1) * 4, :].rearrange("p k n -> p (k n)"), in_=pc), reads=[pk], writes=[f'Wc{i}'])
            return W, f'Wc{i}'

        for ch in range(6):
            W, wkk = load_wchunk(wtok[ch])
            for t in range(NT_OWN):
                b = t % 2
                for kt in range(16):
                    S.op('pe', lambda e: e.matmul(bk(b)[:, :], lhsT=xTo[:, kt, t * 128:(t + 1) * 128], rhs=W[:, kt, :],
                                                  start=(kt == 0), stop=(kt == 15)), reads=[f'xTo{t}', wkk], writes=[f'B{b}'])
                if ch == 0:
                    S.op('act', lambda e: e.copy(out=qst[:, t, :], in_=bk(b)[:, :]), reads=[f'B{b}'], writes=[f'q{t}'])
                elif ch == 1:
                    S.op('dve', lambda e: e.tensor_copy(out=kst[:, t, :], in_=bk(b)[:, :]), reads=[f'B{b}'], writes=[f'k{t}'])
                elif ch in (2, 3):
                    S.op('act', lambda e: e.copy(out=vst[:, t, (ch - 2) * 512:(ch - 1) * 512], in_=bk(b)[:, :]), reads=[f'B{b}'], writes=[f'v{t}'])
                else:
                    S.op('act', lambda e: e.activation(out=sgo[:, t, (ch - 4) * 512:(ch - 3) * 512], in_=bk(b)[:, :], func=AF.Sigmoid),
                         reads=[f'B{b}'], writes=[f'so{t}'])

        if stage == 'own1a':
            S.barrier()
            out_t = out.rearrange("(n p) f -> n p f", p=128)
            S.dma('d_out0', out_t[0][:, 0:128], gpst.rearrange("p t n -> p (t n)"), writes=['out0'])
            S.finish('sp')
            return nc
        S.barrier()
        A.top = o_wc[0]
        o_cvt = A.alloc(3 * 2048); cvt = [A.f32(o_cvt + i * 2048, 512) for i in range(3)]
        o_wcv = A.alloc(3 * 4096); Wcv = [A.bf(o_wcv + i * 4096, 2048).rearrange("p (k n) -> p k n", k=16) for i in range(3)]
        for ct in range(8):
            for part in range(3):
                pc, pk = stage_piece(wconv[ct, part].rearrange("p k n -> p (k n)"), 2048)
                if part == 0:
                    S.op('act', lambda e: e.copy(out=Wcv[part].rearrange("p k n -> p (k n)"), in_=pc), reads=[pk], writes=[f'Wcv{part}'])
                else:
                    S.op('dve' if part == 1 else 'pool', lambda e: e.tensor_copy(out=Wcv[part].rearrange("p k n -> p (k n)"), in_=pc),
                         reads=[pk], writes=[f'Wcv{part}'])
            for th in range(2):
                for part in range(3):
                    b = 2 + part
                    for kt in range(16):
                        S.op('pe', lambda e: e.matmul(bk(b)[:, :], lhsT=Wcv[part][:, kt, :], rhs=xTo[:, kt, th * 512:(th + 1) * 512],
                                                      start=(kt == 0), stop=(kt == 15)),
                             reads=[f'Wcv{part}'] + [f'xTo{th * 4 + q}' for q in range(4)], writes=[f'B{b}'])
                cs, u, y = cvt
                S.op('act', lambda e: e.copy(out=cs, in_=bk(3)[:, :]), reads=['B3'], writes=['cv_c'])
                S.op('dve', lambda e: e.tensor_tensor(out=u, in0=cs, in1=bk(4)[:, :], op=ALU.mult), reads=['cv_c', 'B4'], writes=['cv_u'])
                u3 = u.rearrange("p (r w) -> p r w", w=64); y3 = y.rearrange("p (r w) -> p r w", w=64)
                S.op('act', lambda e: e.activation(out=y, in_=u, func=AF.Copy, scale=cwt[:, ct, 1:2]), reads=['cv_u', 'cw'], writes=['cv_y'])
                S.op('dve', lambda e: e.scalar_tensor_tensor(out=y3[:, :, 1:64], in0=u3[:, :, 0:63], scalar=cwt[:, ct, 0:1], in1=y3[:, :, 1:64],
                                                             op0=ALU.mult, op1=ALU.add), reads=['cv_u', 'cv_y', 'cw'], writes=['cv_y'])
                S.op('dve', lambda e: e.scalar_tensor_tensor(out=y3[:, :, 0:63], in0=u3[:, :, 1:64], scalar=cwt[:, ct, 2:3], in1=y3[:, :, 0:63],
                                                             op0=ALU.mult, op1=ALU.add), reads=['cv_u', 'cv_y', 'cw'], writes=['cv_y'])
                S.op('dve', lambda e: e.tensor_tensor(out=convT[:, ct, th * 512:(th + 1) * 512], in0=y, in1=bk(2)[:, :], op=ALU.mult),
                     reads=['cv_y', 'B2'], writes=[f'convT{th}'])

        if stage == 'own1b':
            S.barrier()
            out_t = out.rearrange("(n p) f -> n p f", p=128)
            S.dma('d_out0', out_t[0][:, 0:128], gpst.rearrange("p t n -> p (t n)"), writes=['out0'])
            S.finish('sp')
            return nc
        S.barrier()
        A.top = O1
        o_hf = A.alloc(8 * 1024 * 4); hf = A.f32(o_hf, 8192).rearrange("p (t n) -> p t n", t=8)
        o_mx = A.alloc(8 * 1024 * 2); mxT = A.bf(o_mx, 8192).rearrange("p (c t) -> p c t", c=8)
        o_qs = A.alloc(1024); qsb = A.bf(o_qs, 512).rearrange("p (h d) -> p h d", h=4)
        o_ks2 = A.alloc(1024); ks2 = A.bf(o_ks2, 512).rearrange("p (h d) -> p h d", h=4)
        o_qT = A.alloc(1024); qsT = A.bf(o_qT, 512).rearrange("p (h d) -> p h d", h=4)
        o_kT = A.alloc(1024); ksT = A.bf(o_kT, 512).rearrange("p (h d) -> p h d", h=4)
        o_sq = A.alloc(1024); sqk = A.bf(o_sq, 512).rearrange("p (h d) -> p h d", h=4)
        o_CTb = A.alloc(2048); CTb = A.bf(o_CTb, 1024).rearrange("p (h v) -> p h v", h=4)
        o_nb = A.alloc(32); nbb = A.bf(o_nb, 16)
        o_dd = A.alloc(64); ddt = A.f32(o_dd, 16)
        A.top = M + 32768
        o_hs = A.alloc(4096); hs = A.f32(o_hs, 1024)
        o_hq = A.alloc(4096); hq = A.f32(o_hq, 1024)
        o_mb = A.alloc(2048); mxb = A.bf(o_mb, 1024)
        CT = CTo; nst = no_

        class _Stop(Exception):
            pass

        def ckpt(i):
            if stage == f'o2_{i}':
                raise _Stop()

        def own_tile(t, d):
            gk = f'gpst{t}'
            wq, wk_, aa, gsk = gates_dir(gpst[:, t, :], gk, d)
            for h in range(4):
                S.op('dve', lambda e: e.tensor_scalar(out=qsb[:, h, :], in0=qst[:, t, h * 128:(h + 1) * 128], scalar1=wq[:, h:h + 1],
                                                      scalar2=QSCALE, op0=ALU.mult, op1=ALU.mult), reads=[f'q{t}', gsk], writes=['qsb'])
                S.op('pool', lambda e: e.tensor_scalar(out=ks2[:, h, :], in0=kst[:, t, h * 128:(h + 1) * 128], scalar1=wk_[:, h:h + 1],
                                                       scalar2=None, op0=ALU.mult), reads=[f'k{t}', gsk], writes=['ks2'])
            ckpt(1)
            pb = bkbf(5)
            pb6 = bkbf(6)
            for h in range(4):
                S.op('pe', lambda e: e.transpose(out=pb[:, h * 128:(h + 1) * 128], in_=qsb[:, h, :], identity=identb),
                     reads=['qsb', 'identb'], writes=['B5'])
                S.op('pe', lambda e: e.transpose(out=pb6[:, h * 128:(h + 1) * 128], in_=ks2[:, h, :], identity=identb),
                     reads=['ks2', 'identb'], writes=['B6'])
            S.op('act', lambda e: e.copy(out=qsT.rearrange("p h d -> p (h d)"), in_=pb[:, 0:512]), reads=['B5'], writes=['qsT'])
            S.op('dve', lambda e: e.tensor_copy(out=ksT.rearrange("p h d -> p (h d)"), in_=pb6[:, 0:512]), reads=['B6'], writes=['ksT'])
            ckpt(2)
            ck = f'CT{d}'; nk = f'n{d}'
            S.op('act', lambda e: e.copy(out=CTb, in_=CT[:, d]), reads=[ck], writes=['CTb'])
            S.op('dve', lambda e: e.tensor_copy(out=nbb[:, 0:4], in_=nst[:, d, :]), reads=[nk], writes=['nbb'])
            for h in range(4):
                S.op('pe', lambda e: e.matmul(bk(6)[:, h * 128:(h + 1) * 128], lhsT=ksT[:, h, :], rhs=qsT[:, h, :], start=True, stop=True),
                     reads=['ksT', 'qsT'], writes=['B6'])
            ckpt(3)
            msk = Ub if d == 0 else Lb
            for h in range(4):
                S.op('dve', lambda e: e.tensor_tensor(out=sqk[:, h, :], in0=bk(6)[:, h * 128:(h + 1) * 128], in1=msk, op=ALU.mult),
                     reads=['B6', 'Ub', 'Lb'], writes=['sqk'])
            ckpt(4)
            vv = vst[:, t, :].rearrange("p (h v) -> p h v", h=4)
            for h in range(4):
                b = h // 2
                osl = bk(b)[:, (h % 2) * 256:(h % 2) * 256 + 256]
                S.op('pe', lambda e: e.matmul(osl, lhsT=sqk[:, h, :], rhs=vv[:, h, :], start=True, stop=False),
                     reads=['sqk', f'v{t}'], writes=[f'B{b}'])
                S.op('pe', lambda e: e.matmul(osl, lhsT=qsT[:, h, :], rhs=CTb[:, h, :], start=False, stop=True),
                     reads=['qsT', 'CTb'], writes=[f'B{b}'])
                S.op('pe', lambda e: e.matmul(bk(7)[:, 32 + h:33 + h], lhsT=sqk[:, h, :], rhs=onesb[:, 0:1], start=True, stop=False),
                     reads=['sqk', 'onesb'], writes=['B7'])
                S.op('pe', lambda e: e.matmul(bk(7)[:, 32 + h:33 + h], lhsT=qsT[:, h, :], rhs=nbb[:, h:h + 1], start=False, stop=True),
                     reads=['qsT', 'nbb'], writes=['B7'])
            ckpt(5)
            dd = ddt[:, 0:4]
            S.op('act', lambda e: e.activation(out=dd, in_=bk(7)[:, 32:36], func=AF.Abs), reads=['B7'], writes=['dd'])
            S.op('dve', lambda e: e.tensor_scalar(out=dd, in0=dd, scalar1=1.0, scalar2=None, op0=ALU.max), reads=['dd'], writes=['dd'])
            S.op('dve', lambda e: e.reciprocal(out=dd, in_=dd), reads=['dd'], writes=['dd'])
            for h in range(4):
                b = h // 2
                osl = bk(b)[:, (h % 2) * 256:(h % 2) * 256 + 256]
                if d == 0:
                    S.op('act', lambda e: e.activation(out=hf[:, t, h * 256:(h + 1) * 256], in_=osl, func=AF.Copy, scale=dd[:, h:h + 1]),
                         reads=[f'B{b}', 'dd'], writes=[f'hf{t}'])
                else:
                    S.op('dve', lambda e: e.scalar_tensor_tensor(out=hs[:, h * 256:(h + 1) * 256], in0=osl, scalar=dd[:, h:h + 1],
                                                                 in1=hf[:, t, h * 256:(h + 1) * 256], op0=ALU.mult, op1=ALU.add),
                         reads=[f'B{b}', 'dd', f'hf{t}'], writes=['hs'])
            ckpt(6)
            state_update(d, ks2, 'ks2', vv, f'v{t}', aa, gsk, None)
            ckpt(7)
            if d == 1:
                ssq = ddt[:, 4:8]
                S.op('pool', lambda e: e.tensor_tensor(out=hq, in0=hs, in1=hs, op=ALU.mult), reads=['hs'], writes=['hq'])
                S.op('dve', lambda e: e.tensor_reduce(out=ssq, in_=hq.rearrange("p (h v) -> p h v", h=4), axis=AX.X, op=ALU.add),
                     reads=['hq'], writes=['ssq'])
                S.op('dve', lambda e: e.tensor_scalar(out=ssq, in0=ssq, scalar1=1.0 / 256, scalar2=EPS, op0=ALU.mult, op1=ALU.add),
                     reads=['ssq'], writes=['ssq'])
                S.op('act', lambda e: e.activation(out=ssq, in_=ssq, func=AF.Ln), reads=['ssq'], writes=['ssq'])
                S.op('act', lambda e: e.activation(out=ssq, in_=ssq, func=AF.Exp, scale=-0.5), reads=['ssq'], writes=['ssq'])
                for h in range(4):
                    S.op('dve', lambda e: e.scalar_tensor_tensor(out=hq[:, h * 256:(h + 1) * 256], in0=hs[:, h * 256:(h + 1) * 256],
                                                                  scalar=ssq[:, h:h + 1], in1=hgt[:, h * 256:(h + 1) * 256],
                                                                  op0=ALU.mult, op1=ALU.mult), reads=['hs', 'ssq', 'hg'], writes=['hq'])
                S.op('dve', lambda e: e.tensor_tensor(out=mxb, in0=hq, in1=sgo[:, t, :], op=ALU.mult), reads=['hq', f'so{t}'], writes=['mxb'])
                pb2 = bkbf(5)
                for c8 in range(8):
                    S.op('pe', lambda e: e.transpose(out=pb2[:, c8 * 128:(c8 + 1) * 128], in_=mxb[:, c8 * 128:(c8 + 1) * 128], identity=identb),
                         reads=['mxb', 'identb'], writes=['B5'])
                S.op('act', lambda e: e.copy(out=mxT[:, :, t * 128:(t + 1) * 128], in_=pb2.rearrange("p (c t) -> p c t", t=128)),
                     reads=['B5'], writes=[f'mxT{t}'])

        try:
            for t in range(NT_OWN):
                own_tile(t, 0)
            ckpt(8)
            for t in reversed(range(NT_OWN)):
                own_tile(t, 1)
                ckpt(9)
        except _Stop:
            S.barrier()
            out_t = out.rearrange("(n p) f -> n p f", p=128)
            S.dma('d_out0', out_t[0][:, 0:1024], hf[:, 0, :], writes=['out0'])
            S.finish('sp')
            return nc

        if stage == 'own2':
            S.barrier()
            out_t = out.rearrange("(n p) f -> n p f", p=128)
            S.dma('d_out0', out_t[0][:, 0:1024], hf[:, 0, :], writes=['out0'])
            S.finish('sp')
            return nc
        S.barrier()
        A.top = O1
        o_bcx = [A.alloc(8192) for _ in range(3)]
        bcx = [A.f32(o, 2048) for o in o_bcx]
        o_gt2 = A.alloc(8192); gt2bc = A.f32(o_gt2, 2048)
        assert A.top <= o_mx
        A.top = o_mx + 16384
        o_tmp = A.alloc(2048); tmpc = A.f32(o_tmp, 512)
        ada_temps(OWN, g2_2)
        rows = tmpl['rows']; g12 = tmpl['g12']
        assert A.top <= o_cv
        mod_row(2, rows[0], 'row0')
        bcast(bcx[0], 'bcx0', rows[0], 'row0', sel0, 'sel0')
        mod_row(3, rows[0], 'row0')
        mod_row(4, rows[1], 'row1')
        S.op('dve', lambda e: e.scalar_tensor_tensor(out=rows[1][0:2, :], in0=rows[1][0:2, :], scalar=1.0, in1=g12[0:2, :],
                                                     op0=ALU.add, op1=ALU.mult), reads=['row1', 'g12'], writes=['row1'])
        bcast(bcx[1], 'bcx1', rows[1], 'row1', sel0, 'sel0')
        bcast(bcx[2], 'bcx2', rows[0], 'row0', sel0, 'sel0')
        mod_row(5, rows[0], 'row0')
        bcast(gt2bc, 'gt2bc', rows[0], 'row0', sel0, 'sel0')

        if stage == 'ada2':
            S.barrier()
            out_t = out.rearrange("(n p) f -> n p f", p=128)
            S.dma('d_out0', out_t[0], gt2bc, writes=['out0'])
            S.finish('sp')
            return nc
        S.barrier()
        o_x1 = M
        x1 = A.f32(o_x1, 8 * 2048).rearrange("p (t n) -> p t n", t=8)
        A.top = M + 65536
        o_wc2 = [A.alloc(16 * 512 * 2) for _ in range(2)]
        assert A.top <= o_cv, (A.top, o_cv)
        Wc[0] = A.bf(o_wc2[0], 8192).rearrange("p (k n) -> p k n", k=16)
        Wc[1] = A.bf(o_wc2[1], 8192).rearrange("p (k n) -> p k n", k=16)
        for t in range(NT_OWN):
            S.dma(f'd_x1_{t}', x1[:, t, :], xo_t[t], writes=[f'x1_{t}'])
        for ch in range(4):
            W, wkk = load_wchunk(wout[ch])
            for t in range(NT_OWN):
                b = t % 2
                for ft in range(16):
                    lh = convT[:, ft, t * 128:(t + 1) * 128] if ft < 8 else mxT[:, ft - 8, t * 128:(t + 1) * 128]
                    S.op('pe', lambda e: e.matmul(bk(b)[:, :], lhsT=lh, rhs=W[:, ft, :], start=(ft == 0), stop=(ft == 15)),
                         reads=[wkk, f'mxT{t}', 'convT0', 'convT1'], writes=[f'B{b}'])
                S.op('dve', lambda e: e.tensor_tensor(out=tmpc, in0=bk(b)[:, :], in1=bcx[0][:, ch * 512:(ch + 1) * 512], op=ALU.mult),
                     reads=[f'B{b}', 'bcx0'], writes=['tmpc'])
                S.op('pool', lambda e: e.tensor_tensor(out=x1[:, t, ch * 512:(ch + 1) * 512], in0=x1[:, t, ch * 512:(ch + 1) * 512], in1=tmpc,
                                                       op=ALU.add), reads=['tmpc', f'x1_{t}'], writes=[f'x1_{t}'])

        if stage == 'mixer':
            out_t = out.rearrange("(n p) f -> n p f", p=128)
            for t in range(NT_OWN):
                S.dma(f'd_out{t}', out_t[t], x1[:, t, :], reads=[f'x1_{t}'], writes=[f'out{t}'])
            S.finish('sp')
            return nc

        S.barrier()
        o_h2 = M + 65536
        h2tok = A.bf(o_h2, 8 * 2048).rearrange("p (t n) -> p t n", t=8)
        A.top = o_h2 + 32768
        o_t1 = A.alloc(8192); t1 = A.f32(o_t1, 2048)
        o_h2f = A.alloc(8192); h2f = A.f32(o_h2f, 2048)
        assert A.top <= O1 + 8192, (A.top, O1)
        A.top = o_mx
        o_ss = A.alloc(64); ssb = A.f32(o_ss, 16)
        o_h2T = A.alloc(8192); h2T = A.f32(o_h2T, 2048).rearrange("p (k t) -> p k t", t=128)
        o_wr = A.alloc(4096); wrt = A.f32(o_wr, 1024).rearrange("p (k n) -> p k n", n=64)
        o_br = A.alloc(256); brt = A.f32(o_br, 64)
        NV = NE + 2
        o_mask = A.alloc(8 * NV * 4); maskt = A.f32(o_mask, 8 * NV).rearrange("p (t n) -> p t n", t=8)
        o_G = A.alloc(8 * NV * 4); Gt = A.f32(o_G, 8 * NV).rearrange("p (t n) -> p t n", t=8)
        o_pos = A.alloc(8 * NV * 4); posm = A.f32(o_pos, 8 * NV).rearrange("p (t n) -> p t n", t=8)
        o_sc = A.alloc(1024); sct = A.f32(o_sc, 256)
        S.dma('d_wr', wrt, wr[:, :, :], writes=['wr'])
        S.dma('d_br', brt, br[:, :], writes=['br'])
        for t in range(NT_OWN):
            S.dma(f'd_shp{t}', posm[:, t, NE:NE + 2], shpos[:, t, :], writes=[f'posm{t}'])
        for t in range(NT_OWN):
            norm_mod(x1[:, t, :], f'x1_{t}', bcx[1], 'bcx1', bcx[2], 'bcx2', h2f, 'h2f')
            S.op('act', lambda e: e.copy(out=h2tok[:, t, :], in_=h2f), reads=['h2f'], writes=[f'h2tok{t}'])
            for g4 in range(4):
                for k4 in range(4):
                    kt = g4 * 4 + k4
                    S.op('pe', lambda e: e.transpose(out=bk(g4)[:, k4 * 128:(k4 + 1) * 128], in_=h2f[:, kt * 128:(kt + 1) * 128], identity=identf),
                         reads=['h2f', 'identf'], writes=[f'B{g4}'])
                S.op('act' if g4 % 2 == 0 else 'dve',
                     (lambda e: e.copy(out=h2T[:, g4 * 4:(g4 + 1) * 4, :], in_=bk(g4)[:, :].rearrange("p (k t) -> p k t", t=128))) if g4 % 2 == 0 else
                     (lambda e: e.tensor_copy(out=h2T[:, g4 * 4:(g4 + 1) * 4, :], in_=bk(g4)[:, :].rearrange("p (k t) -> p k t", t=128))),
                     reads=[f'B{g4}'], writes=['h2T'])
            for kt in range(16):
                S.op('pe', lambda e: e.matmul(bk(7)[:, 64:128], lhsT=h2T[:, kt, :], rhs=wrt[:, kt, :], start=(kt == 0), stop=(kt == 15)),
                     reads=['h2T', 'wr'], writes=['B7'])
            scr = sct[:, 0:64]; bia = sct[:, 64:128]; top8 = sct[:, 128:136]; den = sct[:, 136:137]
            S.op('act', lambda e: e.activation(out=scr, in_=bk(7)[:, 64:128], func=AF.Sigmoid), reads=['B7'], writes=['scr'])
            S.op('dve', lambda e: e.tensor_tensor(out=bia, in0=scr, in1=brt, op=ALU.add), reads=['scr', 'br'], writes=['bia'])
            S.op('dve', lambda e: e.max(out=top8, in_=bia), reads=['bia'], writes=['top8'])
            S.op('dve', lambda e: e.tensor_scalar(out=maskt[:, t, 0:NE], in0=bia, scalar1=top8[:, 5:6], scalar2=None, op0=ALU.is_ge),
                 reads=['bia', 'top8'], writes=[f'mask{t}'])
            S.op('dve', lambda e: e.tensor_tensor(out=scr, in0=scr, in1=maskt[:, t, 0:NE], op=ALU.mult), reads=['scr', f'mask{t}'], writes=['scr'])
            S.op('dve', lambda e: e.tensor_reduce(out=den, in_=scr, axis=AX.X, op=ALU.add), reads=['scr'], writes=['den'])
            S.op('dve', lambda e: e.reciprocal(out=den, in_=den), reads=['den'], writes=['den'])
            S.op('dve', lambda e: e.tensor_scalar(out=Gt[:, t, 0:NE], in0=scr, scalar1=den, scalar2=RSCALE, op0=ALU.mult, op1=ALU.mult),
                 reads=['scr', 'den'], writes=[f'G{t}'])
            S.op('pool', lambda e: e.memset(Gt[:, t, NE:NE + 2], 1.0), writes=[f'G{t}'])
        for t in range(NT_OWN):
            S.op('pe', lambda e: e.matmul(bk(6)[:, 0:NE], lhsT=Uf, rhs=maskt[:, t, 0:NE], start=True, stop=(t == 0)),
                 reads=['Uf', f'mask{t}'], writes=['B6'])
            for t2 in range(t):
                S.op('pe', lambda e: e.matmul(bk(6)[:, 0:NE], lhsT=onesf, rhs=maskt[:, t2, 0:NE], start=False, stop=(t2 == t - 1)),
                     reads=['onesf', f'mask{t2}'], writes=['B6'])
            S.op('dve', lambda e: e.tensor_tensor(out=posm[:, t, 0:NE], in0=bk(6)[:, 0:NE], in1=maskt[:, t, 0:NE], op=ALU.mult),
                 reads=['B6', f'mask{t}'], writes=[f'posm{t}'])
            S.op('dve', lambda e: e.tensor_scalar(out=posm[:, t, 0:NE], in0=posm[:, t, 0:NE], scalar1=-1.0, scalar2=None, op0=ALU.add),
                 reads=[f'posm{t}'], writes=[f'posm{t}'])

        S.barrier()
        MO = o_h2 + 32768
        A.top = MO
        o_gt2n = A.alloc(8192)
        o_Gn = A.alloc(8 * NV * 4); o_posn = A.alloc(8 * NV * 4)
        stg_f = A.f32(o_stage, 4096)
        S.op('dve', lambda e: e.tensor_copy(out=stg_f[:, 0:2048], in_=gt2bc), writes=['mv'])
        S.op('dve', lambda e: e.tensor_copy(out=stg_f[:, 2048:2048 + 8 * NV], in_=Gt.rearrange("p t n -> p (t n)")), writes=['mv'])
        S.op('dve', lambda e: e.tensor_copy(out=stg_f[:, 2048 + 8 * NV:2048 + 16 * NV], in_=posm.rearrange("p t n -> p (t n)")), writes=['mv'])
        S.barrier()
        gt2bc = A.f32(o_gt2n, 2048)
        Gt = A.f32(o_Gn, 8 * NV).rearrange("p (t n) -> p t n", t=8)
        posm = A.f32(o_posn, 8 * NV).rearrange("p (t n) -> p t n", t=8)
        S.op('dve', lambda e: e.tensor_copy(out=gt2bc, in_=stg_f[:, 0:2048]), writes=['gt2bc'])
        S.op('dve', lambda e: e.tensor_copy(out=Gt.rearrange("p t n -> p (t n)"), in_=stg_f[:, 2048:2048 + 8 * NV]), writes=['G'])
        S.op('dve', lambda e: e.tensor_copy(out=posm.rearrange("p t n -> p (t n)"), in_=stg_f[:, 2048 + 8 * NV:2048 + 16 * NV]), writes=['posm'])
        S.barrier()
        o_P = A.alloc(8 * CAP * 2); Pm = A.bf(o_P, 8 * CAP).rearrange("p (t c) -> p t c", t=8)
        o_R = A.alloc(16 * CAP * 2); xgT = A.bf(o_R, 16 * CAP).rearrange("p (k c) -> p k c", k=16)
        PT = A.bf(o_R, NST * 1024).rearrange("p (s t) -> p s t", s=NST)
        o_hb = A.alloc(NHT * CAP * 2); hbT = A.bf(o_hb, NHT * CAP).rearrange("p (h c) -> p h c", h=NHT)
        o_sg = A.alloc(CAP * 2); sgt = A.bf(o_sg, CAP)
        o_yb = [A.alloc(NST * 512 * 2) for _ in range(2)]
        ybb = [A.bf(o, NST * 512).rearrange("p (s n) -> p s n", s=NST) for o in o_yb]
        o_wgu = [A.alloc(4096) for _ in range(4)]
        Wgu = [A.bf(o, 2048).rearrange("p (k n) -> p k n", k=16) for o in o_wgu]
        o_wd = [A.alloc(NHT * 512 * 2) for _ in range(NWD)]
        Wdb = [A.bf(o, NHT * 512).rearrange("p (h n) -> p h n", h=NHT) for o in o_wd]
        print("MoE arena top", A.top, "of", ARENA_BYTES)
        stage_n[0] = 0

        def piece4(src, nelem):
            return stage_piece(src, nelem, nslots=4, slot_elems=1024)

        wgu_n = [0]; wd_n = [0]; yb_n = [0]
        cast_rr = [0]

        def cast(out_ap, in_ap, rk, wk):
            i = cast_rr[0] % 3
            cast_rr[0] += 1
            if i == 0:
                S.op('act', lambda e: e.copy(out=out_ap, in_=in_ap), reads=[rk], writes=[wk])
            elif i == 1:
                S.op('pool', lambda e: e.tensor_copy(out=out_ap, in_=in_ap), reads=[rk], writes=[wk])
            else:
                S.op('dve', lambda e: e.tensor_copy(out=out_ap, in_=in_ap), reads=[rk], writes=[wk])

        order = [NE, NE + 1] + list(range(NE))
        for e_ in order:
            if e_ < NE:
                sg_, su_, sd_ = weg[e_], weu[e_], wed[e_]
            else:
                sg_, su_, sd_ = wsg, wsu, wsd
            for tt in range(8):
                S.op('dve' if tt % 2 == 0 else 'pool',
                     lambda e: e.tensor_scalar(out=Pm[:, tt, :], in0=iota[:, 0:CAP], scalar1=posm[:, tt, e_:e_ + 1], scalar2=None, op0=ALU.is_equal),
                     reads=['iota', 'posm'], writes=[f'P{tt}'])
            for kt in range(16):
                b = kt % 2
                for tt in range(8):
                    S.op('pe', lambda e: e.matmul(bk(b)[:, 0:CAP], lhsT=h2tok[:, tt, kt * 128:(kt + 1) * 128], rhs=Pm[:, tt, :],
                                                  start=(tt == 0), stop=(tt == 7)), reads=[f'h2tok{tt}', f'P{tt}'], writes=[f'B{b}'],
                         signal=(tt == 7))
                if kt % 2 == 0:
                    S.op('act', lambda e: e.copy(out=xgT[:, kt, :], in_=bk(b)[:, 0:CAP]), reads=[f'B{b}'], writes=[f'R{kt}'])
                else:
                    S.op('dve', lambda e: e.tensor_copy(out=xgT[:, kt, :], in_=bk(b)[:, 0:CAP]), reads=[f'B{b}'], writes=[f'R{kt}'])
            for ht in range(NHT):
                ws = []
                for which, src in ((0, sg_), (1, su_)):
                    i = wgu_n[0] % 4
                    wgu_n[0] += 1
                    Wt = Wgu[i]
                    flat = src[ht].rearrange("p k n -> p (k n)")
                    for hf_ in range(2):
                        pc, pk = piece4(flat[:, hf_ * 1024:(hf_ + 1) * 1024], 1024)
                        cast(Wt[:, hf_ * 8:(hf_ + 1) * 8, :].rearrange("p k n -> p (k n)"), pc, pk, f'Wgu{i}')
                    ws.append((Wt, f'Wgu{i}'))
                par = ht % 2
                bg, bu = 2 + 2 * par, 3 + 2 * par
                for (Wt, wkk), b in zip(ws, (bg, bu)):
                    for kt in range(16):
                        S.op('pe', lambda e: e.matmul(bk(b)[:, 0:CAP], lhsT=Wt[:, kt, :], rhs=xgT[:, kt, :], start=(kt == 0), stop=(kt == 15)),
                             reads=[wkk, f'R{kt}'], writes=[f'B{b}'], signal=(kt == 15))
                S.op('act', lambda e: e.activation(out=sgt, in_=bk(bg)[:, 0:CAP], func=AF.Silu), reads=[f'B{bg}'], writes=['sgt'])
                S.op('dve', lambda e: e.tensor_tensor(out=hbT[:, ht, :], in0=sgt, in1=bk(bu)[:, 0:CAP], op=ALU.mult),
                     reads=['sgt', f'B{bu}'], writes=[f'hb{ht}'])
            for stt in range(NST):
                pb = bkbf(1)
                for tt in range(8):
                    S.op('pe', lambda e: e.transpose(out=pb[:, tt * 128:(tt + 1) * 128], in_=Pm[:, tt, stt * 128:(stt + 1) * 128], identity=identb),
                         reads=[f'P{tt}', 'identb'], writes=['B1'], signal=(tt == 7))
                S.op('act', lambda e: e.copy(out=PT[:, stt, :], in_=pb), reads=['B1'], writes=[f'R{2 * stt}', f'R{2 * stt + 1}'])
            for c8 in range(4):
                i = wd_n[0] % NWD
                wd_n[0] += 1
                Wd = Wdb[i]
                for (h0, h1) in ((0, 2), (2, 4), (4, 6), (6, 8), (8, 10), (10, 11)):
                    nh = h1 - h0
                    pc, pk = piece4(sd_[c8][:, h0:h1, :].rearrange("p h n -> p (h n)"), nh * 512)
                    for hh in range(nh):
                        eng = ('pool', 'dve')[(hh + h0) % 2]
                        S.op(eng, lambda e: e.tensor_tensor(out=Wd[:, h0 + hh, :], in0=pc[:, hh * 512:(hh + 1) * 512],
                                                            in1=gt2bc[:, c8 * 512:(c8 + 1) * 512], op=ALU.mult),
                             reads=[pk, 'gt2bc'], writes=[f'Wd{i}'])
                j = yb_n[0] % 2
                yb_n[0] += 1
                yb = ybb[j]
                for stt in range(NST):
                    db = 6 if stt % 2 == 0 else 2
                    osl = bk(db)[:, 0:512]
                    for ht in range(NHT):
                        S.op('pe', lambda e: e.matmul(osl, lhsT=hbT[:, ht, stt * 128:(stt + 1) * 128], rhs=Wd[:, ht, :],
                                                      start=(ht == 0), stop=(ht == NHT - 1)), reads=[f'hb{ht}', f'Wd{i}'], writes=[f'B{db}'],
                             signal=(ht == NHT - 1))
                    S.op('act', lambda e: e.copy(out=yb[:, stt, :], in_=osl), reads=[f'B{db}'], writes=[f'yb{j}'])
                for tt in range(8):
                    cb = 7 if tt % 2 == 0 else 3
                    osl = bk(cb)[:, 0:512]
                    for stt in range(NST):
                        S.op('pe', lambda e: e.matmul(osl, lhsT=PT[:, stt, tt * 128:(tt + 1) * 128], rhs=yb[:, stt, :],
                                                      start=(stt == 0), stop=(stt == NST - 1)),
                             reads=[f'R{2 * stt}', f'R{2 * stt + 1}', f'yb{j}'], writes=[f'B{cb}'], signal=(stt == NST - 1))
                    S.op('dve' if tt % 2 == 0 else 'pool' if False else 'dve',
                         lambda e: e.scalar_tensor_tensor(out=x1[:, tt, c8 * 512:(c8 + 1) * 512], in0=osl, scalar=Gt[:, tt, e_:e_ + 1],
                                                          in1=x1[:, tt, c8 * 512:(c8 + 1) * 512], op0=ALU.mult, op1=ALU.add),
                         reads=[f'B{cb}', 'G', f'x1_{tt}'], writes=[f'x1_{tt}'])

        S.barrier()
        A.top = MO
        o_fg = A.alloc(8192); fgt = A.f32(o_fg, 2048)
        o_z = A.alloc(8192); zt = A.f32(o_z, 2048)
        o_ot = [A.alloc(8192) for _ in range(2)]
        ot = [A.f32(o, 2048) for o in o_ot]
        o_ss2 = A.alloc(64); ss2 = A.f32(o_ss2, 16)
        S.dma('d_fg', fgt, fg[:, :], writes=['fg'])
        out_t = out.rearrange("(n p) f -> n p f", p=128)
        for t in range(NT_OWN):
            ss = ss2[:, t:t + 1]
            S.op('act', lambda e: e.activation(out=zt, in_=x1[:, t, :], func=AF.Square, accum_out=ss), reads=[f'x1_{t}'], writes=['zt', f'fss{t}'])
            S.op('dve', lambda e: e.tensor_scalar(out=ss, in0=ss, scalar1=1.0 / D, scalar2=EPS, op0=ALU.mult, op1=ALU.add), reads=[f'fss{t}'], writes=[f'fss{t}'])
            S.op('act', lambda e: e.activation(out=ss, in_=ss, func=AF.Ln), reads=[f'fss{t}'], writes=[f'fss{t}'])
            S.op('act', lambda e: e.activation(out=ss, in_=ss, func=AF.Exp, scale=-0.5), reads=[f'fss{t}'], writes=[f'fss{t}'])
            S.op('dve', lambda e: e.scalar_tensor_tensor(out=ot[t % 2], in0=x1[:, t, :], scalar=ss, in1=fgt, op0=ALU.mult, op1=ALU.mult),
                 reads=[f'x1_{t}', f'fss{t}', 'fg'], writes=[f'ot{t % 2}'])
            S.dma(f'd_out{t % 2}', out_t[t], ot[t % 2], reads=[f'ot{t % 2}'], writes=[f'out{t}'])
        S.finish('sp')
    return nc


def _layouts(inp, ne=NE):
    f = np.float32
    c = lambda a: np.ascontiguousarray(a, dtype=f)
    w_in = inp['w_in'][0]
    sh = {}
    sh['wada'] = c(inp['w_ada'][0].reshape(16, 128, 6, D).transpose(2, 0, 1, 3))
    sh['bada2'] = c(np.stack([inp['b_ada'][0], inp['b_ada'][0]]))
    sh['g1_2'] = c(np.stack([inp['norm1_g'][0]] * 2)); sh['g2_2'] = c(np.stack([inp['norm2_g'][0]] * 2))
    kvg = np.concatenate([w_in[:, 3584:4096], w_in[:, 4096:5120], w_in[:, 6144:6160]], axis=1)
    sh['wkvg'] = c(kvg.reshape(16, 128, 1552))
    chunks = [w_in[:, 3072:3584], w_in[:, 3584:4096], w_in[:, 4096:4608], w_in[:, 4608:5120], w_in[:, 5120:5632], w_in[:, 5632:6144]]
    sh['wtok'] = c(np.stack([ch.reshape(4, 4, 128, 512).transpose(0, 2, 1, 3) for ch in chunks]))
    sh['wg16'] = c(w_in[:, 6144:6160].reshape(16, 128, 16).transpose(1, 0, 2))
    wc = np.stack([np.stack([w_in[:, part * 1024 + ct * 128: part * 1024 + (ct + 1) * 128].reshape(16, 128, 128).transpose(1, 0, 2)
                             for part in range(3)]) for ct in range(8)])
    sh['wconv'] = c(wc)
    sh['cw'] = c(inp['conv_w'][0].reshape(3, 8, 128).transpose(2, 1, 0))
    sh['gb'] = c(np.broadcast_to(inp['gate_b'][0].reshape(1, 16), (128, 16)))
    sh['hg'] = c(np.broadcast_to(inp['head_g'][0].reshape(1, 1024), (128, 1024)))
    wo = inp['w_out'][0]
    sh['wout'] = c(np.stack([wo[:, ch * 512:(ch + 1) * 512].reshape(4, 4, 128, 512).transpose(0, 2, 1, 3) for ch in range(4)]))
    sh['wr'] = c(inp['w_router'][0].reshape(16, 128, 64).transpose(1, 0, 2))
    sh['br'] = c(np.broadcast_to(inp['b_router'][0].reshape(1, 64), (128, 64)))
    sh['weg'] = c(inp['we_gate'][0][:ne].reshape(ne, 16, 128, NHT, 128).transpose(0, 3, 2, 1, 4))
    sh['weu'] = c(inp['we_up'][0][:ne].reshape(ne, 16, 128, NHT, 128).transpose(0, 3, 2, 1, 4))
    sh['wed'] = c(inp['we_down'][0][:ne].reshape(ne, NHT, 128, 4, 512).transpose(0, 3, 2, 1, 4))
    sh['wsg'] = c(inp['ws_gate'][0].reshape(16, 128, NHT, 128).transpose(2, 1, 0, 3))
    sh['wsu'] = c(inp['ws_up'][0].reshape(16, 128, NHT, 128).transpose(2, 1, 0, 3))
    sh['wsd'] = c(inp['ws_down'][0].reshape(NHT, 128, 4, 512).transpose(2, 1, 0, 3))
    sh['fg'] = c(np.broadcast_to(inp['final_g'].reshape(1, D), (128, D)))
    sp = np.full((128, 8, 2), -1.0, f)
    for t in range(8):
        sp[:, t, 0 if t < 4 else 1] = (t % 4) * 128 + np.arange(128)
    sh['shpos'] = sp
    per = []
    for core in range(8):
        b, seg = core // 4, core % 4
        m = dict(sh)
        m['xb'] = c(inp['x'][b]); m['xo'] = c(inp['x'][b, seg * 1024:(seg + 1) * 1024]); m['cx'] = c(inp['ctx'][b])
        m['cT'] = c(np.stack([inp['c'][b].reshape(16, 128).T, inp['c_ctx'].reshape(16, 128).T], axis=-1))
        fl = np.zeros((128, 68), f)
        fl[:, seg * 8] = 1.0
        fl[:, 33 + (24 - seg * 8)] = 1.0
        m['flags'] = fl
        per.append(m)
    return per


_NC_CACHE = {}


def kernel(**inputs):
    inp = {k: np.asarray(v) for k, v in inputs.items()}
    if 'full' not in _NC_CACHE:
        _NC_CACHE['full'] = build_nc('full')
    nc = _NC_CACHE['full']
    in_maps = _layouts(inp)
    res = run_bass_kernel_spmd(nc, in_maps, core_ids=list(range(8)))
    outs = [np.asarray(r["out"], dtype=np.float32) for r in res.results]
    full = np.zeros((2, 4096, D), np.float32)
    for core in range(8):
        b, seg = core // 4, core % 4
        full[b, seg * 1024:(seg + 1) * 1024] = outs[core]
    return full
```
